# Optimizing a Trainium2 kernel written in Bass

```python
import jax, jax.numpy as jnp
from jax import lax
import numpy as np


D_MODEL = 1024
BATCH = 2
SEQ = 16384
DEPTH = 2

HEAD_DIM = 64
N_HEADS = D_MODEL // HEAD_DIM
ML_HEADS = N_HEADS // 4
FOX_HEADS = (N_HEADS - ML_HEADS) // 2
MOBA_HEADS = N_HEADS - ML_HEADS - FOX_HEADS
FOX_W = FOX_HEADS * HEAD_DIM
ML_W = ML_HEADS * HEAD_DIM
MOBA_W = MOBA_HEADS * HEAD_DIM
Q_BLOCK = 128
ML_CHUNK = 64
ML_CONV = 4
MOBA_BLOCK = 256
MOBA_TOPK = 3
ALIBI_MAX = 8.0
D_FF = ((8 * D_MODEL // 3 + 255) // 256) * 256
EPS = 1e-6
SPLIT_SIZES = (FOX_W, FOX_W, FOX_W, FOX_HEADS,
               ML_W, ML_W, ML_W, ML_HEADS, ML_HEADS, ML_W,
               MOBA_W, MOBA_W, MOBA_W)
IN_COLS = sum(SPLIT_SIZES)

kernel_name = 'hybrid_fox_mlstm_moba_block'


def _rms(x, g=None):
    xf = x.astype(jnp.float32)
    y = xf * lax.rsqrt(jnp.mean(xf * xf, axis=-1, keepdims=True) + EPS)
    return y if g is None else y * g.astype(jnp.float32)


def _to_heads(x, h):
    b, t, _ = x.shape
    return x.reshape(b, t, h, HEAD_DIM).transpose(0, 2, 1, 3)


def _from_heads(x):
    b, h, t, d = x.shape
    return x.transpose(0, 2, 1, 3).reshape(b, t, h * d)


def _alibi_slopes(h):
    return jnp.exp2(-ALIBI_MAX * jnp.arange(1, h + 1, dtype=jnp.float32) / h)


def _causal_conv(x, w, bias):
    ch = x.shape[-1]
    y = lax.conv_general_dilated(x, w.astype(x.dtype)[:, None, :], window_strides=(1,),
                                 padding=((ML_CONV - 1, 0),),
                                 dimension_numbers=('NWC', 'WIO', 'NWC'),
                                 feature_group_count=ch)
    return y + bias.astype(x.dtype)


def _fox_attention(q, k, v, f_pre):
    b, h, t, d = q.shape
    nq = t // Q_BLOCK
    logf = jax.nn.log_sigmoid(f_pre).transpose(0, 2, 1)
    cum = jnp.cumsum(logf, axis=-1)
    kpos = jnp.arange(t)
    qb = q.reshape(b, h, nq, Q_BLOCK, d).transpose(2, 0, 1, 3, 4)
    cq = cum.reshape(b, h, nq, Q_BLOCK).transpose(2, 0, 1, 3)
    starts = jnp.arange(nq) * Q_BLOCK
    scale = HEAD_DIM ** -0.5

    def block(args):
        qc, cqc, st = args
        qpos = st + jnp.arange(Q_BLOCK)
        s = jnp.einsum('bhqd,bhkd->bhqk', qc, k) * scale
        s = s + (cqc[..., None] - cum[:, :, None, :])
        s = jnp.where(kpos[None, :] <= qpos[:, None], s, -jnp.inf)
        p = jax.nn.softmax(s, axis=-1)
        return jnp.einsum('bhqk,bhkd->bhqd', p, v)

    out = lax.map(block, (qb, cq, starts))
    return out.transpose(1, 2, 0, 3, 4).reshape(b, h, t, d)


def _mlstm(q, k, v, i_pre, f_pre):
    b, h, t, d = q.shape
    nc = t // ML_CHUNK
    k = k * (d ** -0.5)
    itil = i_pre.transpose(0, 2, 1).reshape(b, h, nc, ML_CHUNK)
    logf = jax.nn.log_sigmoid(f_pre).transpose(0, 2, 1).reshape(b, h, nc, ML_CHUNK)
    bcum = jnp.cumsum(logf, axis=-1)
    bend = bcum[..., -1]
    wlog = bend[..., None] - bcum + itil
    qc = q.reshape(b, h, nc, ML_CHUNK, d)
    kc = k.reshape(b, h, nc, ML_CHUNK, d)
    vc = v.reshape(b, h, nc, ML_CHUNK, d)

    def step(carry, xs):
        cmat, nvec, m = carry
        kx, vx, wx, bx = xs
        m_new = jnp.maximum(bx + m, wx.max(-1))
        decay = jnp.exp(bx + m - m_new)
        ws = jnp.exp(wx - m_new[..., None])
        c_new = decay[..., None, None] * cmat + jnp.einsum('bhs,bhsd,bhse->bhde', ws, vx, kx)
        n_new = decay[..., None] * nvec + jnp.einsum('bhs,bhse->bhe', ws, kx)
        return (c_new, n_new, m_new), (cmat, nvec, m)

    init = (jnp.zeros((b, h, d, d), jnp.float32), jnp.zeros((b, h, d), jnp.float32),
            jnp.zeros((b, h), jnp.float32))
    xs = (kc.transpose(2, 0, 1, 3, 4), vc.transpose(2, 0, 1, 3, 4),
          wlog.transpose(2, 0, 1, 3), bend.transpose(2, 0, 1))
    _, (cs, ns, ms) = lax.scan(step, init, xs)
    cs = cs.transpose(1, 2, 0, 3, 4)
    ns = ns.transpose(1, 2, 0, 3)
    ms = ms.transpose(1, 2, 0)

    causal = jnp.tril(jnp.ones((ML_CHUNK, ML_CHUNK), dtype=bool))
    logd = bcum[..., :, None] - bcum[..., None, :] + itil[..., None, :]
    logd = jnp.where(causal, logd, -jnp.inf)
    m_inter = bcum + ms[..., None]
    m_j = jnp.maximum(m_inter, logd.max(-1))
    smat = jnp.einsum('bhcjd,bhcsd->bhcjs', qc, kc) * jnp.exp(logd - m_j[..., None])
    inter = jnp.exp(m_inter - m_j)
    num = (inter[..., None] * jnp.einsum('bhcde,bhcje->bhcjd', cs, qc)
           + jnp.einsum('bhcjs,bhcsd->bhcjd', smat, vc))
    den = inter * jnp.einsum('bhce,bhcje->bhcj', ns, qc) + smat.sum(-1)
    hout = num / jnp.maximum(jnp.abs(den), jnp.exp(-m_j))[..., None]
    return hout.reshape(b, h, t, d)


def _moba_attention(q, k, v, slopes):
    b, h, t, d = q.shape
    nb = -(-t // MOBA_BLOCK)
    pad = nb * MOBA_BLOCK - t
    kb = jnp.pad(k, ((0, 0), (0, 0), (0, pad), (0, 0))).reshape(b, h, nb, MOBA_BLOCK, d)
    vb = jnp.pad(v, ((0, 0), (0, 0), (0, pad), (0, 0))).reshape(b, h, nb, MOBA_BLOCK, d)
    kmean = kb.mean(axis=3)
    qblk = jnp.arange(t) // MOBA_BLOCK
    gate = jnp.einsum('bhtd,bhnd->bhtn', q, kmean)
    gate = jnp.where(jnp.arange(nb)[None, :] < qblk[:, None], gate, -jnp.inf)
    topk = min(MOBA_TOPK, nb)
    _, sel = lax.top_k(gate, topk)
    valid = sel < qblk[:, None]
    nq = t // Q_BLOCK
    xs = (q.reshape(b, h, nq, Q_BLOCK, d).transpose(2, 0, 1, 3, 4),
          sel.reshape(b, h, nq, Q_BLOCK, topk).transpose(2, 0, 1, 3, 4),
          valid.reshape(b, h, nq, Q_BLOCK, topk).transpose(2, 0, 1, 3, 4),
          jnp.arange(nq))
    bi = jnp.arange(b)[:, None, None, None]
    hi = jnp.arange(h)[None, :, None, None]
    scale = HEAD_DIM ** -0.5
    offs = jnp.arange(MOBA_BLOCK)

    def block(args):
        qc, selc, validc, ci = args
        qpos = ci * Q_BLOCK + jnp.arange(Q_BLOCK)
        ks = kb[bi, hi, selc]
        vs = vb[bi, hi, selc]
        kpos_sel = selc[..., None] * MOBA_BLOCK + offs
        dist_sel = (qpos[:, None, None] - kpos_sel).astype(jnp.float32)
        s_sel = (jnp.einsum('bhqd,bhqkld->bhqkl', qc, ks) * scale
                 - slopes[:, None, None, None] * dist_sel)
        s_sel = jnp.where(validc[..., None], s_sel, -jnp.inf)
        own = (ci * Q_BLOCK) // MOBA_BLOCK
        ko = lax.dynamic_index_in_dim(kb, own, axis=2, keepdims=False)
        vo = lax.dynamic_index_in_dim(vb, own, axis=2, keepdims=False)
        dist_own = qpos[:, None] - (own * MOBA_BLOCK + offs)[None, :]
        s_own = (jnp.einsum('bhqd,bhld->bhql', qc, ko) * scale
                 - slopes[:, None, None] * dist_own.astype(jnp.float32))
        s_own = jnp.where(dist_own >= 0, s_own, -jnp.inf)
        s_all = jnp.concatenate([s_sel.reshape(b, h, Q_BLOCK, topk * MOBA_BLOCK), s_own], axis=-1)
        p = jax.nn.softmax(s_all, axis=-1)
        p_sel = p[..., :topk * MOBA_BLOCK].reshape(b, h, Q_BLOCK, topk, MOBA_BLOCK)
        p_own = p[..., topk * MOBA_BLOCK:]
        return (jnp.einsum('bhqkl,bhqkld->bhqd', p_sel, vs)
                + jnp.einsum('bhql,bhld->bhqd', p_own, vo))

    out = lax.map(block, xs)
    return out.transpose(1, 2, 0, 3, 4).reshape(b, h, t, d)


def _mixer(hn, w_in, fox_f_bias, fox_q_g, fox_k_g, ml_conv_w, ml_conv_b, ml_i_bias,
           ml_f_bias, ml_h_g, moba_q_g, moba_k_g):
    proj = (hn @ w_in).astype(jnp.float32)
    idx = [int(i) for i in np.cumsum(SPLIT_SIZES)[:-1]]
    fq, fk, fv, ff, mq, mk, mv, mi, mf, mo, bq, bk, bv = jnp.split(proj, idx, axis=-1)
    y_fox = _fox_attention(_rms(_to_heads(fq, FOX_HEADS), fox_q_g),
                           _rms(_to_heads(fk, FOX_HEADS), fox_k_g),
                           _to_heads(fv, FOX_HEADS),
                           ff + fox_f_bias.astype(jnp.float32))
    qk = jax.nn.silu(_causal_conv(jnp.concatenate([mq, mk], axis=-1), ml_conv_w, ml_conv_b))
    mq, mk = jnp.split(qk, 2, axis=-1)
    h_ml = _mlstm(_to_heads(mq, ML_HEADS), _to_heads(mk, ML_HEADS), _to_heads(mv, ML_HEADS),
                  mi + ml_i_bias.astype(jnp.float32), mf + ml_f_bias.astype(jnp.float32))
    y_ml = _from_heads(_rms(h_ml)) * ml_h_g.astype(jnp.float32) * jax.nn.sigmoid(mo)
    y_moba = _moba_attention(_rms(_to_heads(bq, MOBA_HEADS), moba_q_g),
                             _rms(_to_heads(bk, MOBA_HEADS), moba_k_g),
                             _to_heads(bv, MOBA_HEADS), _alibi_slopes(MOBA_HEADS))
    return jnp.concatenate([_from_heads(y_fox), y_ml, _from_heads(y_moba)], axis=-1)


def setup_inputs(seed: int = 0) -> dict:
    key = jax.random.key(seed)
    ks = jax.random.split(key, 22)
    n = jax.random.normal
    f32 = jnp.float32
    return {
        'x': n(ks[0], (BATCH, SEQ, D_MODEL), f32),
        'c': n(ks[1], (BATCH, D_MODEL), f32),
        'w_ada': n(ks[2], (DEPTH, D_MODEL, 6 * D_MODEL), f32) * (0.5 * D_MODEL ** -0.5),
        'b_ada': n(ks[3], (DEPTH, 6 * D_MODEL), f32) * 0.01,
        'norm1_g': 1.0 + 0.02 * n(ks[4], (DEPTH, D_MODEL), f32),
        'norm2_g': 1.0 + 0.02 * n(ks[5], (DEPTH, D_MODEL), f32),
        'w_in': n(ks[6], (DEPTH, D_MODEL, IN_COLS), f32) * D_MODEL ** -0.5,
        'fox_f_bias': 2.0 + 0.1 * n(ks[7], (DEPTH, FOX_HEADS), f32),
        'fox_q_g': 1.0 + 0.02 * n(ks[8], (DEPTH, HEAD_DIM), f32),
        'fox_k_g': 1.0 + 0.02 * n(ks[9], (DEPTH, HEAD_DIM), f32),
        'ml_conv_w': n(ks[10], (DEPTH, ML_CONV, 2 * ML_W), f32) * ML_CONV ** -0.5,
        'ml_conv_b': 0.01 * n(ks[11], (DEPTH, 2 * ML_W), f32),
        'ml_i_bias': 0.1 * n(ks[12], (DEPTH, ML_HEADS), f32),
        'ml_f_bias': 3.0 + 0.1 * n(ks[13], (DEPTH, ML_HEADS), f32),
        'ml_h_g': 1.0 + 0.02 * n(ks[14], (DEPTH, ML_W), f32),
        'moba_q_g': 1.0 + 0.02 * n(ks[15], (DEPTH, HEAD_DIM), f32),
        'moba_k_g': 1.0 + 0.02 * n(ks[16], (DEPTH, HEAD_DIM), f32),
        'w_out': n(ks[17], (DEPTH, D_MODEL, D_MODEL), f32) * D_MODEL ** -0.5,
        'w_gate_up': n(ks[18], (DEPTH, D_MODEL, 2 * D_FF), f32) * D_MODEL ** -0.5,
        'w_down': n(ks[19], (DEPTH, D_FF, D_MODEL), f32) * D_FF ** -0.5,
    }


def reference(x, c, w_ada, b_ada, norm1_g, norm2_g, w_in, fox_f_bias, fox_q_g, fox_k_g,
              ml_conv_w, ml_conv_b, ml_i_bias, ml_f_bias, ml_h_g, moba_q_g, moba_k_g,
              w_out, w_gate_up, w_down):
    for l in range(DEPTH):
        mod = jax.nn.silu(c) @ w_ada[l] + b_ada[l]
        sh1, sc1, g1, sh2, sc2, g2 = jnp.split(mod[:, None, :], 6, axis=-1)
        hn = (_rms(x, norm1_g[l]) * (1 + sc1) + sh1).astype(x.dtype)
        y = _mixer(hn, w_in[l], fox_f_bias[l], fox_q_g[l], fox_k_g[l], ml_conv_w[l],
                   ml_conv_b[l], ml_i_bias[l], ml_f_bias[l], ml_h_g[l], moba_q_g[l], moba_k_g[l])
        x = x + g1 * (y.astype(x.dtype) @ w_out[l])
        hn = (_rms(x, norm2_g[l]) * (1 + sc2) + sh2).astype(x.dtype)
        gate, up = jnp.split(hn @ w_gate_up[l], 2, axis=-1)
        x = x + g2 * ((jax.nn.silu(gate) * up) @ w_down[l])
    return x
```

```python
import numpy as np
from contextlib import ExitStack
import ml_dtypes
import concourse.bass as bass
import concourse.mybir as mybir
from concourse.bass_utils import run_bass_kernel_spmd

F32 = mybir.dt.float32
BF16 = mybir.dt.bfloat16
AF = mybir.ActivationFunctionType
ALU = mybir.AluOpType
AX = mybir.AxisListType
NPBF = ml_dtypes.bfloat16

D = 1024
B = 2
T = 16384
DEPTH = 2
HD = 64
NCORE = 8
DFF = 2816
FOXH, MLH, MOBH = 6, 4, 6
EPS = 1e-6
IN_COLS = 3342
O_FQ, O_FK, O_FV, O_FF = 0, 384, 768, 1152
O_MQ, O_MK, O_MV, O_MI, O_MF, O_MO = 1158, 1414, 1670, 1926, 1930, 1934
O_BQ, O_BK, O_BV = 2190, 2574, 2958
NEG = -30000.0


class Buf:
    def __init__(self, t=None):
        self.t = t
        self.w = None
        self.r = {}

    def __getitem__(self, idx):
        return self.t[idx]


class View:
    def __init__(self, parent, t):
        self.p = parent
        self.t = t

    def __getitem__(self, idx):
        return self.t[idx]

    @property
    def w(self):
        return self.p.w

    @w.setter
    def w(self, v):
        self.p.w = v

    @property
    def r(self):
        return self.p.r

    @r.setter
    def r(self, v):
        self.p.r = v


GROUPS = [[0, 1, 2, 3], [4, 5, 6, 7]]


class Sched:
    NDMA = 32

    def __init__(self, nc, es):
        self.nc = nc
        self.es = es
        self.eng = {'pe': nc.tensor, 'act': nc.scalar, 'dve': nc.vector, 'pool': nc.gpsimd,
                    'sp': nc.sync}
        self.esem = {k: es.enter_context(nc.semaphore('s_' + k)) for k in ['pe', 'act', 'dve', 'pool', 'cc']}
        self.ecnt = {k: 0 for k in self.esem}
        self.dsem = [es.enter_context(nc.semaphore('d%d' % i)) for i in range(self.NDMA)]
        self.dcnt = [0] * self.NDMA
        self.dnext = 0
        self.known = {k: {} for k in self.eng}
        self.out_tags = {}
        self.ntile = 0

    def tile(self, shape, dtype, name=None):
        self.ntile += 1
        name = name or ('t%d' % self.ntile)
        return Buf(self.es.enter_context(self.nc.sbuf_tensor(name, list(shape), dtype)))

    def psum(self, shape, dtype, name=None):
        self.ntile += 1
        name = name or ('p%d' % self.ntile)
        return Buf(self.es.enter_context(self.nc.psum_tensor(name, list(shape), dtype)))

    def dram_in(self, name, shape, dtype):
        return Buf(self.nc.dram_tensor(name, list(shape), dtype, kind="ExternalInput").ap())

    def dram_out(self, name, shape, dtype):
        return Buf(self.nc.dram_tensor(name, list(shape), dtype, kind="ExternalOutput").ap())

    def dram_int(self, name, shape, dtype):
        return Buf(self.nc.dram_tensor(name, list(shape), dtype).ap())

    def barrier(self):
        deps = {k: v for k, v in self.ecnt.items() if v > 0}
        for i in range(self.NDMA):
            if self.dcnt[i] > 0:
                deps[i] = self.dcnt[i]
        for e in self.eng:
            self._wait(e, dict(deps))

    def collective(self, src, dst, src_ap=None, dst_ap=None):
        self._wait('pool', self._deps('pool', [src], [dst]))
        self.ecnt['cc'] += 1
        sa = src.t if src_ap is None else src_ap
        da = dst.t if dst_ap is None else dst_ap
        self.eng['pool'].collective_compute("AllGather", ALU.bypass, replica_groups=GROUPS,
                                            ins=[sa.opt()], outs=[da.opt()]).then_inc(self.esem['cc'], 1)
        self._mark(('cc', self.ecnt['cc']), [src], [dst])

    def collective_chunks(self, src, dst, nch, rows):
        for ch in range(nch):
            self.collective(src, dst, src.t[ch * rows:(ch + 1) * rows, :], dst.t[ch * 4 * rows:(ch + 1) * 4 * rows, :])

    def idma(self, out_ap, in_ap, idx_ap, reads=(), writes=()):
        deps = self._deps('pool', reads, writes)
        i = self.dnext
        self.dnext = (i + 1) % self.NDMA
        if self.dcnt[i] > 0 and deps.get(i, 0) < self.dcnt[i]:
            deps[i] = self.dcnt[i]
        self._wait('pool', deps)
        self.dcnt[i] += 16
        self.eng['pool'].indirect_dma_start(out=out_ap, out_offset=None, in_=in_ap,
                                            in_offset=bass.IndirectOffsetOnAxis(ap=idx_ap, axis=0)
                                            ).then_inc(self.dsem[i], 16)
        self._mark((i, self.dcnt[i]), reads, writes)

    def _deps(self, e, reads, writes):
        deps = {}

        def add(tag):
            if tag is None:
                return
            k, v = tag
            if deps.get(k, 0) < v:
                deps[k] = v
        for b in reads:
            add(b.w)
        for b in writes:
            add(b.w)
            for k, v in b.r.items():
                add((k, v))
        if e == 'pe':
            deps.pop('pe', None)
        return deps

    def _wait(self, e, deps):
        kn = self.known[e]
        for k, v in deps.items():
            if kn.get(k, 0) < v:
                sem = self.esem[k] if isinstance(k, str) else self.dsem[k]
                self.eng[e].wait_ge(sem, v)
                kn[k] = v

    def _mark(self, tag, reads, writes):
        k, v = tag
        for b in writes:
            b.w = tag
            b.r = {}
        for b in reads:
            if b not in writes:
                if b.r.get(k, 0) < v:
                    b.r[k] = v

    def op(self, e, fn, reads=(), writes=()):
        self._wait(e, self._deps(e, reads, writes))
        ins = fn(self.eng[e])
        self.ecnt[e] += 1
        ins.then_inc(self.esem[e], 1)
        self._mark((e, self.ecnt[e]), reads, writes)

    def dma(self, out_ap, in_ap, reads=(), writes=(), q='sp'):
        deps = self._deps(q, reads, writes)
        i = self.dnext
        self.dnext = (i + 1) % self.NDMA
        if self.dcnt[i] > 0 and deps.get(i, 0) < self.dcnt[i]:
            deps[i] = self.dcnt[i]
        self._wait(q, deps)
        self.dcnt[i] += 16
        self.eng[q].dma_start(out=out_ap, in_=in_ap).then_inc(self.dsem[i], 16)
        tag = (i, self.dcnt[i])
        self._mark(tag, reads, writes)
        return tag

    def finish(self, out_bufs):
        deps = {}
        for b in out_bufs:
            if b.w is not None:
                k, v = b.w
                deps[k] = max(deps.get(k, 0), v)
        for i in range(self.NDMA):
            if self.dcnt[i] > 0:
                deps[i] = self.dcnt[i]
        self.known['sp'] = {}
        self._wait('sp', deps)


def new_nc():
    return bass.Bass("TRN2", target_bir_lowering=False)


def run_spmd(nc, in_maps):
    res = run_bass_kernel_spmd(nc, in_maps, core_ids=list(range(NCORE)))
    return res.results


MODC = 6 * D // NCORE
WCOLS_L = 8 * IN_COLS + 8 * D + 8 * 2 * DFF + 22 * D
WCOLS = DEPTH * WCOLS_L
WC_CORE = (WCOLS + NCORE - 1) // NCORE
WC_TILE = 2048
WC_CORE = ((WC_CORE + WC_TILE - 1) // WC_TILE) * WC_TILE


def build_prep():
    nc = new_nc()
    with ExitStack() as es:
        S = Sched(nc, es)
        cT = S.dram_in("cT", [128, 8, B], F32)
        wada = S.dram_in("wada", [128, DEPTH, 8, MODC], F32)
        bada = S.dram_in("bada", [B, DEPTH, MODC], F32)
        wf = S.dram_in("wf", [128, WC_CORE], F32)
        mod = S.dram_out("mod", [B, DEPTH, MODC], F32)
        wb = S.dram_out("wb", [128, WC_CORE], BF16)

        c_sb = S.tile([128, 8, B], F32)
        sc_sb = S.tile([128, 8, B], F32)
        sg_sb = S.tile([128, 8, B], F32)
        b_sb = S.tile([B, DEPTH, MODC], F32)
        o_sb = S.tile([B, DEPTH, MODC], F32)
        S.dma(c_sb[:], cT[:], reads=[cT], writes=[c_sb])
        S.dma(b_sb[:], bada[:], reads=[bada], writes=[b_sb])
        S.op('act', lambda e: e.activation(out=sg_sb[:], in_=c_sb[:], func=AF.Sigmoid),
             reads=[c_sb], writes=[sg_sb])
        S.op('dve', lambda e: e.tensor_tensor(out=sc_sb[:], in0=c_sb[:], in1=sg_sb[:], op=ALU.mult),
             reads=[c_sb, sg_sb], writes=[sc_sb])
        wts = [S.tile([128, 8, MODC], F32) for _ in range(2)]
        pss = [S.psum([B, 512], F32) for _ in range(2)]
        pi = 0
        for l in range(DEPTH):
            wt = wts[l % 2]
            S.dma(wt[:], wada[:, l, :, :], reads=[wada], writes=[wt])
            for (c0, cn) in ((0, 512), (512, MODC - 512)):
                ps = pss[pi % 2]
                pi += 1
                for k in range(8):
                    S.op('pe', lambda e, k=k, ps=ps, wt=wt, c0=c0, cn=cn: e.matmul(
                        ps[:, 0:cn], sc_sb[:, k, :], wt[:, k, c0:c0 + cn], start=(k == 0), stop=(k == 7)),
                        reads=[sc_sb, wt], writes=[ps])
                S.op('dve', lambda e, ps=ps, l=l, c0=c0, cn=cn: e.tensor_tensor(
                    out=o_sb[:, l, c0:c0 + cn], in0=ps[:, 0:cn], in1=b_sb[:, l, c0:c0 + cn], op=ALU.add),
                    reads=[ps, b_sb], writes=[o_sb])
        S.dma(mod[:], o_sb[:], reads=[o_sb], writes=[mod])
        fts = [S.tile([128, WC_TILE], F32) for _ in range(3)]
        bts = [S.tile([128, WC_TILE], BF16) for _ in range(3)]
        for i in range(WC_CORE // WC_TILE):
            ft = fts[i % 3]
            bt = bts[i % 3]
            sl = slice(i * WC_TILE, (i + 1) * WC_TILE)
            S.dma(ft[:], wf[:, sl], reads=[wf], writes=[ft])
            eng = 'dve' if i % 2 == 0 else 'pool'
            S.op(eng, lambda e, ft=ft, bt=bt: e.tensor_copy(out=bt[:], in_=ft[:]), reads=[ft], writes=[bt])
            S.dma(wb[:, sl], bt[:], reads=[bt], writes=[wb])
        S.finish([mod, wb])
    return nc


def w_to_pk(w):
    K, N = w.shape
    return np.ascontiguousarray(w.reshape(K // 128, 128, N).transpose(1, 0, 2)).reshape(128, -1)


def run_prep(c, w_ada, b_ada, w_in, w_out, w_gate_up, w_down):
    cT = np.ascontiguousarray(c.T.reshape(8, 128, B).transpose(1, 0, 2))
    flat = []
    for l in range(DEPTH):
        flat += [w_to_pk(w_in[l]), w_to_pk(w_out[l]), w_to_pk(w_gate_up[l]), w_to_pk(w_down[l])]
    flat = np.concatenate(flat, axis=1)
    pad = NCORE * WC_CORE - flat.shape[1]
    flat = np.concatenate([flat, np.zeros((128, pad), np.float32)], axis=1)
    in_maps = []
    for i in range(NCORE):
        wsl = w_ada[:, :, i * MODC:(i + 1) * MODC]
        wsl = np.ascontiguousarray(wsl.reshape(DEPTH, 8, 128, MODC).transpose(2, 0, 1, 3))
        bsl = np.ascontiguousarray(np.broadcast_to(b_ada[None, :, i * MODC:(i + 1) * MODC], (B, DEPTH, MODC)))
        in_maps.append({"cT": cT, "wada": wsl, "bada": bsl,
                        "wf": np.ascontiguousarray(flat[:, i * WC_CORE:(i + 1) * WC_CORE])})
    res = run_spmd(build_prep(), in_maps)
    mod = np.concatenate([r["mod"] for r in res], axis=2)
    mod = np.ascontiguousarray(mod.transpose(1, 0, 2))
    wbf = np.concatenate([r["wb"] for r in res], axis=1)[:, :WCOLS]
    ws = []
    off = 0
    for l in range(DEPTH):
        d = {}
        for name, kk, n in (("w_in", 8, IN_COLS), ("w_out", 8, D), ("w_gu", 8, 2 * DFF), ("w_down", 22, D)):
            d[name] = wbf[:, off:off + kk * n].reshape(128, kk, n)
            off += kk * n
        ws.append(d)
    return mod, ws


NQG = T // 512


class MixCtx:
    pass


def load_hn_plain(S, hn, hnT, tt):
    S.dma(hn[:], hnT[:, :, tt * 512:(tt + 1) * 512], reads=[hnT], writes=[hn])


def load_hn_gathered(S, hn, hna, tt):
    q = tt // 8
    src = hna.t.rearrange("(k r p) t -> r p k t", k=8, r=4)[q]
    S.dma(hn[:], src[:, :, (tt % 8) * 512:(tt % 8 + 1) * 512], reads=[hna], writes=[hn])


def load_w(S, C, wu, wu_d):
    if C.w32 is None:
        S.dma(wu[:], wu_d[:], reads=[wu_d], writes=[wu])
    else:
        n = wu_d.t.shape[2]
        S.dma(C.w32[:, :, 0:n], wu_d[:], reads=[wu_d], writes=[C.w32])
        S.op('dve', lambda e: e.tensor_copy(out=wu[:], in_=C.w32[:, :, 0:n]), reads=[C.w32], writes=[wu])


def mix_common(S, banks=None, fused=False):
    C = MixCtx()
    C.load_hn = load_hn_gathered if fused else load_hn_plain
    C.w32 = S.tile([128, 8, 260], F32) if fused else None
    C.banks = banks if banks is not None else [S.psum([128, 512], F32) for _ in range(8)]
    C.ones64 = S.tile([128, 128], F32)
    S.op('dve', lambda e: e.memset(C.ones64[:], 1.0), writes=[C.ones64])
    C.epsc = S.tile([128, 1], F32)
    S.op('dve', lambda e: e.memset(C.epsc[:], EPS), writes=[C.epsc])
    C.lnsc = S.tile([128, 1], F32)
    S.op('dve', lambda e: e.memset(C.lnsc[:], float(np.log(HD ** -0.5))), writes=[C.lnsc])
    C.sel = S.tile([65, 64], F32)
    S.op('dve', lambda e: e.memset(C.sel[:], 0.0), writes=[C.sel])
    S.op('dve', lambda e: e.memset(C.sel[64:65, :], 1.0), writes=[C.sel])
    C.hn = [S.tile([128, 8, 512], BF16) for _ in range(2)]
    C.sq = [S.tile([128, 512], F32) for _ in range(2)]
    C.rs = [S.tile([128, 512], F32) for _ in range(2)]
    return C


def attn_alloc(S, C, with_pageB):
    C.Qa = S.tile([128, T], BF16)
    C.QaB = S.tile([128, T // 2], BF16) if with_pageB else None
    C.Ka = S.tile([128, T], BF16)
    C.Va = S.tile([128, T // 128, 65], BF16)
    S.op('pool', lambda e: e.memset(C.Va[:], 1.0), writes=[C.Va])
    C.P = [S.tile([128, 512], BF16) for _ in range(4)]
    C.M = [S.tile([128, 512], F32) for _ in range(2)]
    C.dm = S.tile([128, 4, 512], F32)
    C.osb = [S.tile([65, 512], F32) for _ in range(2)]
    C.rd = [S.tile([64, 512], F32) for _ in range(2)]
    C.ysb = [S.tile([64, 512], BF16) for _ in range(2)]


def emit_qknorm(S, C, ps, lo, gcol, dests, idx):
    hi = lo + 64
    sq = C.sq[idx % 2]
    rs = C.rs[idx % 2]
    bSS = C.banks[3 + 4 * (idx % 2)]
    S.op('act', lambda e: e.activation(out=sq[lo:hi, :], in_=ps[lo:hi, :], func=AF.Square),
         reads=[ps], writes=[sq])
    S.op('pe', lambda e: e.matmul(bSS[lo:hi, :], C.ones64[lo:hi, lo:hi], sq[lo:hi, :], start=True, stop=True),
         reads=[C.ones64, sq], writes=[bSS])
    S.op('act', lambda e: e.activation(out=sq[lo:hi, :], in_=bSS[lo:hi, :], func=AF.Sqrt, bias=C.epsc[lo:hi, 0:1],
                                       scale=1.0 / HD), reads=[bSS, C.epsc], writes=[sq])
    S.op('dve', lambda e: e.reciprocal(out=rs[lo:hi, :], in_=sq[lo:hi, :]), reads=[sq], writes=[rs])
    for ap, bufs in dests:
        S.op('dve', lambda e, ap=ap: e.scalar_tensor_tensor(out=ap, in0=ps[lo:hi, :], scalar=gcol, in1=rs[lo:hi, :],
                                                            op0=ALU.mult, op1=ALU.mult),
             reads=[ps, rs], writes=bufs)


def emit_attention(S, C, yout, prow, drow, dsplit=False):
    Qa, QaB, Ka, Va, dm = C.Qa, C.QaB, C.Ka, C.Va, C.dm
    banks = C.banks
    for g in range(NQG):
        n = 4 * g + 4
        bO = banks[4 + g % 2]
        q0 = g * 512

        def emitS(kt):
            lo, hi = drow if kt >= 4 * g else prow
            if kt < 64 or QaB is None:
                Qp, qc = Qa, q0
            else:
                Qp, qc = QaB, q0 - T // 2
            bS = banks[kt % 4]
            if dsplit and 4 * g <= kt < 4 * g + 2:
                S.op('pe', lambda e: e.matmul(bS[:, 0:256], Ka[lo:hi, kt * 128:(kt + 1) * 128], Qp[lo:hi, qc:qc + 256],
                                              start=True, stop=True), reads=[Ka, Qp], writes=[bS])
                plo, phi = prow
                S.op('pe', lambda e: e.matmul(bS[:, 256:512], Ka[plo:phi, kt * 128:(kt + 1) * 128],
                                              Qp[plo:phi, qc + 256:qc + 512], start=True, stop=True),
                     reads=[Ka, Qp], writes=[bS])
            else:
                S.op('pe', lambda e: e.matmul(bS[:, :], Ka[lo:hi, kt * 128:(kt + 1) * 128], Qp[lo:hi, qc:qc + 512],
                                              start=True, stop=True), reads=[Ka, Qp], writes=[bS])
            P = C.P[kt % 4]
            if kt >= 4 * g:
                M = C.M[kt % 2]
                S.op('dve', lambda e: e.tensor_tensor(out=M[:], in0=bS[:], in1=dm[:, kt - 4 * g, :], op=ALU.add),
                     reads=[bS, dm], writes=[M])
                S.op('act', lambda e: e.activation(out=P[:], in_=M[:], func=AF.Exp), reads=[M], writes=[P])
            else:
                S.op('act', lambda e: e.activation(out=P[:], in_=bS[:], func=AF.Exp), reads=[bS], writes=[P])

        def emitO(kt):
            P = C.P[kt % 4]
            S.op('pe', lambda e: e.matmul(bO[0:65, :], Va[:, kt, :], P[:], start=(kt == 0), stop=(kt == n - 1)),
                 reads=[Va, P], writes=[bO])
        LAG = 2
        for i in range(n + LAG):
            if i < n:
                emitS(i)
            if i >= LAG:
                emitO(i - LAG)
        osb = C.osb[g % 2]
        rd = C.rd[g % 2]
        ysb = C.ysb[g % 2]
        bD = banks[6]
        S.op('act', lambda e: e.activation(out=osb[:], in_=bO[0:65, :], func=AF.Copy), reads=[bO], writes=[osb])
        S.op('pe', lambda e: e.matmul(bD[0:64, :], C.sel[:, :], osb[:], start=True, stop=True),
             reads=[C.sel, osb], writes=[bD])
        S.op('dve', lambda e: e.reciprocal(out=rd[:], in_=bD[0:64, :]), reads=[bD], writes=[rd])
        S.op('dve', lambda e: e.tensor_tensor(out=ysb[:], in0=osb[0:64, :], in1=rd[:], op=ALU.mult),
             reads=[osb, rd], writes=[ysb])
        S.dma(yout[0:64, q0:q0 + 512], ysb[:], reads=[ysb], writes=[yout])


def emit_proj_mm(S, C, hnT, wu, tt, qcols, kcols, vcols):
    hn = C.hn[tt % 2]
    C.load_hn(S, hn, hnT, tt)
    st = 4 * (tt % 2)
    bQ, bK, bV = C.banks[st], C.banks[st + 1], C.banks[st + 2]
    nq = qcols[1] - qcols[0]
    nk = kcols[1] - kcols[0]
    nv = vcols[1] - vcols[0]
    for k in range(8):
        S.op('pe', lambda e, k=k: e.matmul(bQ[0:nq, :], wu[:, k, qcols[0]:qcols[1]], hn[:, k, :],
                                           start=(k == 0), stop=(k == 7)), reads=[wu, hn], writes=[bQ])
    for k in range(8):
        S.op('pe', lambda e, k=k: e.matmul(bK[0:nk, :], wu[:, k, kcols[0]:kcols[1]], hn[:, k, :],
                                           start=(k == 0), stop=(k == 7)), reads=[wu, hn], writes=[bK])
    for j in range(4):
        for k in range(8):
            S.op('pe', lambda e, k=k, j=j: e.matmul(bV[:, j * nv:(j + 1) * nv], hn[:, k, j * 128:(j + 1) * 128],
                                                    wu[:, k, vcols[0]:vcols[1]], start=(k == 0), stop=(k == 7)),
                 reads=[wu, hn], writes=[bV])
    return bQ, bK, bV


def fox_alloc(S, C):
    C.e1 = S.tile([65, 512], F32)
    C.lf = S.tile([65, 512], F32)
    C.cn = [S.tile([65, 512], F32) for _ in range(2)]
    C.rt = [S.tile([65, 512], F32) for _ in range(2)]
    C.stg = [S.tile([65, 3, 512], BF16) for _ in range(2)]
    C.onesrow = S.tile([65, 512], F32)
    S.op('dve', lambda e: e.memset(C.onesrow[:], 1.0), writes=[C.onesrow])
    C.fwu = S.tile([128, 8, 200], BF16)
    C.fg = S.tile([64, 2], F32)
    C.fgs = S.tile([64, 1], F32)
    C.ffb = S.tile([65, 1], F32)
    C.fnb = S.tile([65, 1], F32)


def emit_fox_unit(S, C, hnT, wu_d, g_d, fb_d, dm_d, yout):
    Qa, Ka, Va = C.Qa, C.Ka, C.Va
    wu, gq, gs, fb, nfb = C.fwu, C.fg, C.fgs, C.ffb, C.fnb
    load_w(S, C, wu, wu_d)
    S.dma(gq[:], g_d[:], reads=[g_d], writes=[gq])
    S.dma(fb[64:65, :], fb_d[64:65, :], reads=[fb_d], writes=[fb])
    S.dma(C.dm[:], dm_d[:], reads=[dm_d], writes=[C.dm])
    S.op('dve', lambda e: e.tensor_scalar(out=gs[:], in0=gq[:, 0:1], scalar1=HD ** -0.5, scalar2=None,
                                          op0=ALU.mult), reads=[gq], writes=[gs])
    S.op('dve', lambda e: e.tensor_scalar(out=nfb[64:65, :], in0=fb[64:65, :], scalar1=-1.0, scalar2=None,
                                          op0=ALU.mult), reads=[fb], writes=[nfb])
    S.op('pool', lambda e: e.memset(Qa[64:70, :], 1.0), writes=[Qa])
    S.op('pool', lambda e: e.memset(Ka[64:70, :], -1.0), writes=[Ka])
    for tt in range(T // 512):
        cs = slice(tt * 512, (tt + 1) * 512)
        bQ, bK, bV = emit_proj_mm(S, C, hnT, wu, tt, (0, 65), (65, 129), (129, 193))
        emit_qknorm(S, C, bQ, 0, gs[:, 0:1], [(Qa[0:64, cs], [Qa])], 2 * tt)
        emit_qknorm(S, C, bK, 0, gq[:, 1:2], [(Ka[0:64, cs], [Ka])], 2 * tt + 1)
        S.op('act', lambda e: e.activation(out=Va[:, 4 * tt:4 * tt + 4, 0:64],
                                           in_=bV[:, 0:256].rearrange("p (j d) -> p j d", j=4), func=AF.Copy),
             reads=[bV], writes=[Va])
        e1, lf = C.e1, C.lf
        cn, cnp = C.cn[tt % 2], C.cn[(tt + 1) % 2]
        rt = C.rt
        stg = C.stg[tt % 2]
        r = slice(64, 65)
        S.op('act', lambda e: e.activation(out=e1[r, :], in_=bQ[r, :], func=AF.Exp, bias=nfb[r, 0:1], scale=-1.0),
             reads=[bQ, nfb], writes=[e1])
        S.op('act', lambda e: e.activation(out=lf[r, :], in_=e1[r, :], func=AF.Ln, bias=1.0, scale=1.0),
             reads=[e1], writes=[lf])
        init = 0.0 if tt == 0 else cnp[r, 511:512]
        S.op('dve', lambda e: e.tensor_tensor_scan(out=cn[r, :], data0=C.onesrow[r, :], data1=lf[r, :], initial=init,
                                                   op0=ALU.mult, op1=ALU.add),
             reads=[C.onesrow, lf, cnp], writes=[cn])
        S.op('dve', lambda e: e.tensor_copy(out=stg[r, 0, :], in_=cn[r, :]), reads=[cn], writes=[stg])
        S.op('dve', lambda e: e.tensor_tensor(out=rt[0][r, :], in0=cn[r, :], in1=stg[r, 0, :], op=ALU.subtract),
             reads=[cn, stg], writes=[rt[0]])
        S.op('dve', lambda e: e.tensor_copy(out=stg[r, 1, :], in_=rt[0][r, :]), reads=[rt[0]], writes=[stg])
        S.op('dve', lambda e: e.tensor_tensor(out=rt[1][r, :], in0=rt[0][r, :], in1=stg[r, 1, :], op=ALU.subtract),
             reads=[rt[0], stg], writes=[rt[1]])
        S.op('dve', lambda e: e.tensor_copy(out=stg[r, 2, :], in_=rt[1][r, :]), reads=[rt[1]], writes=[stg])
        for i in range(3):
            S.dma(Qa[64 + i:65 + i, cs], stg[r, i, :], reads=[stg], writes=[Qa])
            S.dma(Ka[67 + i:68 + i, cs], stg[r, i, :], reads=[stg], writes=[Ka])
    emit_attention(S, C, yout, (0, 70), (0, 70))


def fox_dmask():
    m = np.zeros((128, 4, 512), np.float32)
    k = np.arange(128)[:, None]
    q = np.arange(512)[None, :]
    for a in range(4):
        m[:, a, :] = np.where(a * 128 + k <= q, 0.0, NEG)
    return m


def build_mix(kinds):
    nc = new_nc()
    with ExitStack() as es:
        S = Sched(nc, es)
        hnT = S.dram_in("hnT", [128, 8, T], BF16)
        C = mix_common(S)
        outs = []
        if 'fox' in kinds or 'moba' in kinds:
            attn_alloc(S, C, 'moba' in kinds)
        if 'fox' in kinds:
            fox_alloc(S, C)
            fdm = S.dram_in("fox_dm", [128, 4, 512], F32)
        if 'ml' in kinds:
            ml_alloc(S, C)
            mU = S.dram_in("ml_U", [128, 128], F32)
        if 'moba' in kinds or 'ml' in kinds:
            idd = S.dram_in("ident", [128, 128], F32)
        if 'moba' in kinds:
            moba_alloc(S, C)
            bdm = S.dram_in("moba_dm", [128, 4, 512], F32)
            boh = S.dram_in("moba_oh", [32, T], BF16)
        for u, kind in enumerate(kinds):
            if kind == 'fox':
                wu_d = S.dram_in("wu%d" % u, [128, 8, 200], BF16)
                g_d = S.dram_in("g%d" % u, [64, 2], F32)
                fb_d = S.dram_in("fb%d" % u, [65, 1], F32)
                yout = S.dram_out("y%d" % u, [64, T], BF16)
                emit_fox_unit(S, C, hnT, wu_d, g_d, fb_d, fdm, yout)
                outs.append(yout)
            elif kind == 'moba':
                wu_d = S.dram_in("wu%d" % u, [128, 8, 200], BF16)
                g_d = S.dram_in("g%d" % u, [64, 2], F32)
                qc_d = S.dram_in("qc%d" % u, [8, T], BF16)
                kc_d = S.dram_in("kc%d" % u, [8, T], BF16)
                yout = S.dram_out("y%d" % u, [64, T], BF16)
                emit_moba_unit(S, C, hnT, wu_d, g_d, qc_d, kc_d, boh, bdm, idd, yout)
                outs.append(yout)
            elif kind == 'ml':
                wu_d = S.dram_in("wu%d" % u, [128, 8, 260], BF16)
                cw_d = S.dram_in("cw%d" % u, [64, 2, 4], F32)
                cb_d = S.dram_in("cb%d" % u, [64, 2], F32)
                ib_d = S.dram_in("ib%d" % u, [128, 1], F32)
                fb_d = S.dram_in("fb%d" % u, [128, 1], F32)
                g_d = S.dram_in("mg%d" % u, [128, 4, 64], F32)
                yout = S.dram_out("y%d" % u, [64, T], BF16)
                emit_ml_unit(S, C, hnT, wu_d, cw_d, cb_d, ib_d, fb_d, g_d, mU, idd, yout)
                outs.append(yout)
        S.finish(outs)
    return nc


def to_fm(a):
    n, f = a.shape
    return np.ascontiguousarray(a.reshape(n, f // 128, 128).transpose(2, 1, 0))


def fox_unit_inputs(u, ws_l, inp, l, h):
    w = ws_l['w_in']
    wu = np.zeros((128, 8, 200), w.dtype)
    wu[:, :, 0:64] = w[:, :, O_FQ + 64 * h:O_FQ + 64 * h + 64]
    wu[:, :, 64] = w[:, :, O_FF + h]
    wu[:, :, 65:129] = w[:, :, O_FK + 64 * h:O_FK + 64 * h + 64]
    wu[:, :, 129:193] = w[:, :, O_FV + 64 * h:O_FV + 64 * h + 64]
    g = np.stack([inp['fox_q_g'][l], inp['fox_k_g'][l]], axis=1).astype(np.float32)
    fb = np.zeros((65, 1), np.float32)
    fb[64, 0] = inp['fox_f_bias'][l][h]
    return {"wu%d" % u: wu, "g%d" % u: np.ascontiguousarray(g), "fb%d" % u: fb}


def moba_alloc(S, C):
    C.bwu = S.tile([128, 8, 200], BF16)
    C.bg = S.tile([64, 2], F32)
    C.bgs = S.tile([64, 1], F32)
    C.qn32 = [S.tile([64, 512], F32) for _ in range(2)]
    C.kn32 = [S.tile([64, 512], F32) for _ in range(2)]
    C.kmT = S.tile([64, 64], F32)
    C.gsb = S.tile([128, 4, 64], F32)
    C.m8 = S.tile([128, 4, 8], F32)
    C.thr = S.tile([128, 4], F32)
    C.MB = S.tile([128, 4, 2, 128], F32)
    S.op('pool', lambda e: e.memset(C.MB[:], 0.0), writes=[C.MB])
    C.ident = S.tile([128, 128], F32)
    C.mstg = [S.tile([32, 512], BF16) for _ in range(4)]


def emit_moba_unit(S, C, hnT, wu_d, g_d, qc_d, kc_d, oh_d, dm_d, id_d, yout):
    Qa, QaB, Ka, Va = C.Qa, C.QaB, C.Ka, C.Va
    wu, gq, gs = C.bwu, C.bg, C.bgs
    H2 = T // 2
    load_w(S, C, wu, wu_d)
    S.dma(gq[:], g_d[:], reads=[g_d], writes=[gq])
    S.dma(C.dm[:], dm_d[:], reads=[dm_d], writes=[C.dm])
    S.dma(C.ident[:], id_d[:], reads=[id_d], writes=[C.ident])
    S.op('dve', lambda e: e.tensor_scalar(out=gs[:], in0=gq[:, 0:1], scalar1=HD ** -0.5, scalar2=None,
                                          op0=ALU.mult), reads=[gq], writes=[gs])
    S.op('pool', lambda e: e.memset(Qa[64:96, :], 0.0), writes=[Qa])
    S.op('pool', lambda e: e.memset(QaB[64:96, :], 0.0), writes=[QaB])
    S.op('pool', lambda e: e.memset(Ka[64:96, :], 0.0), writes=[Ka])
    S.dma(Qa[64:72, :], qc_d[:], reads=[qc_d], writes=[Qa])
    S.dma(QaB[64:72, :], qc_d[:, H2:], reads=[qc_d], writes=[QaB])
    S.dma(Ka[64:72, :], kc_d[:], reads=[kc_d], writes=[Ka])
    S.dma(Ka[96:128, :], oh_d[:], reads=[oh_d], writes=[Ka])
    S.op('dve', lambda e: e.memset(C.kmT[:], 0.0), writes=[C.kmT])
    for tt in range(T // 512):
        cs = slice(tt * 512, (tt + 1) * 512)
        csB = slice(tt * 512 - H2, (tt + 1) * 512 - H2)
        second = tt >= 16
        bQ, bK, bV = emit_proj_mm(S, C, hnT, wu, tt, (0, 64), (65, 129), (129, 193))
        qn = C.qn32[tt % 2]
        kn = C.kn32[tt % 2]
        qd = [(Qa[0:64, cs], [Qa]), (qn[:, :], [qn])]
        if second:
            qd.append((QaB[0:64, csB], [QaB]))
        emit_qknorm(S, C, bQ, 0, gs[:, 0:1], qd, 2 * tt)
        emit_qknorm(S, C, bK, 0, gq[:, 1:2], [(Ka[0:64, cs], [Ka]), (kn[:, :], [kn])], 2 * tt + 1)
        S.op('act', lambda e: e.activation(out=Va[:, 4 * tt:4 * tt + 4, 0:64],
                                           in_=bV[:, 0:256].rearrange("p (j d) -> p j d", j=4), func=AF.Copy),
             reads=[bV], writes=[Va])
        S.op('dve', lambda e: e.tensor_reduce(out=C.kmT[:, 2 * tt:2 * tt + 2],
                                              in_=kn[:, :].rearrange("p (a n) -> p a n", a=2),
                                              axis=AX.X, op=ALU.add), reads=[kn], writes=[C.kmT])
        o4 = 4 * ((tt + 1) % 2)
        bG = C.banks[2 + o4]
        for j in range(4):
            S.op('pe', lambda e, j=j: e.matmul(bG[:, j * 64:(j + 1) * 64], qn[:, j * 128:(j + 1) * 128],
                                               C.kmT[:, :], start=True, stop=True),
                 reads=[qn, C.kmT], writes=[bG])
        gsb, m8, thr, MB = C.gsb, C.m8, C.thr, C.MB
        S.op('act', lambda e: e.activation(out=gsb[:], in_=bG[:, 0:256].rearrange("p (j n) -> p j n", j=4),
                                           func=AF.Copy), reads=[bG], writes=[gsb])
        for half in range(2):
            qblk = 2 * tt + half
            S.op('dve', lambda e: e.memset(gsb[:, 2 * half:2 * half + 2, qblk:64], -1e30), writes=[gsb])
        for j in range(4):
            S.op('dve', lambda e, j=j: e.max(out=m8[:, j, :], in_=gsb[:, j, :]), reads=[gsb], writes=[m8])
        S.op('dve', lambda e: e.tensor_scalar(out=thr[:], in0=m8[:, :, 2], scalar1=-1e29, scalar2=None, op0=ALU.max),
             reads=[m8], writes=[thr])
        for j in range(4):
            S.op('dve', lambda e, j=j: e.tensor_scalar(out=MB[:, j, :, 0:32],
                                                       in0=gsb[:, j, :].rearrange("p (a n) -> p a n", a=2),
                                                       scalar1=thr[:, j:j + 1], scalar2=-NEG,
                                                       op0=ALU.is_ge, op1=ALU.mult), reads=[gsb, thr], writes=[MB])
        S.op('dve', lambda e: e.tensor_scalar(out=MB[:, :, :, 0:32], in0=MB[:, :, :, 0:32], scalar1=NEG, scalar2=None,
                                              op0=ALU.add), reads=[MB], writes=[MB])
        for pg in range(2 if second else 1):
            bT = C.banks[pg + o4]
            for j in range(4):
                S.op('pe', lambda e, j=j, pg=pg: e.transpose(out=bT[:, j * 128:(j + 1) * 128], in_=MB[:, j, pg, :],
                                                            identity=C.ident[:, :]),
                     reads=[MB, C.ident], writes=[bT])
            stg = C.mstg[(2 * tt + pg) % 4]
            S.op('act', lambda e: e.activation(out=stg[:], in_=bT[0:32, :], func=AF.Copy), reads=[bT], writes=[stg])
            if pg == 0:
                S.dma(Qa[96:128, cs], stg[:], reads=[stg], writes=[Qa])
            else:
                S.dma(QaB[96:128, csB], stg[:], reads=[stg], writes=[QaB])
    emit_attention(S, C, yout, (0, 128), (0, 72), dsplit=True)


def moba_dmask():
    m = np.full((128, 4, 512), NEG, np.float32)
    k = np.arange(128)[:, None]
    q = np.arange(512)[None, :]
    for a in range(4):
        kk = a * 128 + k
        ok = (kk // 256 == q // 256) & (kk <= q)
        if a < 2:
            ok = ok | (q >= 256)
        m[:, a, :] = np.where(ok, 0.0, NEG)
    return m


def moba_consts(h):
    slope = float(2.0 ** (-8.0 * (h + 1) / MOBH))
    s1 = np.float32(np.float32(slope).astype(NPBF))
    s2 = np.float32(np.float32(np.float32(slope) - s1).astype(NPBF))
    pos = np.arange(T)
    a = (pos // 256).astype(np.float32)
    b = (pos % 256).astype(np.float32)
    one = np.ones(T, np.float32)
    qc = np.stack([a, b, a, b, 256 * s1 * one, s1 * one, 256 * s2 * one, s2 * one]).astype(NPBF)
    kc = np.stack([-256 * s1 * one, -s1 * one, -256 * s2 * one, -s2 * one, a, b, a, b]).astype(NPBF)
    return qc, kc


def moba_onehot():
    pos = np.arange(T)
    oh = ((pos[None, :] // 256) % 32 == np.arange(32)[:, None]).astype(np.float32)
    return oh.astype(NPBF)


def moba_unit_inputs(u, ws_l, inp, l, h):
    w = ws_l['w_in']
    wu = np.zeros((128, 8, 200), w.dtype)
    wu[:, :, 0:64] = w[:, :, O_BQ + 64 * h:O_BQ + 64 * h + 64]
    wu[:, :, 65:129] = w[:, :, O_BK + 64 * h:O_BK + 64 * h + 64]
    wu[:, :, 129:193] = w[:, :, O_BV + 64 * h:O_BV + 64 * h + 64]
    g = np.stack([inp['moba_q_g'][l], inp['moba_k_g'][l]], axis=1).astype(np.float32)
    qc, kc = moba_consts(h)
    return {"wu%d" % u: wu, "g%d" % u: np.ascontiguousarray(g), "qc%d" % u: qc, "kc%d" % u: kc}


def ml_alloc(S, C):
    C.mwu = S.tile([128, 8, 260], BF16)
    C.mcw = S.tile([64, 2, 4], F32)
    C.mcb = S.tile([64, 2], F32)
    C.mib = S.tile([128, 1], F32)
    C.mfb = S.tile([128, 1], F32)
    C.mnfb = S.tile([128, 1], F32)
    C.mg = S.tile([128, 4, 64], F32)
    C.mU = S.tile([128, 128], F32)
    C.mUb = S.tile([128, 128], BF16)
    C.mid = S.tile([64, 64], F32)
    C.pc = [S.tile([64, 515], F32) for _ in range(2)]
    C.acc = [S.tile([64, 512], F32) for _ in range(2)]
    C.qc = [S.tile([64, 512], BF16) for _ in range(2)]
    C.kc = [S.tile([64, 512], BF16) for _ in range(2)]
    C.kcf = [S.tile([64, 512], F32) for _ in range(2)]
    C.e4 = S.tile([128, 4], F32)
    C.lf4 = S.tile([128, 4], F32)
    C.ii4 = S.tile([128, 4], F32)
    C.tmp4 = S.tile([128, 4], F32)
    C.qs4 = [S.tile([128, 4], F32) for _ in range(2)]
    C.ks4 = [S.tile([128, 4], F32) for _ in range(2)]
    C.eB4 = [S.tile([128, 4], F32) for _ in range(2)]
    C.Vt = [S.tile([128, 4, 65], BF16) for _ in range(2)]
    C.sg = S.tile([128, 4, 64], F32)
    C.GS = [S.tile([128, 4, 64], F32) for _ in range(2)]
    C.sm = [S.tile([128, 128], BF16) for _ in range(2)]
    C.Ktok = [S.tile([128, 64], BF16) for _ in range(2)]
    C.ctmp = S.tile([64, 65], F32)
    C.Cf = S.tile([64, 65], F32)
    C.Cbf = [S.tile([64, 65], BF16) for _ in range(2)]
    C.dn4 = S.tile([128, 4], F32)
    C.fac4 = S.tile([128, 4], F32)
    C.hh = S.tile([128, 4, 64], F32)
    C.hsq = S.tile([128, 4, 64], F32)
    C.ss4 = S.tile([128, 4], F32)
    C.rstd4 = S.tile([128, 4], F32)
    C.Y = [S.tile([64, 512], BF16) for _ in range(2)]
    C.Yf = S.tile([128, 4, 64], F32)
    C.mid128 = S.tile([128, 128], F32)


def emit_ml_unit(S, C, hnT, wu_d, cw_d, cb_d, ib_d, fb_d, g_d, U_d, id_d, yout):
    wu = C.mwu
    load_w(S, C, wu, wu_d)
    S.dma(C.mcw[:], cw_d[:], reads=[cw_d], writes=[C.mcw])
    S.dma(C.mcb[:], cb_d[:], reads=[cb_d], writes=[C.mcb])
    S.dma(C.mib[:], ib_d[:], reads=[ib_d], writes=[C.mib])
    S.dma(C.mfb[:], fb_d[:], reads=[fb_d], writes=[C.mfb])
    S.dma(C.mg[:], g_d[:], reads=[g_d], writes=[C.mg])
    S.dma(C.mU[:], U_d[:], reads=[U_d], writes=[C.mU])
    S.dma(C.mid[:], id_d[0:64, 0:64], reads=[id_d], writes=[C.mid])
    S.dma(C.mid128[:], id_d[:, :], reads=[id_d], writes=[C.mid128])
    S.op('dve', lambda e: e.tensor_copy(out=C.mUb[:], in_=C.mU[:]), reads=[C.mU], writes=[C.mUb])
    S.op('dve', lambda e: e.tensor_scalar(out=C.mnfb[:], in0=C.mfb[:], scalar1=-1.0, scalar2=None, op0=ALU.mult),
         reads=[C.mfb], writes=[C.mnfb])
    for i in range(2):
        S.op('dve', lambda e, i=i: e.memset(C.pc[i][:, 0:3], 0.0), writes=[C.pc[i]])
    S.op('dve', lambda e: e.memset(C.Cf[:], 0.0), writes=[C.Cf])
    S.op('dve', lambda e: e.memset(C.Cbf[0][:], 0.0), writes=[C.Cbf[0]])
    b = C.banks
    cidx = 0
    for tt in range(T // 512):
        hn = C.hn[tt % 2]
        C.load_hn(S, hn, hnT, tt)
        bQ, bK, bA, bB, bGt, bS, bKC, bO = b
        for k in range(8):
            S.op('pe', lambda e, k=k: e.matmul(bQ[0:64, :], wu[:, k, 0:64], hn[:, k, :], start=(k == 0), stop=(k == 7)),
                 reads=[wu, hn], writes=[bQ])
        for k in range(8):
            S.op('pe', lambda e, k=k: e.matmul(bK[0:64, :], wu[:, k, 64:128], hn[:, k, :], start=(k == 0), stop=(k == 7)),
                 reads=[wu, hn], writes=[bK])
        for j in range(4):
            for k in range(8):
                S.op('pe', lambda e, k=k, j=j: e.matmul(bA[:, j * 66:(j + 1) * 66], hn[:, k, j * 128:(j + 1) * 128],
                                                        wu[:, k, 128:194], start=(k == 0), stop=(k == 7)),
                     reads=[wu, hn], writes=[bA])
        for j in range(4):
            for k in range(8):
                S.op('pe', lambda e, k=k, j=j: e.matmul(bB[:, j * 64:(j + 1) * 64], hn[:, k, j * 128:(j + 1) * 128],
                                                        wu[:, k, 194:258], start=(k == 0), stop=(k == 7)),
                     reads=[wu, hn], writes=[bB])
        bAv = bA[:, 0:264].rearrange("p (j d) -> p j d", j=4)
        qc, kc, kcf = C.qc[tt % 2], C.kc[tt % 2], C.kcf[tt % 2]
        for which, ps in ((0, bQ), (1, bK)):
            pc = C.pc[which]
            acc = C.acc[which]
            S.op('act', lambda e: e.activation(out=pc[:, 3:515], in_=ps[0:64, :], func=AF.Copy), reads=[ps], writes=[pc])
            S.op('dve', lambda e: e.tensor_scalar(out=acc[:], in0=pc[:, 3:515], scalar1=C.mcw[:, which, 3:4],
                                                  scalar2=C.mcb[:, which:which + 1], op0=ALU.mult, op1=ALU.add),
                 reads=[pc, C.mcw, C.mcb], writes=[acc])
            for tap in (2, 1, 0):
                S.op('dve', lambda e, tap=tap: e.scalar_tensor_tensor(out=acc[:], in0=pc[:, tap:tap + 512],
                                                                      scalar=C.mcw[:, which, tap:tap + 1], in1=acc[:],
                                                                      op0=ALU.mult, op1=ALU.add),
                     reads=[pc, C.mcw, acc], writes=[acc])
            S.op('dve', lambda e: e.tensor_copy(out=pc[:, 0:3], in_=pc[:, 512:515]), reads=[pc], writes=[pc])
            if which == 0:
                S.op('act', lambda e: e.activation(out=qc[:], in_=acc[:], func=AF.Silu), reads=[acc], writes=[qc])
            else:
                S.op('act', lambda e: e.activation(out=kcf[:], in_=acc[:], func=AF.Silu), reads=[acc], writes=[kcf])
                S.op('pool', lambda e: e.tensor_copy(out=kc[:], in_=kcf[:]), reads=[kcf], writes=[kc])
        e4, lf4, ii4, tmp4 = C.e4, C.lf4, C.ii4, C.tmp4
        qs4, ks4, eB4 = C.qs4[tt % 2], C.ks4[tt % 2], C.eB4[tt % 2]
        S.op('act', lambda e: e.activation(out=e4[:], in_=bAv[:, :, 65], func=AF.Exp, bias=C.mnfb[:, 0:1], scale=-1.0),
             reads=[bA, C.mnfb], writes=[e4])
        S.op('act', lambda e: e.activation(out=lf4[:], in_=e4[:], func=AF.Ln, bias=1.0, scale=1.0),
             reads=[e4], writes=[lf4])
        S.op('dve', lambda e: e.tensor_scalar(out=ii4[:], in0=bAv[:, :, 64], scalar1=C.mib[:, 0:1], scalar2=None,
                                              op0=ALU.add), reads=[bA, C.mib], writes=[ii4])
        S.op('pe', lambda e: e.matmul(bGt[:, 0:4], C.mU[:, :], lf4[:, :], start=True, stop=True),
             reads=[C.mU, lf4], writes=[bGt])
        S.op('pe', lambda e: e.matmul(bGt[:, 4:8], C.ones64[:, :], lf4[:, :], start=True, stop=True),
             reads=[C.ones64, lf4], writes=[bGt])
        S.op('act', lambda e: e.activation(out=qs4[:], in_=bGt[:, 0:4], func=AF.Exp, scale=-1.0), reads=[bGt], writes=[qs4])
        S.op('act', lambda e: e.activation(out=eB4[:], in_=bGt[:, 4:8], func=AF.Exp, scale=-1.0), reads=[bGt], writes=[eB4])
        S.op('dve', lambda e: e.tensor_tensor(out=tmp4[:], in0=bGt[:, 0:4], in1=ii4[:], op=ALU.add),
             reads=[bGt, ii4], writes=[tmp4])
        S.op('act', lambda e: e.activation(out=ks4[:], in_=tmp4[:], func=AF.Exp, bias=C.lnsc[:, 0:1], scale=1.0),
             reads=[tmp4, C.lnsc], writes=[ks4])
        Vt = C.Vt[tt % 2]
        for j in range(4):
            S.op('dve', lambda e, j=j: e.tensor_scalar(out=Vt[:, j, 0:64], in0=bAv[:, j, 0:64], scalar1=ks4[:, j:j + 1],
                                                       scalar2=None, op0=ALU.mult), reads=[bA, ks4], writes=[Vt])
        S.op('dve', lambda e: e.tensor_copy(out=Vt[:, :, 64], in_=ks4[:, :]), reads=[ks4], writes=[Vt])
        GS = C.GS[tt % 2]
        S.op('act', lambda e: e.activation(out=C.sg[:], in_=bB[:, 0:256].rearrange("p (j d) -> p j d", j=4),
                                           func=AF.Sigmoid), reads=[bB], writes=[C.sg])
        S.op('pool', lambda e: e.tensor_tensor(out=GS[:], in0=C.sg[:], in1=C.mg[:], op=ALU.mult),
             reads=[C.sg, C.mg], writes=[GS])
        for j in range(4):
            js = slice(j * 128, (j + 1) * 128)
            sm = C.sm[cidx % 2]
            Ktok = C.Ktok[cidx % 2]
            Cb_in = C.Cbf[cidx % 2]
            Cb_out = C.Cbf[(cidx + 1) % 2]
            S.op('pe', lambda e: e.matmul(bS[:, 0:128], kc[:, js], qc[:, js], start=True, stop=True),
                 reads=[kc, qc], writes=[bS])
            S.op('dve', lambda e: e.tensor_tensor(out=sm[:], in0=bS[:, 0:128], in1=C.mUb[:], op=ALU.mult),
                 reads=[bS, C.mUb], writes=[sm])
            S.op('pe', lambda e: e.transpose(out=bKC[:, 0:64], in_=kcf[:, js], identity=C.mid[:, :]),
                 reads=[kcf, C.mid], writes=[bKC])
            S.op('act', lambda e: e.activation(out=Ktok[:], in_=bKC[:, 0:64], func=AF.Copy), reads=[bKC], writes=[Ktok])
            S.op('pe', lambda e: e.matmul(bO[:, j * 65:(j + 1) * 65], sm[:, :], Vt[:, j, :], start=True, stop=False),
                 reads=[sm, Vt], writes=[bO])
            S.op('pe', lambda e: e.matmul(bO[:, j * 65:(j + 1) * 65], qc[:, js], Cb_in[:, :], start=False, stop=True),
                 reads=[qc, Cb_in], writes=[bO])
            S.op('pe', lambda e: e.matmul(bKC[0:64, 256:321], Ktok[:, :], Vt[:, j, :], start=True, stop=True),
                 reads=[Ktok, Vt], writes=[bKC])
            S.op('act', lambda e: e.activation(out=C.ctmp[:], in_=bKC[0:64, 256:321], func=AF.Copy,
                                               scale=eB4[0:64, j:j + 1]), reads=[bKC, eB4], writes=[C.ctmp])
            S.op('dve', lambda e: e.scalar_tensor_tensor(out=C.Cf[:], in0=C.Cf[:], scalar=eB4[0:64, j:j + 1],
                                                         in1=C.ctmp[:], op0=ALU.mult, op1=ALU.add),
                 reads=[C.Cf, eB4, C.ctmp], writes=[C.Cf])
            S.op('pool', lambda e: e.tensor_copy(out=Cb_out[:], in_=C.Cf[:]), reads=[C.Cf], writes=[Cb_out])
            cidx += 1
        bOv = bO[:, 0:260].rearrange("p (j d) -> p j d", j=4)
        dn4, fac4, hh, hsq, ss4, rstd4 = C.dn4, C.fac4, C.hh, C.hsq, C.ss4, C.rstd4
        S.op('dve', lambda e: e.tensor_tensor(out=dn4[:], in0=bOv[:, :, 64], in1=qs4[:], op=ALU.mult),
             reads=[bO, qs4], writes=[dn4])
        S.op('act', lambda e: e.activation(out=dn4[:], in_=dn4[:], func=AF.Abs), reads=[dn4], writes=[dn4])
        S.op('dve', lambda e: e.tensor_scalar(out=dn4[:], in0=dn4[:], scalar1=1.0, scalar2=None, op0=ALU.max),
             reads=[dn4], writes=[dn4])
        S.op('dve', lambda e: e.reciprocal(out=dn4[:], in_=dn4[:]), reads=[dn4], writes=[dn4])
        S.op('dve', lambda e: e.tensor_tensor(out=fac4[:], in0=dn4[:], in1=qs4[:], op=ALU.mult),
             reads=[dn4, qs4], writes=[fac4])
        for j in range(4):
            S.op('act', lambda e, j=j: e.activation(out=hh[:, j, :], in_=bOv[:, j, 0:64], func=AF.Copy,
                                                    scale=fac4[:, j:j + 1]), reads=[bO, fac4], writes=[hh])
        S.op('dve', lambda e: e.tensor_tensor(out=hsq[:], in0=hh[:], in1=hh[:], op=ALU.mult), reads=[hh], writes=[hsq])
        S.op('dve', lambda e: e.tensor_reduce(out=ss4[:], in_=hsq[:], axis=AX.X, op=ALU.add), reads=[hsq], writes=[ss4])
        S.op('act', lambda e: e.activation(out=ss4[:], in_=ss4[:], func=AF.Sqrt, bias=C.epsc[:, 0:1], scale=1.0 / HD),
             reads=[ss4, C.epsc], writes=[ss4])
        S.op('dve', lambda e: e.reciprocal(out=rstd4[:], in_=ss4[:]), reads=[ss4], writes=[rstd4])
        Yf = C.Yf
        for j in range(4):
            S.op('dve', lambda e, j=j: e.scalar_tensor_tensor(out=Yf[:, j, :], in0=hh[:, j, :], scalar=rstd4[:, j:j + 1],
                                                              in1=GS[:, j, :], op0=ALU.mult, op1=ALU.mult),
                 reads=[hh, rstd4, GS], writes=[Yf])
        for j in range(4):
            S.op('pe', lambda e, j=j: e.transpose(out=bS[0:64, j * 128:(j + 1) * 128], in_=Yf[:, j, :],
                                                  identity=C.mid128[:, :]), reads=[Yf, C.mid128], writes=[bS])
        Y = C.Y[tt % 2]
        S.op('act', lambda e: e.activation(out=Y[:], in_=bS[0:64, :], func=AF.Copy), reads=[bS], writes=[Y])
        S.dma(yout[0:64, tt * 512:(tt + 1) * 512], Y[:], reads=[Y], writes=[yout])


def ml_consts():
    s = np.arange(128)[:, None]
    j = np.arange(128)[None, :]
    return (s <= j).astype(np.float32)


def ml_unit_inputs(u, ws_l, inp, l, h):
    w = ws_l['w_in']
    wu = np.zeros((128, 8, 260), w.dtype)
    wu[:, :, 0:64] = w[:, :, O_MQ + 64 * h:O_MQ + 64 * h + 64]
    wu[:, :, 64:128] = w[:, :, O_MK + 64 * h:O_MK + 64 * h + 64]
    wu[:, :, 128:192] = w[:, :, O_MV + 64 * h:O_MV + 64 * h + 64]
    wu[:, :, 192] = w[:, :, O_MI + h]
    wu[:, :, 193] = w[:, :, O_MF + h]
    wu[:, :, 194:258] = w[:, :, O_MO + 64 * h:O_MO + 64 * h + 64]
    cw = inp['ml_conv_w'][l]
    cb = inp['ml_conv_b'][l]
    cwq = cw[:, 64 * h:64 * h + 64].T
    cwk = cw[:, 256 + 64 * h:256 + 64 * h + 64].T
    cwu = np.ascontiguousarray(np.stack([cwq, cwk], axis=1)).astype(np.float32)
    cbu = np.ascontiguousarray(np.stack([cb[64 * h:64 * h + 64], cb[256 + 64 * h:256 + 64 * h + 64]], axis=1)).astype(np.float32)
    ib = np.full((128, 1), inp['ml_i_bias'][l][h], np.float32)
    fb = np.full((128, 1), inp['ml_f_bias'][l][h], np.float32)
    g = np.ascontiguousarray(np.broadcast_to(inp['ml_h_g'][l][64 * h:64 * h + 64][None, None, :], (128, 4, 64))).astype(np.float32)
    return {"wu%d" % u: wu, "cw%d" % u: cwu, "cb%d" % u: cbu, "ib%d" % u: ib, "fb%d" % u: fb, "mg%d" % u: g}


NTOK = B * T // NCORE
NTT = 1024
MFF = DFF // 128


def emit_norm_mod(S, C, x, hn, a, shb, shi, ncols):
    for h0 in range(0, ncols, 512):
        hs = slice(h0, h0 + 512)
        bSS = C.nextbank()
        for k in range(8):
            sq = C.sq[k % 2]
            S.op('act', lambda e, k=k: e.activation(out=sq[:], in_=x[:, k, hs], func=AF.Square), reads=[x], writes=[sq])
            S.op('pe', lambda e, k=k: e.matmul(bSS[:, :], C.ones[:, :], sq[:], start=(k == 0), stop=(k == 7)),
                 reads=[C.ones, sq], writes=[bSS])
        rs = C.rs
        S.op('act', lambda e: e.activation(out=rs[:], in_=bSS[:, :], func=AF.Sqrt, bias=C.epsc[:, 0:1], scale=1.0 / D),
             reads=[bSS, C.epsc], writes=[rs])
        S.op('dve', lambda e: e.reciprocal(out=rs[:], in_=rs[:]), reads=[rs], writes=[rs])
        for k in range(8):
            t = C.t[k % 2]
            S.op('dve', lambda e, k=k: e.tensor_tensor(out=t[:], in0=x[:, k, hs], in1=rs[:], op=ALU.mult),
                 reads=[x, rs], writes=[t])
            S.op('act', lambda e, k=k: e.activation(out=hn[:, k, hs], in_=t[:], func=AF.Identity,
                                                    bias=shb[:, shi, k:k + 1], scale=a[:, k:k + 1]),
                 reads=[t, a, shb], writes=[hn])


def build_dense(post, nextnorm):
    nc = new_nc()
    with ExitStack() as es:
        S = Sched(nc, es)
        C = MixCtx()
        banks = [S.psum([128, 512], F32) for _ in range(8)]
        C.bi = 0

        def nextbank():
            C.bi += 1
            return banks[C.bi % 8]
        C.nextbank = nextbank
        C.ones = S.tile([128, 128], F32)
        S.op('dve', lambda e: e.memset(C.ones[:], 1.0), writes=[C.ones])
        C.epsc = S.tile([128, 1], F32)
        S.op('dve', lambda e: e.memset(C.epsc[:], EPS), writes=[C.epsc])
        C.sq = [S.tile([128, 512], F32) for _ in range(2)]
        C.t = [S.tile([128, 512], F32) for _ in range(2)]
        C.rs = S.tile([128, 512], F32)
        xT = S.dram_in("xT", [128, 8, NTOK], F32)
        vec = S.dram_in("vec", [128, 10, 8], F32)
        v = S.tile([128, 10, 8], F32)
        S.dma(v[:], vec[:], reads=[vec], writes=[v])
        a2 = S.tile([128, 8], F32)
        a1n = S.tile([128, 8], F32)
        S.op('dve', lambda e: e.tensor_scalar(out=a2[:], in0=v[:, 2, :], scalar1=1.0, scalar2=None, op0=ALU.add),
             reads=[v], writes=[a2])
        S.op('dve', lambda e: e.tensor_tensor(out=a2[:], in0=a2[:], in1=v[:, 1, :], op=ALU.mult), reads=[a2, v], writes=[a2])
        S.op('dve', lambda e: e.tensor_scalar(out=a1n[:], in0=v[:, 6, :], scalar1=1.0, scalar2=None, op0=ALU.add),
             reads=[v], writes=[a1n])
        S.op('dve', lambda e: e.tensor_tensor(out=a1n[:], in0=a1n[:], in1=v[:, 5, :], op=ALU.mult), reads=[a1n, v], writes=[a1n])
        outs = []
        if post:
            yT = S.dram_in("yT", [128, 8, NTOK], BF16)
            wo = S.dram_in("wo", [8, 128, 8, 128], BF16)
            wg = S.dram_in("wg", [MFF, 128, 8, 128], BF16)
            wu = S.dram_in("wu", [MFF, 128, 8, 128], BF16)
            wd = S.dram_in("wd", [8, 128, MFF, 128], BF16)
            xo = S.dram_out("xo", [128, 8, NTOK], F32)
            outs.append(xo)
            yt = [S.tile([128, 8, NTT], BF16) for _ in range(1)]
            hn2 = S.tile([128, 8, NTT], BF16)
            A = S.tile([128, MFF, NTT], BF16)
            wot = [S.tile([128, 8, 128], BF16) for _ in range(2)]
            wgt = [S.tile([128, 8, 128], BF16) for _ in range(2)]
            wut = [S.tile([128, 8, 128], BF16) for _ in range(2)]
            wdt = [S.tile([128, MFF, 128], BF16) for _ in range(2)]
            sgt = [S.tile([128, 512], F32) for _ in range(2)]
        if nextnorm:
            hno = S.dram_out("hno", [128, 8, NTOK], BF16)
            outs.append(hno)
            hnn = [S.tile([128, 8, NTT], BF16) for _ in range(1)]
        xt = [S.tile([128, 8, NTT], F32) for _ in range(2)]
        for ti in range(NTOK // NTT):
            ts = slice(ti * NTT, (ti + 1) * NTT)
            x = xt[ti % 2]
            S.dma(x[:], xT[:, :, ts], reads=[xT], writes=[x])
            if post:
                y = yt[0]
                S.dma(y[:], yT[:, :, ts], reads=[yT], writes=[y])
                wi = 0
                for m in range(8):
                    w = wot[m % 2]
                    S.dma(w[:], wo[m], reads=[wo], writes=[w])
                    for h0 in range(0, NTT, 512):
                        hs = slice(h0, h0 + 512)
                        ps = nextbank()
                        for k in range(8):
                            S.op('pe', lambda e, k=k: e.matmul(ps[:, :], w[:, k, :], y[:, k, hs], start=(k == 0), stop=(k == 7)),
                                 reads=[w, y], writes=[ps])
                        S.op('dve', lambda e: e.scalar_tensor_tensor(out=x[:, m, hs], in0=ps[:, :], scalar=v[:, 0, m:m + 1],
                                                                     in1=x[:, m, hs], op0=ALU.mult, op1=ALU.add),
                             reads=[ps, v, x], writes=[x])
                emit_norm_mod(S, C, x, hn2, a2, v, 3, NTT)
                for m in range(MFF):
                    w1 = wgt[m % 2]
                    w2 = wut[m % 2]
                    S.dma(w1[:], wg[m], reads=[wg], writes=[w1])
                    S.dma(w2[:], wu[m], reads=[wu], writes=[w2])
                    for h0 in range(0, NTT, 512):
                        hs = slice(h0, h0 + 512)
                        pg = nextbank()
                        pu = nextbank()
                        for k in range(8):
                            S.op('pe', lambda e, k=k: e.matmul(pg[:, :], w1[:, k, :], hn2[:, k, hs], start=(k == 0), stop=(k == 7)),
                                 reads=[w1, hn2], writes=[pg])
                        for k in range(8):
                            S.op('pe', lambda e, k=k: e.matmul(pu[:, :], w2[:, k, :], hn2[:, k, hs], start=(k == 0), stop=(k == 7)),
                                 reads=[w2, hn2], writes=[pu])
                        sg = sgt[(h0 // 512) % 2]
                        S.op('act', lambda e: e.activation(out=sg[:], in_=pg[:, :], func=AF.Silu), reads=[pg], writes=[sg])
                        S.op('dve', lambda e: e.tensor_tensor(out=A[:, m, hs], in0=pu[:, :], in1=sg[:], op=ALU.mult),
                             reads=[pu, sg], writes=[A])
                for f in range(8):
                    w = wdt[f % 2]
                    S.dma(w[:], wd[f], reads=[wd], writes=[w])
                    for h0 in range(0, NTT, 512):
                        hs = slice(h0, h0 + 512)
                        ps = nextbank()
                        for m in range(MFF):
                            S.op('pe', lambda e, m=m: e.matmul(ps[:, :], w[:, m, :], A[:, m, hs], start=(m == 0), stop=(m == MFF - 1)),
                                 reads=[w, A], writes=[ps])
                        S.op('dve', lambda e: e.scalar_tensor_tensor(out=x[:, f, hs], in0=ps[:, :], scalar=v[:, 4, f:f + 1],
                                                                     in1=x[:, f, hs], op0=ALU.mult, op1=ALU.add),
                             reads=[ps, v, x], writes=[x])
                S.dma(xo[:, :, ts], x[:], reads=[x], writes=[xo])
            if nextnorm:
                hq = hnn[0]
                emit_norm_mod(S, C, x, hq, a1n, v, 7, NTT)
                S.dma(hno[:, :, ts], hq[:], reads=[hq], writes=[hno])
        S.finish(outs)
    return nc


def pk8(vv):
    return np.ascontiguousarray(vv.reshape(8, 128).T)


def dense_weights(ws_l):
    wo = ws_l['w_out']
    wgu = ws_l['w_gu']
    wdn = ws_l['w_down']
    wo_t = np.ascontiguousarray(wo.reshape(128, 8, 8, 128).transpose(2, 0, 1, 3))
    wg_t = np.ascontiguousarray(wgu[:, :, :DFF].reshape(128, 8, MFF, 128).transpose(2, 0, 1, 3))
    wu_t = np.ascontiguousarray(wgu[:, :, DFF:].reshape(128, 8, MFF, 128).transpose(2, 0, 1, 3))
    wd_t = np.ascontiguousarray(wdn.reshape(128, MFF, 8, 128).transpose(2, 0, 1, 3))
    return {"wo": wo_t, "wg": wg_t, "wu": wu_t, "wd": wd_t}


DEBUG = False
MODQ = 6 * D // 4
MODCH = MODQ // 128
UROWS = 320
TWO = [[0, 1], [2, 3], [4, 5], [4, 5]]


CW = 4096


def plan_dense_layout():
    tiles = []
    for l in range(DEPTH):
        tiles += [(('wo', l, m), 1024) for m in range(8)]
        tiles += [((nm, l, m), 1024) for m in range(MFF) for nm in ('wg', 'wu')]
        tiles += [(('wd', l, f), MFF * 128) for f in range(8)]
    nchq = 1
    while True:
        pos = {}
        q, ch, off = 0, 0, 0
        ok = True
        for key, n in tiles:
            if off + n > CW:
                ch += 1
                off = 0
                if ch == nchq:
                    q += 1
                    ch = 0
            if q > 3:
                ok = False
                break
            pos[key] = (q, ch, off)
            off += n
        if ok:
            return nchq * CW, pos
        nchq += 1


WQ, WPOS = plan_dense_layout()


def build_fused(debug=False):
    nc = new_nc()
    with ExitStack() as es:
        S = Sched(nc, es)
        banks = [S.psum([128, 512], F32) for _ in range(8)]
        xT = S.dram_in("xT", [128, 8, NTOK], F32)
        cT = S.dram_in("cT", [128, 8, 1], F32)
        wada = S.dram_in("wada", [128, DEPTH, 8, MODQ], F32)
        bada = S.dram_in("bada", [128, DEPTH * MODCH], F32)
        wf = S.dram_in("wf", [128, WQ], F32)
        ngin = S.dram_in("ngin", [128, DEPTH, 2, 8], F32)
        yidx_d = S.dram_in("yidx", [128, NTOK // NTT, 8], mybir.dt.int32)
        fdm = S.dram_in("fox_dm", [128, 4, 512], F32)
        bdm = S.dram_in("moba_dm", [128, 4, 512], F32)
        boh = S.dram_in("moba_oh", [32, T], BF16)
        idd = S.dram_in("ident", [128, 128], F32)
        mU = S.dram_in("ml_U", [128, 128], F32)
        uin = {}
        for l in range(DEPTH):
            for u, kind in enumerate(['fox', 'fox', 'ml', 'moba', 'moba']):
                pf = "L%du%d_" % (l, u)
                d = {}
                if kind == 'fox':
                    d['wu'] = S.dram_in(pf + "wu", [128, 8, 200], F32)
                    d['g'] = S.dram_in(pf + "g", [64, 2], F32)
                    d['fb'] = S.dram_in(pf + "fb", [65, 1], F32)
                elif kind == 'moba':
                    d['wu'] = S.dram_in(pf + "wu", [128, 8, 200], F32)
                    d['g'] = S.dram_in(pf + "g", [64, 2], F32)
                    d['qc'] = S.dram_in(pf + "qc", [8, T], BF16)
                    d['kc'] = S.dram_in(pf + "kc", [8, T], BF16)
                else:
                    d['wu'] = S.dram_in(pf + "wu", [128, 8, 260], F32)
                    d['cw'] = S.dram_in(pf + "cw", [64, 2, 4], F32)
                    d['cb'] = S.dram_in(pf + "cb", [64, 2], F32)
                    d['ib'] = S.dram_in(pf + "ib", [128, 1], F32)
                    d['fb'] = S.dram_in(pf + "fb", [128, 1], F32)
                    d['mg'] = S.dram_in(pf + "mg", [128, 4, 64], F32)
                uin[(l, u)] = d
        xo = S.dram_out("xo", [128, 8, NTOK], F32)
        modl = S.dram_int("modl", [128, DEPTH * MODCH], F32)
        moda = S.dram_int("moda", [512, DEPTH * MODCH], F32)
        NWCH = WQ // CW
        wbl = S.dram_int("wbl", [NWCH * 128, CW], BF16)
        wba = S.dram_int("wba", [NWCH * 512, CW], BF16)
        hnl = S.dram_int("hnl", [8 * 128, NTOK], BF16)
        hna = S.dram_int("hna", [8 * 512, NTOK], BF16)
        yl = S.dram_int("yl", [UROWS * 4, NTOK], BF16)
        ya = S.dram_int("ya", [UROWS * 16, NTOK], BF16)
        xs = S.dram_int("xs", [128, 8, NTOK], F32)
        ylv = yl.t.rearrange("(u q) t -> u (q t)", q=4)
        hnlv = hnl.t.rearrange("(k p) t -> p k t", k=8)
        yav = ya.t.rearrange("r (a t) -> (r a) t", a=NTOK // NTT)

        vecall = S.tile([128, 4, DEPTH * MODCH], F32)
        ng = S.tile([128, DEPTH, 2, 8], F32)
        yidx = S.tile([128, NTOK // NTT, 8], mybir.dt.int32)
        S.dma(ng[:], ngin[:], reads=[ngin], writes=[ng])
        S.dma(yidx[:], yidx_d[:], reads=[yidx_d], writes=[yidx])
        ones = S.tile([128, 128], F32)
        S.op('dve', lambda e: e.memset(ones[:], 1.0), writes=[ones])
        epsc = S.tile([128, 1], F32)
        S.op('dve', lambda e: e.memset(epsc[:], EPS), writes=[epsc])
        vts = [S.tile([128, 10, 8], F32) for _ in range(DEPTH + 1)]
        a_t = [S.tile([128, 2, 8], F32) for _ in range(DEPTH + 1)]

        with ExitStack() as es2:
            S.es = es2
            c_sb = S.tile([128, 8, 1], F32)
            sg_sb = S.tile([128, 8, 1], F32)
            sc_sb = S.tile([128, 8, 1], F32)
            b_sb = S.tile([128, DEPTH * MODCH], F32)
            o_sb = S.tile([128, DEPTH * MODCH], F32)
            S.dma(c_sb[:], cT[:], reads=[cT], writes=[c_sb])
            S.dma(b_sb[:], bada[:], reads=[bada], writes=[b_sb])
            S.op('act', lambda e: e.activation(out=sg_sb[:], in_=c_sb[:], func=AF.Sigmoid), reads=[c_sb], writes=[sg_sb])
            S.op('dve', lambda e: e.tensor_tensor(out=sc_sb[:], in0=c_sb[:], in1=sg_sb[:], op=ALU.mult),
                 reads=[c_sb, sg_sb], writes=[sc_sb])
            wt = S.tile([128, 8, MODQ], F32)
            psm = banks[0]
            for l in range(DEPTH):
                S.dma(wt[:], wada[:, l, :, :], reads=[wada], writes=[wt])
                for ci in range(MODCH):
                    col = l * MODCH + ci
                    for k in range(8):
                        S.op('pe', lambda e, k=k: e.matmul(psm[:, col:col + 1], wt[:, k, ci * 128:(ci + 1) * 128],
                                                           sc_sb[:, k, 0:1], start=(k == 0), stop=(k == 7)),
                             reads=[wt, sc_sb], writes=[psm])
            S.op('dve', lambda e: e.tensor_tensor(out=o_sb[:], in0=psm[:, 0:DEPTH * MODCH], in1=b_sb[:], op=ALU.add),
                 reads=[psm, b_sb], writes=[o_sb])
            S.dma(modl[:], o_sb[:], reads=[o_sb], writes=[modl])
            S.collective(modl, moda)
            S.dma(vecall[:], moda.t.rearrange("(r p) c -> p r c", p=128), reads=[moda], writes=[vecall])
            fts = [S.tile([128, WC_TILE], F32) for _ in range(3)]
            bts = [S.tile([128, WC_TILE], BF16) for _ in range(3)]
            for i in range(WQ // WC_TILE):
                ft, bt = fts[i % 3], bts[i % 3]
                sl = slice(i * WC_TILE, (i + 1) * WC_TILE)
                S.dma(ft[:], wf[:, sl], reads=[wf], writes=[ft])
                eng = 'dve' if i % 2 == 0 else 'pool'
                S.op(eng, lambda e: e.tensor_copy(out=bt[:], in_=ft[:]), reads=[ft], writes=[bt])
                ch, co = (i * WC_TILE) // CW, (i * WC_TILE) % CW
                S.dma(wbl[ch * 128:(ch + 1) * 128, co:co + WC_TILE], bt[:], reads=[bt], writes=[wbl])
            S.collective_chunks(wbl, wba, NWCH, 128)
            S.barrier()
        S.es = es

        def vcol(l, j, k):
            ci = j * 8 + k
            return vecall[:, ci // MODCH, l * MODCH + ci % MODCH:l * MODCH + ci % MODCH + 1]

        def fill_vec(vt, at, lpost, lnext):
            S.op('dve', lambda e: e.memset(vt[:], 0.0), writes=[vt])
            items = []
            if lpost is not None:
                items += [(0, lpost, 2), (2, lpost, 4), (3, lpost, 3), (4, lpost, 5)]
                S.op('dve', lambda e: e.tensor_copy(out=vt[:, 1, :], in_=ng[:, lpost, 1, :]), reads=[ng], writes=[vt])
            if lnext is not None:
                items += [(6, lnext, 1), (7, lnext, 0)]
                S.op('dve', lambda e: e.tensor_copy(out=vt[:, 5, :], in_=ng[:, lnext, 0, :]), reads=[ng], writes=[vt])
            for row, l, j in items:
                for k in range(8):
                    S.op('dve', lambda e, k=k: e.tensor_copy(out=vt[:, row, k:k + 1], in_=vcol(l, j, k)),
                         reads=[vecall], writes=[vt])
            for ai, (srow, grow) in enumerate(((2, 1), (6, 5))):
                S.op('dve', lambda e: e.tensor_scalar(out=at[:, ai, :], in0=vt[:, srow, :], scalar1=1.0, scalar2=None,
                                                      op0=ALU.add), reads=[vt], writes=[at])
                S.op('dve', lambda e: e.tensor_tensor(out=at[:, ai, :], in0=at[:, ai, :], in1=vt[:, grow, :],
                                                      op=ALU.mult), reads=[at, vt], writes=[at])

        fill_vec(vts[0], a_t[0], None, 0)
        for l in range(DEPTH):
            fill_vec(vts[l + 1], a_t[l + 1], l, l + 1 if l + 1 < DEPTH else None)

        def dense_phase(l, post, nextnorm, xsrc, xdst):
            v = vts[0] if not post else vts[l + 1]
            at = a_t[0] if not post else a_t[l + 1]
            with ExitStack() as es2:
                S.es = es2
                C = MixCtx()
                C.bi = 0

                def nextbank():
                    C.bi += 1
                    return banks[C.bi % 8]
                C.nextbank = nextbank
                C.ones, C.epsc = ones, epsc
                C.sq = [S.tile([128, 512], F32) for _ in range(2)]
                C.t = [S.tile([128, 512], F32) for _ in range(2)]
                C.rs = S.tile([128, 512], F32)
                a2 = View(at, at.t[:, 0, :])
                a1n = View(at, at.t[:, 1, :])
                if post:
                    yt = S.tile([128, 8, NTT], BF16)
                    hn2 = S.tile([128, 8, NTT], BF16)
                    A = S.tile([128, MFF, NTT], BF16)
                    wot = [S.tile([128, 8, 128], BF16) for _ in range(2)]
                    wgt = [S.tile([128, 8, 128], BF16) for _ in range(2)]
                    wut = [S.tile([128, 8, 128], BF16) for _ in range(2)]
                    wdt = [S.tile([128, MFF, 128], BF16) for _ in range(2)]
                    sgt = [S.tile([128, 512], F32) for _ in range(2)]

                    def wsrc(key, kk):
                        q, ch, off = WPOS[key]
                        r0 = ch * 512 + q * 128
                        return wba.t[r0:r0 + 128, off:off + kk * 128].rearrange("p (k j) -> p k j", k=kk)
                if nextnorm:
                    hq = S.tile([128, 8, NTT], BF16)
                xt = [S.tile([128, 8, NTT], F32) for _ in range(2)]
                for ti in range(NTOK // NTT):
                    ts = slice(ti * NTT, (ti + 1) * NTT)
                    x = xt[ti % 2]
                    S.dma(x[:], xsrc[:, :, ts], reads=[xsrc], writes=[x])
                    if post:
                        y = yt
                        for k in range(8):
                            S.idma(y[:, k, :], yav, yidx[:, ti, k:k + 1], reads=[ya, yidx], writes=[y])
                        for m in range(8):
                            w = wot[m % 2]
                            S.dma(w[:], wsrc(('wo', l, m), 8), reads=[wba], writes=[w])
                            for h0 in range(0, NTT, 512):
                                hs = slice(h0, h0 + 512)
                                ps = nextbank()
                                for k in range(8):
                                    S.op('pe', lambda e, k=k: e.matmul(ps[:, :], w[:, k, :], y[:, k, hs], start=(k == 0),
                                                                       stop=(k == 7)), reads=[w, y], writes=[ps])
                                S.op('dve', lambda e: e.scalar_tensor_tensor(out=x[:, m, hs], in0=ps[:, :],
                                                                             scalar=v[:, 0, m:m + 1], in1=x[:, m, hs],
                                                                             op0=ALU.mult, op1=ALU.add),
                                     reads=[ps, v, x], writes=[x])
                        emit_norm_mod(S, C, x, hn2, a2, v, 3, NTT)
                        for m in range(MFF):
                            w1, w2 = wgt[m % 2], wut[m % 2]
                            S.dma(w1[:], wsrc(('wg', l, m), 8), reads=[wba], writes=[w1])
                            S.dma(w2[:], wsrc(('wu', l, m), 8), reads=[wba], writes=[w2])
                            for h0 in range(0, NTT, 512):
                                hs = slice(h0, h0 + 512)
                                pg = nextbank()
                                pu = nextbank()
                                for k in range(8):
                                    S.op('pe', lambda e, k=k: e.matmul(pg[:, :], w1[:, k, :], hn2[:, k, hs], start=(k == 0),
                                                                       stop=(k == 7)), reads=[w1, hn2], writes=[pg])
                                for k in range(8):
                                    S.op('pe', lambda e, k=k: e.matmul(pu[:, :], w2[:, k, :], hn2[:, k, hs], start=(k == 0),
                                                                       stop=(k == 7)), reads=[w2, hn2], writes=[pu])
                                sg = sgt[(h0 // 512) % 2]
                                S.op('act', lambda e: e.activation(out=sg[:], in_=pg[:, :], func=AF.Silu), reads=[pg], writes=[sg])
                                S.op('dve', lambda e: e.tensor_tensor(out=A[:, m, hs], in0=pu[:, :], in1=sg[:], op=ALU.mult),
                                     reads=[pu, sg], writes=[A])
                        for f in range(8):
                            w = wdt[f % 2]
                            S.dma(w[:], wsrc(('wd', l, f), MFF), reads=[wba], writes=[w])
                            for h0 in range(0, NTT, 512):
                                hs = slice(h0, h0 + 512)
                                ps = nextbank()
                                for m in range(MFF):
                                    S.op('pe', lambda e, m=m: e.matmul(ps[:, :], w[:, m, :], A[:, m, hs], start=(m == 0),
                                                                       stop=(m == MFF - 1)), reads=[w, A], writes=[ps])
                                S.op('dve', lambda e: e.scalar_tensor_tensor(out=x[:, f, hs], in0=ps[:, :],
                                                                             scalar=v[:, 4, f:f + 1], in1=x[:, f, hs],
                                                                             op0=ALU.mult, op1=ALU.add),
                                     reads=[ps, v, x], writes=[x])
                        S.dma(xdst[:, :, ts], x[:], reads=[x], writes=[xdst])
                    if nextnorm:
                        emit_norm_mod(S, C, x, hq, a1n, v, 7, NTT)
                        S.dma(hnlv[:, :, ts], hq[:], reads=[hq], writes=[hnl])
                if nextnorm:
                    S.collective_chunks(hnl, hna, 8, 128)
                S.barrier()
            S.es = es

        def mix_phase(l, kinds, slots):
            with ExitStack() as es2:
                S.es = es2
                C = mix_common(S, banks=banks, fused=True)
                if 'fox' in kinds or 'moba' in kinds:
                    attn_alloc(S, C, 'moba' in kinds)
                if 'fox' in kinds:
                    fox_alloc(S, C)
                if 'ml' in kinds:
                    ml_alloc(S, C)
                if 'moba' in kinds:
                    moba_alloc(S, C)
                for kind, u in zip(kinds, slots):
                    d = uin[(l, u)]
                    yout = View(yl, ylv[64 * u:64 * u + 64, :])
                    if kind == 'fox':
                        emit_fox_unit(S, C, hna, d['wu'], d['g'], d['fb'], fdm, yout)
                    elif kind == 'moba':
                        emit_moba_unit(S, C, hna, d['wu'], d['g'], d['qc'], d['kc'], boh, bdm, idd, yout)
                    else:
                        emit_ml_unit(S, C, hna, d['wu'], d['cw'], d['cb'], d['ib'], d['fb'], d['mg'], mU, idd, yout)
                S.barrier()
            S.es = es

        dense_phase(0, False, True, xT, None)
        for l in range(DEPTH):
            mix_phase(l, ['fox', 'fox', 'ml'], [0, 1, 2])
            mix_phase(l, ['moba', 'moba'], [3, 4])
            S.collective_chunks(yl, ya, UROWS * 4 // 128, 128)
            if debug:
                dy = S.dram_out("dbg_y", [UROWS * 4, NTOK], BF16)
                dh = S.dram_out("dbg_hn", [8 * 128, NTOK], BF16)
                dya = S.dram_out("dbg_ya", [UROWS * 16, NTOK], BF16)
                dv = S.dram_out("dbg_v", [128, 10, 8], F32)
                S.dma(dy[:], yl[:], reads=[yl], writes=[dy])
                S.dma(dh[:], hnl[:], reads=[hnl], writes=[dh])
                S.dma(dya[:], ya[:], reads=[ya], writes=[dya])
                S.dma(dv[:], vts[1][:], reads=[vts[1]], writes=[dv])
                S.finish([dy, dh, dya, dv])
                return nc
            last = l == DEPTH - 1
            dense_phase(l, True, not last, xT if l == 0 else xs, xo if last else xs)
        S.finish([xo])
    return nc


def dense_tiles_f32(inp, l):
    wo = w_to_pk(inp['w_out'][l]).reshape(128, 8, D)
    wgu = w_to_pk(inp['w_gate_up'][l]).reshape(128, 8, 2 * DFF)
    wdn = w_to_pk(inp['w_down'][l]).reshape(128, MFF, D)
    t = {}
    for m in range(8):
        t[('wo', l, m)] = wo[:, :, m * 128:(m + 1) * 128].reshape(128, -1)
        t[('wd', l, m)] = wdn[:, :, m * 128:(m + 1) * 128].reshape(128, -1)
    for m in range(MFF):
        t[('wg', l, m)] = wgu[:, :, m * 128:(m + 1) * 128].reshape(128, -1)
        t[('wu', l, m)] = wgu[:, :, DFF + m * 128:DFF + (m + 1) * 128].reshape(128, -1)
    return t


def kernel(**inp):
    inp = {k: np.asarray(v) for k, v in inp.items()}
    x = inp['x']
    wq = [np.zeros((128, WQ), np.float32) for _ in range(4)]
    for l in range(DEPTH):
        for key, arr in dense_tiles_f32(inp, l).items():
            q, ch, off = WPOS[key]
            wq[q][:, ch * CW + off:ch * CW + off + arr.shape[1]] = arr
    w_in_pk = [w_to_pk(inp['w_in'][l]).reshape(128, 8, IN_COLS) for l in range(DEPTH)]
    consts = {"fox_dm": fox_dmask(), "moba_dm": moba_dmask(), "moba_oh": moba_onehot(),
              "ident": np.eye(128, dtype=np.float32), "ml_U": ml_consts()}
    ngin = np.ascontiguousarray(np.stack([np.stack([pk8(inp['norm1_g'][l]), pk8(inp['norm2_g'][l])], axis=1)
                                          for l in range(DEPTH)], axis=1)).astype(np.float32)
    in_maps = []
    for c in range(NCORE):
        b, r = c // 4, c % 4
        m = dict(consts)
        m["xT"] = to_fm(x[b, r * NTOK:(r + 1) * NTOK, :])
        m["cT"] = np.ascontiguousarray(inp['c'][b].reshape(8, 128).T)[:, :, None].astype(np.float32)
        wsl = inp['w_ada'][:, :, r * MODQ:(r + 1) * MODQ]
        m["wada"] = np.ascontiguousarray(wsl.reshape(DEPTH, 8, 128, MODQ).transpose(2, 0, 1, 3))
        bsl = inp['b_ada'][:, r * MODQ:(r + 1) * MODQ].reshape(DEPTH, MODCH, 128)
        m["bada"] = np.ascontiguousarray(bsl.transpose(2, 0, 1)).reshape(128, DEPTH * MODCH).astype(np.float32)
        m["wf"] = wq[r]
        m["ngin"] = ngin
        idx = np.zeros((D,), np.int32)
        for f in range(D):
            if f < 384:
                h, dd = f // 64, f % 64
                rk, ur = min(h // 2, 2), 64 * (h % 2) + dd
            elif f < 640:
                h, dd = (f - 384) // 64, f % 64
                rk, ur = h, 128 + dd
            else:
                h, dd = (f - 640) // 64, f % 64
                rk, ur = min(h // 2, 2), 192 + 64 * (h % 2) + dd
            lr = ur * 4 + r
            idx[f] = (lr // 128) * 512 + rk * 128 + lr % 128
        nti = NTOK // NTT
        idx2 = idx.reshape(8, 128).T
        m["yidx"] = np.ascontiguousarray(np.stack([idx2 * nti + ti for ti in range(nti)], axis=1)).astype(np.int32)
        for l in range(DEPTH):
            ws_l = {'w_in': w_in_pk[l]}
            for u, kind in enumerate(['fox', 'fox', 'ml', 'moba', 'moba']):
                pf = "L%du%d_" % (l, u)
                if kind == 'fox':
                    dd_ = fox_unit_inputs(0, ws_l, inp, l, TWO[r][u])
                    m[pf + "wu"], m[pf + "g"], m[pf + "fb"] = dd_["wu0"], dd_["g0"], dd_["fb0"]
                elif kind == 'moba':
                    dd_ = moba_unit_inputs(0, ws_l, inp, l, TWO[r][u - 3])
                    m[pf + "wu"], m[pf + "g"], m[pf + "qc"], m[pf + "kc"] = dd_["wu0"], dd_["g0"], dd_["qc0"], dd_["kc0"]
                else:
                    dd_ = ml_unit_inputs(0, ws_l, inp, l, r)
                    for nm in ("wu", "cw", "cb", "ib", "fb", "mg"):
                        m[pf + nm] = dd_[nm + "0"]
        in_maps.append(m)
    if DEBUG:
        return run_spmd(build_fused(True), in_maps)
    res = run_spmd(build_fused(), in_maps)
    out = np.zeros((B, T, D), np.float32)
    for c in range(NCORE):
        b, q = c // 4, c % 4
        out[b, q * NTOK:(q + 1) * NTOK, :] = res[c]["xo"].transpose(2, 1, 0).reshape(NTOK, D)
    return out
```

```python
import numpy as np
from contextlib import ExitStack
import ml_dtypes
import concourse.bass as bass
import concourse.mybir as mybir
from concourse.bass_utils import run_bass_kernel_spmd

F32 = mybir.dt.float32
BF16 = mybir.dt.bfloat16
AF = mybir.ActivationFunctionType
ALU = mybir.AluOpType
AX = mybir.AxisListType
NPBF = ml_dtypes.bfloat16

D = 1024
B = 2
T = 16384
DEPTH = 2
HD = 64
NCORE = 8
DFF = 2816
FOXH, MLH, MOBH = 6, 4, 6
EPS = 1e-6
IN_COLS = 3342
O_FQ, O_FK, O_FV, O_FF = 0, 384, 768, 1152
O_MQ, O_MK, O_MV, O_MI, O_MF, O_MO = 1158, 1414, 1670, 1926, 1930, 1934
O_BQ, O_BK, O_BV = 2190, 2574, 2958
NEG = -30000.0


class Buf:
    def __init__(self, t=None):
        self.t = t
        self.w = None
        self.r = {}

    def __getitem__(self, idx):
        return self.t[idx]


class View:
    def __init__(self, parent, t):
        self.p = parent
        self.t = t

    def __getitem__(self, idx):
        return self.t[idx]

    @property
    def w(self):
        return self.p.w

    @w.setter
    def w(self, v):
        self.p.w = v

    @property
    def r(self):
        return self.p.r

    @r.setter
    def r(self, v):
        self.p.r = v


GROUPS = [[0, 1, 2, 3], [4, 5, 6, 7]]


class Sched:
    NDMA = 32

    def __init__(self, nc, es):
        self.nc = nc
        self.es = es
        self.eng = {'pe': nc.tensor, 'act': nc.scalar, 'dve': nc.vector, 'pool': nc.gpsimd,
                    'sp': nc.sync}
        self.esem = {k: es.enter_context(nc.semaphore('s_' + k)) for k in ['pe', 'act', 'dve', 'pool', 'cc']}
        self.ecnt = {k: 0 for k in self.esem}
        self.dsem = [es.enter_context(nc.semaphore('d%d' % i)) for i in range(self.NDMA)]
        self.dcnt = [0] * self.NDMA
        self.dnext = 0
        self.known = {k: {} for k in self.eng}
        self.out_tags = {}
        self.ntile = 0

    def tile(self, shape, dtype, name=None):
        self.ntile += 1
        name = name or ('t%d' % self.ntile)
        return Buf(self.es.enter_context(self.nc.sbuf_tensor(name, list(shape), dtype)))

    def psum(self, shape, dtype, name=None):
        self.ntile += 1
        name = name or ('p%d' % self.ntile)
        return Buf(self.es.enter_context(self.nc.psum_tensor(name, list(shape), dtype)))

    def dram_in(self, name, shape, dtype):
        return Buf(self.nc.dram_tensor(name, list(shape), dtype, kind="ExternalInput").ap())

    def dram_out(self, name, shape, dtype):
        return Buf(self.nc.dram_tensor(name, list(shape), dtype, kind="ExternalOutput").ap())

    def dram_int(self, name, shape, dtype):
        return Buf(self.nc.dram_tensor(name, list(shape), dtype).ap())

    def barrier(self):
        deps = {k: v for k, v in self.ecnt.items() if v > 0}
        for i in range(self.NDMA):
            if self.dcnt[i] > 0:
                deps[i] = self.dcnt[i]
        for e in self.eng:
            self._wait(e, dict(deps))

    def collective(self, src, dst, src_ap=None, dst_ap=None):
        self._wait('pool', self._deps('pool', [src], [dst]))
        self.ecnt['cc'] += 1
        sa = src.t if src_ap is None else src_ap
        da = dst.t if dst_ap is None else dst_ap
        self.eng['pool'].collective_compute("AllGather", ALU.bypass, replica_groups=GROUPS,
                                            ins=[sa.opt()], outs=[da.opt()]).then_inc(self.esem['cc'], 1)
        self._mark(('cc', self.ecnt['cc']), [src], [dst])

    def collective_chunks(self, src, dst, nch, rows):
        for ch in range(nch):
            self.collective(src, dst, src.t[ch * rows:(ch + 1) * rows, :], dst.t[ch * 4 * rows:(ch + 1) * 4 * rows, :])

    def idma(self, out_ap, in_ap, idx_ap, reads=(), writes=()):
        deps = self._deps('pool', reads, writes)
        i = self.dnext
        self.dnext = (i + 1) % self.NDMA
        if self.dcnt[i] > 0 and deps.get(i, 0) < self.dcnt[i]:
            deps[i] = self.dcnt[i]
        self._wait('pool', deps)
        self.dcnt[i] += 16
        self.eng['pool'].indirect_dma_start(out=out_ap, out_offset=None, in_=in_ap,
                                            in_offset=bass.IndirectOffsetOnAxis(ap=idx_ap, axis=0)
                                            ).then_inc(self.dsem[i], 16)
        self._mark((i, self.dcnt[i]), reads, writes)

    def _deps(self, e, reads, writes):
        deps = {}

        def add(tag):
            if tag is None:
                return
            k, v = tag
            if deps.get(k, 0) < v:
                deps[k] = v
        for b in reads:
            add(b.w)
        for b in writes:
            add(b.w)
            for k, v in b.r.items():
                add((k, v))
        if e == 'pe':
            deps.pop('pe', None)
        return deps

    def _wait(self, e, deps):
        kn = self.known[e]
        for k, v in deps.items():
            if kn.get(k, 0) < v:
                sem = self.esem[k] if isinstance(k, str) else self.dsem[k]
                self.eng[e].wait_ge(sem, v)
                kn[k] = v

    def _mark(self, tag, reads, writes):
        k, v = tag
        for b in writes:
            b.w = tag
            b.r = {}
        for b in reads:
            if b not in writes:
                if b.r.get(k, 0) < v:
                    b.r[k] = v

    def op(self, e, fn, reads=(), writes=()):
        self._wait(e, self._deps(e, reads, writes))
        ins = fn(self.eng[e])
        self.ecnt[e] += 1
        ins.then_inc(self.esem[e], 1)
        self._mark((e, self.ecnt[e]), reads, writes)

    def dma(self, out_ap, in_ap, reads=(), writes=(), q='sp'):
        deps = self._deps(q, reads, writes)
        i = self.dnext
        self.dnext = (i + 1) % self.NDMA
        if self.dcnt[i] > 0 and deps.get(i, 0) < self.dcnt[i]:
            deps[i] = self.dcnt[i]
        self._wait(q, deps)
        self.dcnt[i] += 16
        self.eng[q].dma_start(out=out_ap, in_=in_ap).then_inc(self.dsem[i], 16)
        tag = (i, self.dcnt[i])
        self._mark(tag, reads, writes)
        return tag

    def finish(self, out_bufs):
        deps = {}
        for b in out_bufs:
            if b.w is not None:
                k, v = b.w
                deps[k] = max(deps.get(k, 0), v)
        for i in range(self.NDMA):
            if self.dcnt[i] > 0:
                deps[i] = self.dcnt[i]
        self.known['sp'] = {}
        self._wait('sp', deps)


def new_nc():
    return bass.Bass("TRN2", target_bir_lowering=False)


def run_spmd(nc, in_maps):
    res = run_bass_kernel_spmd(nc, in_maps, core_ids=list(range(NCORE)))
    return res.results


MODC = 6 * D // NCORE
WCOLS_L = 8 * IN_COLS + 8 * D + 8 * 2 * DFF + 22 * D
WCOLS = DEPTH * WCOLS_L
WC_CORE = (WCOLS + NCORE - 1) // NCORE
WC_TILE = 2048
WC_CORE = ((WC_CORE + WC_TILE - 1) // WC_TILE) * WC_TILE


def build_prep():
    nc = new_nc()
    with ExitStack() as es:
        S = Sched(nc, es)
        cT = S.dram_in("cT", [128, 8, B], F32)
        wada = S.dram_in("wada", [128, DEPTH, 8, MODC], F32)
        bada = S.dram_in("bada", [B, DEPTH, MODC], F32)
        wf = S.dram_in("wf", [128, WC_CORE], F32)
        mod = S.dram_out("mod", [B, DEPTH, MODC], F32)
        wb = S.dram_out("wb", [128, WC_CORE], BF16)

        c_sb = S.tile([128, 8, B], F32)
        sc_sb = S.tile([128, 8, B], F32)
        sg_sb = S.tile([128, 8, B], F32)
        b_sb = S.tile([B, DEPTH, MODC], F32)
        o_sb = S.tile([B, DEPTH, MODC], F32)
        S.dma(c_sb[:], cT[:], reads=[cT], writes=[c_sb])
        S.dma(b_sb[:], bada[:], reads=[bada], writes=[b_sb])
        S.op('act', lambda e: e.activation(out=sg_sb[:], in_=c_sb[:], func=AF.Sigmoid),
             reads=[c_sb], writes=[sg_sb])
        S.op('dve', lambda e: e.tensor_tensor(out=sc_sb[:], in0=c_sb[:], in1=sg_sb[:], op=ALU.mult),
             reads=[c_sb, sg_sb], writes=[sc_sb])
        wts = [S.tile([128, 8, MODC], F32) for _ in range(2)]
        pss = [S.psum([B, 512], F32) for _ in range(2)]
        pi = 0
        for l in range(DEPTH):
            wt = wts[l % 2]
            S.dma(wt[:], wada[:, l, :, :], reads=[wada], writes=[wt])
            for (c0, cn) in ((0, 512), (512, MODC - 512)):
                ps = pss[pi % 2]
                pi += 1
                for k in range(8):
                    S.op('pe', lambda e, k=k, ps=ps, wt=wt, c0=c0, cn=cn: e.matmul(
                        ps[:, 0:cn], sc_sb[:, k, :], wt[:, k, c0:c0 + cn], start=(k == 0), stop=(k == 7)),
                        reads=[sc_sb, wt], writes=[ps])
                S.op('dve', lambda e, ps=ps, l=l, c0=c0, cn=cn: e.tensor_tensor(
                    out=o_sb[:, l, c0:c0 + cn], in0=ps[:, 0:cn], in1=b_sb[:, l, c0:c0 + cn], op=ALU.add),
                    reads=[ps, b_sb], writes=[o_sb])
        S.dma(mod[:], o_sb[:], reads=[o_sb], writes=[mod])
        fts = [S.tile([128, WC_TILE], F32) for _ in range(3)]
        bts = [S.tile([128, WC_TILE], BF16) for _ in range(3)]
        for i in range(WC_CORE // WC_TILE):
            ft = fts[i % 3]
            bt = bts[i % 3]
            sl = slice(i * WC_TILE, (i + 1) * WC_TILE)
            S.dma(ft[:], wf[:, sl], reads=[wf], writes=[ft])
            eng = 'dve' if i % 2 == 0 else 'pool'
            S.op(eng, lambda e, ft=ft, bt=bt: e.tensor_copy(out=bt[:], in_=ft[:]), reads=[ft], writes=[bt])
            S.dma(wb[:, sl], bt[:], reads=[bt], writes=[wb])
        S.finish([mod, wb])
    return nc


def w_to_pk(w):
    K, N = w.shape
    return np.ascontiguousarray(w.reshape(K // 128, 128, N).transpose(1, 0, 2)).reshape(128, -1)


def run_prep(c, w_ada, b_ada, w_in, w_out, w_gate_up, w_down):
    cT = np.ascontiguousarray(c.T.reshape(8, 128, B).transpose(1, 0, 2))
    flat = []
    for l in range(DEPTH):
        flat += [w_to_pk(w_in[l]), w_to_pk(w_out[l]), w_to_pk(w_gate_up[l]), w_to_pk(w_down[l])]
    flat = np.concatenate(flat, axis=1)
    pad = NCORE * WC_CORE - flat.shape[1]
    flat = np.concatenate([flat, np.zeros((128, pad), np.float32)], axis=1)
    in_maps = []
    for i in range(NCORE):
        wsl = w_ada[:, :, i * MODC:(i + 1) * MODC]
        wsl = np.ascontiguousarray(wsl.reshape(DEPTH, 8, 128, MODC).transpose(2, 0, 1, 3))
        bsl = np.ascontiguousarray(np.broadcast_to(b_ada[None, :, i * MODC:(i + 1) * MODC], (B, DEPTH, MODC)))
        in_maps.append({"cT": cT, "wada": wsl, "bada": bsl,
                        "wf": np.ascontiguousarray(flat[:, i * WC_CORE:(i + 1) * WC_CORE])})
    res = run_spmd(build_prep(), in_maps)
    mod = np.concatenate([r["mod"] for r in res], axis=2)
    mod = np.ascontiguousarray(mod.transpose(1, 0, 2))
    wbf = np.concatenate([r["wb"] for r in res], axis=1)[:, :WCOLS]
    ws = []
    off = 0
    for l in range(DEPTH):
        d = {}
        for name, kk, n in (("w_in", 8, IN_COLS), ("w_out", 8, D), ("w_gu", 8, 2 * DFF), ("w_down", 22, D)):
            d[name] = wbf[:, off:off + kk * n].reshape(128, kk, n)
            off += kk * n
        ws.append(d)
    return mod, ws


NQG = T // 512


class MixCtx:
    pass


def load_hn_plain(S, hn, hnT, tt):
    S.dma(hn[:], hnT[:, :, tt * 512:(tt + 1) * 512], reads=[hnT], writes=[hn])


def load_hn_gathered(S, hn, hna, tt):
    q = tt // 8
    src = hna.t.rearrange("(k r p) t -> r p k t", k=8, r=4)[q]
    S.dma(hn[:], src[:, :, (tt % 8) * 512:(tt % 8 + 1) * 512], reads=[hna], writes=[hn])


def load_w(S, C, wu, wu_d):
    if C.w32 is None:
        S.dma(wu[:], wu_d[:], reads=[wu_d], writes=[wu])
    else:
        n = wu_d.t.shape[2]
        S.dma(C.w32[:, :, 0:n], wu_d[:], reads=[wu_d], writes=[C.w32])
        S.op('dve', lambda e: e.tensor_copy(out=wu[:], in_=C.w32[:, :, 0:n]), reads=[C.w32], writes=[wu])


def mix_common(S, banks=None, fused=False):
    C = MixCtx()
    C.load_hn = load_hn_gathered if fused else load_hn_plain
    C.w32 = S.tile([128, 8, 260], F32) if fused else None
    C.banks = banks if banks is not None else [S.psum([128, 512], F32) for _ in range(8)]
    C.ones64 = S.tile([128, 128], F32)
    S.op('dve', lambda e: e.memset(C.ones64[:], 1.0), writes=[C.ones64])
    C.epsc = S.tile([128, 1], F32)
    S.op('dve', lambda e: e.memset(C.epsc[:], EPS), writes=[C.epsc])
    C.lnsc = S.tile([128, 1], F32)
    S.op('dve', lambda e: e.memset(C.lnsc[:], float(np.log(HD ** -0.5))), writes=[C.lnsc])
    C.sel = S.tile([65, 64], F32)
    S.op('dve', lambda e: e.memset(C.sel[:], 0.0), writes=[C.sel])
    S.op('dve', lambda e: e.memset(C.sel[64:65, :], 1.0), writes=[C.sel])
    C.hn = [S.tile([128, 8, 512], BF16) for _ in range(2)]
    C.sq = [S.tile([128, 512], F32) for _ in range(2)]
    C.rs = [S.tile([128, 512], F32) for _ in range(2)]
    return C


def attn_alloc(S, C, with_pageB):
    C.Qa = S.tile([128, T], BF16)
    C.QaB = S.tile([128, T // 2], BF16) if with_pageB else None
    C.Ka = S.tile([128, T], BF16)
    NT_ = T // 512
    C.QF = [Buf() for _ in range(NT_)]
    C.QR = [Buf() for _ in range(NT_)]
    C.KF = [Buf() for _ in range(NT_)]
    C.KR = [Buf() for _ in range(NT_)]
    C.QBF = [Buf() for _ in range(NT_)]
    C.QBR = [Buf() for _ in range(NT_)]
    C.Va = S.tile([128, T // 128, 65], BF16)
    S.op('pool', lambda e: e.memset(C.Va[:], 1.0), writes=[C.Va])
    C.P = [S.tile([128, 512], BF16) for _ in range(5)]
    C.M = [S.tile([128, 512], F32) for _ in range(2)]
    C.dm = S.tile([128, 4, 512], F32)
    C.osb = [S.tile([65, 512], F32) for _ in range(2)]
    C.rd = [S.tile([64, 512], F32) for _ in range(2)]
    C.ysb = [S.tile([64, 512], BF16) for _ in range(2)]


def emit_qknorm(S, C, ps, lo, gcol, dests, idx):
    hi = lo + 64
    sq = C.sq[idx % 2]
    rs = C.rs[idx % 2]
    bSS = C.banks[3 + 4 * (idx % 2)]
    S.op('act', lambda e: e.activation(out=sq[lo:hi, :], in_=ps[lo:hi, :], func=AF.Square),
         reads=[ps], writes=[sq])
    S.op('pe', lambda e: e.matmul(bSS[lo:hi, :], C.ones64[lo:hi, lo:hi], sq[lo:hi, :], start=True, stop=True),
         reads=[C.ones64, sq], writes=[bSS])
    S.op('act', lambda e: e.activation(out=sq[lo:hi, :], in_=bSS[lo:hi, :], func=AF.Ln, bias=C.epsc[lo:hi, 0:1],
                                       scale=1.0 / HD), reads=[bSS, C.epsc], writes=[sq])
    S.op('act', lambda e: e.activation(out=rs[lo:hi, :], in_=sq[lo:hi, :], func=AF.Exp, scale=-0.5),
         reads=[sq], writes=[rs])
    for ap, bufs in dests:
        S.op('dve', lambda e, ap=ap: e.scalar_tensor_tensor(out=ap, in0=ps[lo:hi, :], scalar=gcol, in1=rs[lo:hi, :],
                                                            op0=ALU.mult, op1=ALU.mult),
             reads=[ps, rs], writes=bufs)


def emit_attention(S, C, yout, prow, drow, dsplit=False):
    Qa, QaB, Ka, Va, dm = C.Qa, C.QaB, C.Ka, C.Va, C.dm
    banks = C.banks
    for g in range(NQG):
        n = 4 * g + 4
        bO = banks[4 + g % 2]
        q0 = g * 512

        def emitS(kt):
            lo, hi = drow if kt >= 4 * g else prow
            if kt < 64 or QaB is None:
                Qp, qc = Qa, q0
            else:
                Qp, qc = QaB, q0 - T // 2
            bS = banks[kt % 4]
            kq = kt // 4
            if Qp is Qa:
                rd = [C.KF[kq], C.KR[kq], C.QF[g], C.QR[g]]
            else:
                rd = [C.KF[kq], C.KR[kq], C.QBF[g], C.QBR[g]]
            if dsplit and 4 * g <= kt < 4 * g + 2:
                S.op('pe', lambda e: e.matmul(bS[:, 0:256], Ka[lo:hi, kt * 128:(kt + 1) * 128], Qp[lo:hi, qc:qc + 256],
                                              start=True, stop=True), reads=rd, writes=[bS])
                plo, phi = prow
                S.op('pe', lambda e: e.matmul(bS[:, 256:512], Ka[plo:phi, kt * 128:(kt + 1) * 128],
                                              Qp[plo:phi, qc + 256:qc + 512], start=True, stop=True),
                     reads=rd, writes=[bS])
            else:
                S.op('pe', lambda e: e.matmul(bS[:, :], Ka[lo:hi, kt * 128:(kt + 1) * 128], Qp[lo:hi, qc:qc + 512],
                                              start=True, stop=True), reads=rd, writes=[bS])
            P = C.P[kt % 5]
            if kt >= 4 * g:
                M = C.M[kt % 2]
                S.op('dve', lambda e: e.tensor_tensor(out=M[:], in0=bS[:], in1=dm[:, kt - 4 * g, :], op=ALU.add),
                     reads=[bS, dm], writes=[M])
                S.op('act', lambda e: e.activation(out=P[:], in_=M[:], func=AF.Exp), reads=[M], writes=[P])
            else:
                S.op('act', lambda e: e.activation(out=P[:], in_=bS[:], func=AF.Exp), reads=[bS], writes=[P])

        def emitO(kt):
            P = C.P[kt % 5]
            S.op('pe', lambda e: e.matmul(bO[0:65, :], Va[:, kt, :], P[:], start=(kt == 0), stop=(kt == n - 1)),
                 reads=[Va, P], writes=[bO])
        LAG = 3
        for i in range(n + LAG):
            if i < n:
                emitS(i)
            if i >= LAG:
                emitO(i - LAG)
        osb = C.osb[g % 2]
        rd = C.rd[g % 2]
        ysb = C.ysb[g % 2]
        bD = banks[6]
        S.op('act', lambda e: e.activation(out=osb[:], in_=bO[0:65, :], func=AF.Copy), reads=[bO], writes=[osb])
        S.op('pe', lambda e: e.matmul(bD[0:64, :], C.sel[:, :], osb[:], start=True, stop=True),
             reads=[C.sel, osb], writes=[bD])
        S.op('dve', lambda e: e.reciprocal(out=rd[:], in_=bD[0:64, :]), reads=[bD], writes=[rd])
        S.op('dve', lambda e: e.tensor_tensor(out=ysb[:], in0=osb[0:64, :], in1=rd[:], op=ALU.mult),
             reads=[osb, rd], writes=[ysb])
        S.dma(yout[0:64, q0:q0 + 512], ysb[:], reads=[ysb], writes=[yout])


def emit_proj_mm(S, C, hnT, wu, tt, qcols, kcols, vcols):
    hn = C.hn[tt % 2]
    if tt == 0:
        C.load_hn(S, hn, hnT, 0)
    if tt + 1 < T // 512:
        C.load_hn(S, C.hn[(tt + 1) % 2], hnT, tt + 1)
    st = 4 * (tt % 2)
    bQ, bK, bV = C.banks[st], C.banks[st + 1], C.banks[st + 2]
    nq = qcols[1] - qcols[0]
    nk = kcols[1] - kcols[0]
    nv = vcols[1] - vcols[0]
    for k in range(8):
        S.op('pe', lambda e, k=k: e.matmul(bQ[0:nq, :], wu[:, k, qcols[0]:qcols[1]], hn[:, k, :],
                                           start=(k == 0), stop=(k == 7)), reads=[wu, hn], writes=[bQ])
    for k in range(8):
        S.op('pe', lambda e, k=k: e.matmul(bK[0:nk, :], wu[:, k, kcols[0]:kcols[1]], hn[:, k, :],
                                           start=(k == 0), stop=(k == 7)), reads=[wu, hn], writes=[bK])
    for j in range(4):
        for k in range(8):
            S.op('pe', lambda e, k=k, j=j: e.matmul(bV[:, j * nv:(j + 1) * nv], hn[:, k, j * 128:(j + 1) * 128],
                                                    wu[:, k, vcols[0]:vcols[1]], start=(k == 0), stop=(k == 7)),
                 reads=[wu, hn], writes=[bV])
    return bQ, bK, bV


def fox_alloc(S, C):
    C.e1 = S.tile([65, 512], F32)
    C.lf = S.tile([65, 512], F32)
    C.cn = [S.tile([65, 512], F32) for _ in range(2)]
    C.rt = [S.tile([65, 512], F32) for _ in range(2)]
    C.stg = [S.tile([65, 3, 512], BF16) for _ in range(2)]
    C.onesrow = S.tile([65, 512], F32)
    S.op('dve', lambda e: e.memset(C.onesrow[:], 1.0), writes=[C.onesrow])
    C.fwu = S.tile([128, 8, 200], BF16)
    C.fg = S.tile([64, 2], F32)
    C.fgs = S.tile([64, 1], F32)
    C.ffb = S.tile([65, 1], F32)
    C.fnb = S.tile([65, 1], F32)


def emit_fox_unit(S, C, hnT, wu_d, g_d, fb_d, dm_d, yout):
    Qa, Ka, Va = C.Qa, C.Ka, C.Va
    wu, gq, gs, fb, nfb = C.fwu, C.fg, C.fgs, C.ffb, C.fnb
    load_w(S, C, wu, wu_d)
    S.dma(gq[:], g_d[:], reads=[g_d], writes=[gq])
    S.dma(fb[64:65, :], fb_d[64:65, :], reads=[fb_d], writes=[fb])
    S.dma(C.dm[:], dm_d[:], reads=[dm_d], writes=[C.dm])
    S.op('dve', lambda e: e.tensor_scalar(out=gs[:], in0=gq[:, 0:1], scalar1=HD ** -0.5, scalar2=None,
                                          op0=ALU.mult), reads=[gq], writes=[gs])
    S.op('dve', lambda e: e.tensor_scalar(out=nfb[64:65, :], in0=fb[64:65, :], scalar1=-1.0, scalar2=None,
                                          op0=ALU.mult), reads=[fb], writes=[nfb])
    S.op('pool', lambda e: e.memset(Qa[64:70, :], 1.0), writes=C.QR)
    S.op('pool', lambda e: e.memset(Ka[64:70, :], -1.0), writes=C.KR)
    for tt in range(T // 512):
        cs = slice(tt * 512, (tt + 1) * 512)
        bQ, bK, bV = emit_proj_mm(S, C, hnT, wu, tt, (0, 65), (65, 129), (129, 193))
        emit_qknorm(S, C, bQ, 0, gs[:, 0:1], [(Qa[0:64, cs], [C.QF[tt]])], 2 * tt)
        emit_qknorm(S, C, bK, 0, gq[:, 1:2], [(Ka[0:64, cs], [C.KF[tt]])], 2 * tt + 1)
        S.op('act', lambda e: e.activation(out=Va[:, 4 * tt:4 * tt + 4, 0:64],
                                           in_=bV[:, 0:256].rearrange("p (j d) -> p j d", j=4), func=AF.Copy),
             reads=[bV], writes=[Va])
        e1, lf = C.e1, C.lf
        cn, cnp = C.cn[tt % 2], C.cn[(tt + 1) % 2]
        rt = C.rt
        stg = C.stg[tt % 2]
        r = slice(64, 65)
        S.op('act', lambda e: e.activation(out=e1[r, :], in_=bQ[r, :], func=AF.Exp, bias=nfb[r, 0:1], scale=-1.0),
             reads=[bQ, nfb], writes=[e1])
        S.op('act', lambda e: e.activation(out=lf[r, :], in_=e1[r, :], func=AF.Ln, bias=1.0, scale=1.0),
             reads=[e1], writes=[lf])
        init = 0.0 if tt == 0 else cnp[r, 511:512]
        S.op('dve', lambda e: e.tensor_tensor_scan(out=cn[r, :], data0=C.onesrow[r, :], data1=lf[r, :], initial=init,
                                                   op0=ALU.mult, op1=ALU.add),
             reads=[C.onesrow, lf, cnp], writes=[cn])
        S.op('dve', lambda e: e.tensor_copy(out=stg[r, 0, :], in_=cn[r, :]), reads=[cn], writes=[stg])
        S.op('dve', lambda e: e.tensor_tensor(out=rt[0][r, :], in0=cn[r, :], in1=stg[r, 0, :], op=ALU.subtract),
             reads=[cn, stg], writes=[rt[0]])
        S.op('dve', lambda e: e.tensor_copy(out=stg[r, 1, :], in_=rt[0][r, :]), reads=[rt[0]], writes=[stg])
        S.op('dve', lambda e: e.tensor_tensor(out=rt[1][r, :], in0=rt[0][r, :], in1=stg[r, 1, :], op=ALU.subtract),
             reads=[rt[0], stg], writes=[rt[1]])
        S.op('dve', lambda e: e.tensor_copy(out=stg[r, 2, :], in_=rt[1][r, :]), reads=[rt[1]], writes=[stg])
        for i in range(3):
            S.dma(Qa[64 + i:65 + i, cs], stg[r, i, :], reads=[stg], writes=[C.QR[tt]], q='pool')
            S.dma(Ka[67 + i:68 + i, cs], stg[r, i, :], reads=[stg], writes=[C.KR[tt]], q='pool')
    emit_attention(S, C, yout, (0, 70), (0, 70))


def fox_dmask():
    m = np.zeros((128, 4, 512), np.float32)
    k = np.arange(128)[:, None]
    q = np.arange(512)[None, :]
    for a in range(4):
        m[:, a, :] = np.where(a * 128 + k <= q, 0.0, NEG)
    return m


def build_mix(kinds):
    nc = new_nc()
    with ExitStack() as es:
        S = Sched(nc, es)
        hnT = S.dram_in("hnT", [128, 8, T], BF16)
        C = mix_common(S)
        outs = []
        if 'fox' in kinds or 'moba' in kinds:
            attn_alloc(S, C, 'moba' in kinds)
        if 'fox' in kinds:
            fox_alloc(S, C)
            fdm = S.dram_in("fox_dm", [128, 4, 512], F32)
        if 'ml' in kinds:
            ml_alloc(S, C)
            mU = S.dram_in("ml_U", [128, 128], F32)
        if 'moba' in kinds or 'ml' in kinds:
            idd = S.dram_in("ident", [128, 128], F32)
        if 'moba' in kinds:
            moba_alloc(S, C)
            bdm = S.dram_in("moba_dm", [128, 4, 512], F32)
            boh = S.dram_in("moba_oh", [32, T], BF16)
        for u, kind in enumerate(kinds):
            if kind == 'fox':
                wu_d = S.dram_in("wu%d" % u, [128, 8, 200], BF16)
                g_d = S.dram_in("g%d" % u, [64, 2], F32)
                fb_d = S.dram_in("fb%d" % u, [65, 1], F32)
                yout = S.dram_out("y%d" % u, [64, T], BF16)
                emit_fox_unit(S, C, hnT, wu_d, g_d, fb_d, fdm, yout)
                outs.append(yout)
            elif kind == 'moba':
                wu_d = S.dram_in("wu%d" % u, [128, 8, 200], BF16)
                g_d = S.dram_in("g%d" % u, [64, 2], F32)
                qc_d = S.dram_in("qc%d" % u, [8, T], BF16)
                kc_d = S.dram_in("kc%d" % u, [8, T], BF16)
                yout = S.dram_out("y%d" % u, [64, T], BF16)
                emit_moba_unit(S, C, hnT, wu_d, g_d, qc_d, kc_d, boh, bdm, idd, yout)
                outs.append(yout)
            elif kind == 'ml':
                wu_d = S.dram_in("wu%d" % u, [128, 8, 260], BF16)
                cw_d = S.dram_in("cw%d" % u, [64, 2, 4], F32)
                cb_d = S.dram_in("cb%d" % u, [64, 2], F32)
                ib_d = S.dram_in("ib%d" % u, [128, 1], F32)
                fb_d = S.dram_in("fb%d" % u, [128, 1], F32)
                g_d = S.dram_in("mg%d" % u, [128, 4, 64], F32)
                yout = S.dram_out("y%d" % u, [64, T], BF16)
                emit_ml_unit(S, C, hnT, wu_d, cw_d, cb_d, ib_d, fb_d, g_d, mU, idd, yout)
                outs.append(yout)
        S.finish(outs)
    return nc


def to_fm(a):
    n, f = a.shape
    return np.ascontiguousarray(a.reshape(n, f // 128, 128).transpose(2, 1, 0))


def fox_unit_inputs(u, ws_l, inp, l, h):
    w = ws_l['w_in']
    wu = np.zeros((128, 8, 200), w.dtype)
    wu[:, :, 0:64] = w[:, :, O_FQ + 64 * h:O_FQ + 64 * h + 64]
    wu[:, :, 64] = w[:, :, O_FF + h]
    wu[:, :, 65:129] = w[:, :, O_FK + 64 * h:O_FK + 64 * h + 64]
    wu[:, :, 129:193] = w[:, :, O_FV + 64 * h:O_FV + 64 * h + 64]
    g = np.stack([inp['fox_q_g'][l], inp['fox_k_g'][l]], axis=1).astype(np.float32)
    fb = np.zeros((65, 1), np.float32)
    fb[64, 0] = inp['fox_f_bias'][l][h]
    return {"wu%d" % u: wu, "g%d" % u: np.ascontiguousarray(g), "fb%d" % u: fb}


def moba_alloc(S, C):
    C.bwu = S.tile([128, 8, 200], BF16)
    C.bg = S.tile([64, 2], F32)
    C.bgs = S.tile([64, 1], F32)
    C.qn32 = [S.tile([64, 512], F32) for _ in range(2)]
    C.kn32 = [S.tile([64, 512], F32) for _ in range(2)]
    C.kmT = S.tile([64, 64], F32)
    C.gsb = S.tile([128, 4, 64], F32)
    C.m8 = S.tile([128, 4, 8], F32)
    C.thr = S.tile([128, 4], F32)
    C.MB = S.tile([128, 4, 2, 128], F32)
    S.op('pool', lambda e: e.memset(C.MB[:], 0.0), writes=[C.MB])
    C.ident = S.tile([128, 128], F32)
    C.mstg = [S.tile([32, 512], BF16) for _ in range(4)]


def emit_moba_unit(S, C, hnT, wu_d, g_d, qc_d, kc_d, oh_d, dm_d, id_d, yout):
    Qa, QaB, Ka, Va = C.Qa, C.QaB, C.Ka, C.Va
    wu, gq, gs = C.bwu, C.bg, C.bgs
    H2 = T // 2
    load_w(S, C, wu, wu_d)
    S.dma(gq[:], g_d[:], reads=[g_d], writes=[gq])
    S.dma(C.dm[:], dm_d[:], reads=[dm_d], writes=[C.dm])
    S.dma(C.ident[:], id_d[:], reads=[id_d], writes=[C.ident])
    S.op('dve', lambda e: e.tensor_scalar(out=gs[:], in0=gq[:, 0:1], scalar1=HD ** -0.5, scalar2=None,
                                          op0=ALU.mult), reads=[gq], writes=[gs])
    S.op('pool', lambda e: e.memset(Qa[64:96, :], 0.0), writes=C.QR)
    S.op('pool', lambda e: e.memset(QaB[64:96, :], 0.0), writes=C.QBR)
    S.op('pool', lambda e: e.memset(Ka[64:96, :], 0.0), writes=C.KR)
    S.dma(Qa[64:72, :], qc_d[:], reads=[qc_d], writes=C.QR)
    S.dma(QaB[64:72, :], qc_d[:, H2:], reads=[qc_d], writes=C.QBR)
    S.dma(Ka[64:72, :], kc_d[:], reads=[kc_d], writes=C.KR)
    S.dma(Ka[96:128, :], oh_d[:], reads=[oh_d], writes=C.KR)
    S.op('dve', lambda e: e.memset(C.kmT[:], 0.0), writes=[C.kmT])
    for tt in range(T // 512):
        cs = slice(tt * 512, (tt + 1) * 512)
        csB = slice(tt * 512 - H2, (tt + 1) * 512 - H2)
        second = tt >= 16
        bQ, bK, bV = emit_proj_mm(S, C, hnT, wu, tt, (0, 64), (65, 129), (129, 193))
        qn = C.qn32[tt % 2]
        kn = C.kn32[tt % 2]
        qd = [(Qa[0:64, cs], [C.QF[tt]]), (qn[:, :], [qn])]
        if second:
            qd.append((QaB[0:64, csB], [C.QBF[tt]]))
        emit_qknorm(S, C, bQ, 0, gs[:, 0:1], qd, 2 * tt)
        emit_qknorm(S, C, bK, 0, gq[:, 1:2], [(Ka[0:64, cs], [C.KF[tt]]), (kn[:, :], [kn])], 2 * tt + 1)
        S.op('act', lambda e: e.activation(out=Va[:, 4 * tt:4 * tt + 4, 0:64],
                                           in_=bV[:, 0:256].rearrange("p (j d) -> p j d", j=4), func=AF.Copy),
             reads=[bV], writes=[Va])
        S.op('dve', lambda e: e.tensor_reduce(out=C.kmT[:, 2 * tt:2 * tt + 2],
                                              in_=kn[:, :].rearrange("p (a n) -> p a n", a=2),
                                              axis=AX.X, op=ALU.add), reads=[kn], writes=[C.kmT])
        o4 = 4 * ((tt + 1) % 2)
        bG = C.banks[2 + o4]
        for j in range(4):
            S.op('pe', lambda e, j=j: e.matmul(bG[:, j * 64:(j + 1) * 64], qn[:, j * 128:(j + 1) * 128],
                                               C.kmT[:, :], start=True, stop=True),
                 reads=[qn, C.kmT], writes=[bG])
        gsb, m8, thr, MB = C.gsb, C.m8, C.thr, C.MB
        S.op('act', lambda e: e.activation(out=gsb[:], in_=bG[:, 0:256].rearrange("p (j n) -> p j n", j=4),
                                           func=AF.Copy), reads=[bG], writes=[gsb])
        for half in range(2):
            qblk = 2 * tt + half
            S.op('dve', lambda e: e.memset(gsb[:, 2 * half:2 * half + 2, qblk:64], -1e30), writes=[gsb])
        for j in range(4):
            S.op('dve', lambda e, j=j: e.max(out=m8[:, j, :], in_=gsb[:, j, :]), reads=[gsb], writes=[m8])
        S.op('dve', lambda e: e.tensor_scalar(out=thr[:], in0=m8[:, :, 2], scalar1=-1e29, scalar2=None, op0=ALU.max),
             reads=[m8], writes=[thr])
        for j in range(4):
            S.op('dve', lambda e, j=j: e.tensor_scalar(out=MB[:, j, :, 0:32],
                                                       in0=gsb[:, j, :].rearrange("p (a n) -> p a n", a=2),
                                                       scalar1=thr[:, j:j + 1], scalar2=-NEG,
                                                       op0=ALU.is_ge, op1=ALU.mult), reads=[gsb, thr], writes=[MB])
        S.op('dve', lambda e: e.tensor_scalar(out=MB[:, :, :, 0:32], in0=MB[:, :, :, 0:32], scalar1=NEG, scalar2=None,
                                              op0=ALU.add), reads=[MB], writes=[MB])
        for pg in range(2 if second else 1):
            bT = C.banks[pg + o4]
            for j in range(4):
                S.op('pe', lambda e, j=j, pg=pg: e.transpose(out=bT[:, j * 128:(j + 1) * 128], in_=MB[:, j, pg, :],
                                                            identity=C.ident[:, :]),
                     reads=[MB, C.ident], writes=[bT])
            stg = C.mstg[(2 * tt + pg) % 4]
            S.op('act', lambda e: e.activation(out=stg[:], in_=bT[0:32, :], func=AF.Copy), reads=[bT], writes=[stg])
            if pg == 0:
                S.dma(Qa[96:128, cs], stg[:], reads=[stg], writes=[C.QR[tt]], q='pool')
            else:
                S.dma(QaB[96:128, csB], stg[:], reads=[stg], writes=[C.QBR[tt]], q='pool')
    emit_attention(S, C, yout, (0, 128), (0, 72), dsplit=True)


def moba_dmask():
    m = np.full((128, 4, 512), NEG, np.float32)
    k = np.arange(128)[:, None]
    q = np.arange(512)[None, :]
    for a in range(4):
        kk = a * 128 + k
        ok = (kk // 256 == q // 256) & (kk <= q)
        if a < 2:
            ok = ok | (q >= 256)
        m[:, a, :] = np.where(ok, 0.0, NEG)
    return m


def moba_consts(h):
    slope = float(2.0 ** (-8.0 * (h + 1) / MOBH))
    s1 = np.float32(np.float32(slope).astype(NPBF))
    s2 = np.float32(np.float32(np.float32(slope) - s1).astype(NPBF))
    pos = np.arange(T)
    a = (pos // 256).astype(np.float32)
    b = (pos % 256).astype(np.float32)
    one = np.ones(T, np.float32)
    qc = np.stack([a, b, a, b, 256 * s1 * one, s1 * one, 256 * s2 * one, s2 * one]).astype(NPBF)
    kc = np.stack([-256 * s1 * one, -s1 * one, -256 * s2 * one, -s2 * one, a, b, a, b]).astype(NPBF)
    return qc, kc


def moba_onehot():
    pos = np.arange(T)
    oh = ((pos[None, :] // 256) % 32 == np.arange(32)[:, None]).astype(np.float32)
    return oh.astype(NPBF)


def moba_unit_inputs(u, ws_l, inp, l, h):
    w = ws_l['w_in']
    wu = np.zeros((128, 8, 200), w.dtype)
    wu[:, :, 0:64] = w[:, :, O_BQ + 64 * h:O_BQ + 64 * h + 64]
    wu[:, :, 65:129] = w[:, :, O_BK + 64 * h:O_BK + 64 * h + 64]
    wu[:, :, 129:193] = w[:, :, O_BV + 64 * h:O_BV + 64 * h + 64]
    g = np.stack([inp['moba_q_g'][l], inp['moba_k_g'][l]], axis=1).astype(np.float32)
    qc, kc = moba_consts(h)
    return {"wu%d" % u: wu, "g%d" % u: np.ascontiguousarray(g), "qc%d" % u: qc, "kc%d" % u: kc}


def ml_alloc(S, C):
    C.mwu = S.tile([128, 8, 260], BF16)
    C.mcw = S.tile([64, 2, 4], F32)
    C.mcb = S.tile([64, 2], F32)
    C.mib = S.tile([128, 1], F32)
    C.mfb = S.tile([128, 1], F32)
    C.mnfb = S.tile([128, 1], F32)
    C.mg = S.tile([128, 4, 64], F32)
    C.mU = S.tile([128, 128], F32)
    C.mUb = S.tile([128, 128], BF16)
    C.mid = S.tile([64, 64], F32)
    C.pc = [S.tile([64, 515], F32) for _ in range(2)]
    C.acc = [S.tile([64, 512], F32) for _ in range(2)]
    C.qc = [S.tile([64, 512], BF16) for _ in range(2)]
    C.kc = [S.tile([64, 512], BF16) for _ in range(2)]
    C.kcf = [S.tile([64, 512], F32) for _ in range(2)]
    C.e4 = S.tile([128, 4], F32)
    C.lf4 = S.tile([128, 4], F32)
    C.ii4 = S.tile([128, 4], F32)
    C.tmp4 = S.tile([128, 4], F32)
    C.qs4 = [S.tile([128, 4], F32) for _ in range(2)]
    C.ks4 = [S.tile([128, 4], F32) for _ in range(2)]
    C.eB4 = [S.tile([128, 4], F32) for _ in range(2)]
    C.Vt = [S.tile([128, 4, 65], BF16) for _ in range(2)]
    C.sg = S.tile([128, 4, 64], F32)
    C.GS = [S.tile([128, 4, 64], F32) for _ in range(2)]
    C.sm = [S.tile([128, 128], BF16) for _ in range(2)]
    C.Ktok = [S.tile([128, 64], BF16) for _ in range(2)]
    C.ctmp = S.tile([64, 65], F32)
    C.Cf = S.tile([64, 65], F32)
    C.Cbf = [S.tile([64, 65], BF16) for _ in range(2)]
    C.dn4 = S.tile([128, 4], F32)
    C.fac4 = S.tile([128, 4], F32)
    C.hh = S.tile([128, 4, 64], F32)
    C.hsq = S.tile([128, 4, 64], F32)
    C.ss4 = S.tile([128, 4], F32)
    C.rstd4 = S.tile([128, 4], F32)
    C.Y = [S.tile([64, 512], BF16) for _ in range(2)]
    C.Yf = S.tile([128, 4, 64], F32)
    C.mid128 = S.tile([128, 128], F32)


def emit_ml_unit(S, C, hnT, wu_d, cw_d, cb_d, ib_d, fb_d, g_d, U_d, id_d, yout):
    wu = C.mwu
    load_w(S, C, wu, wu_d)
    S.dma(C.mcw[:], cw_d[:], reads=[cw_d], writes=[C.mcw])
    S.dma(C.mcb[:], cb_d[:], reads=[cb_d], writes=[C.mcb])
    S.dma(C.mib[:], ib_d[:], reads=[ib_d], writes=[C.mib])
    S.dma(C.mfb[:], fb_d[:], reads=[fb_d], writes=[C.mfb])
    S.dma(C.mg[:], g_d[:], reads=[g_d], writes=[C.mg])
    S.dma(C.mU[:], U_d[:], reads=[U_d], writes=[C.mU])
    S.dma(C.mid[:], id_d[0:64, 0:64], reads=[id_d], writes=[C.mid])
    S.dma(C.mid128[:], id_d[:, :], reads=[id_d], writes=[C.mid128])
    S.op('dve', lambda e: e.tensor_copy(out=C.mUb[:], in_=C.mU[:]), reads=[C.mU], writes=[C.mUb])
    S.op('dve', lambda e: e.tensor_scalar(out=C.mnfb[:], in0=C.mfb[:], scalar1=-1.0, scalar2=None, op0=ALU.mult),
         reads=[C.mfb], writes=[C.mnfb])
    for i in range(2):
        S.op('dve', lambda e, i=i: e.memset(C.pc[i][:, 0:3], 0.0), writes=[C.pc[i]])
    S.op('dve', lambda e: e.memset(C.Cf[:], 0.0), writes=[C.Cf])
    S.op('dve', lambda e: e.memset(C.Cbf[0][:], 0.0), writes=[C.Cbf[0]])
    b = C.banks
    cidx = 0
    for tt in range(T // 512):
        hn = C.hn[tt % 2]
        if tt == 0:
            C.load_hn(S, hn, hnT, 0)
        if tt + 1 < T // 512:
            C.load_hn(S, C.hn[(tt + 1) % 2], hnT, tt + 1)
        bQ, bK, bA, bB, bGt, bS, bKC, bO = b
        for k in range(8):
            S.op('pe', lambda e, k=k: e.matmul(bQ[0:64, :], wu[:, k, 0:64], hn[:, k, :], start=(k == 0), stop=(k == 7)),
                 reads=[wu, hn], writes=[bQ])
        for k in range(8):
            S.op('pe', lambda e, k=k: e.matmul(bK[0:64, :], wu[:, k, 64:128], hn[:, k, :], start=(k == 0), stop=(k == 7)),
                 reads=[wu, hn], writes=[bK])
        for j in range(4):
            for k in range(8):
                S.op('pe', lambda e, k=k, j=j: e.matmul(bA[:, j * 66:(j + 1) * 66], hn[:, k, j * 128:(j + 1) * 128],
                                                        wu[:, k, 128:194], start=(k == 0), stop=(k == 7)),
                     reads=[wu, hn], writes=[bA])
        for j in range(4):
            for k in range(8):
                S.op('pe', lambda e, k=k, j=j: e.matmul(bB[:, j * 64:(j + 1) * 64], hn[:, k, j * 128:(j + 1) * 128],
                                                        wu[:, k, 194:258], start=(k == 0), stop=(k == 7)),
                     reads=[wu, hn], writes=[bB])
        bAv = bA[:, 0:264].rearrange("p (j d) -> p j d", j=4)
        qc, kc, kcf = C.qc[tt % 2], C.kc[tt % 2], C.kcf[tt % 2]
        for which, ps in ((0, bQ), (1, bK)):
            pc = C.pc[which]
            acc = C.acc[which]
            S.op('act', lambda e: e.activation(out=pc[:, 3:515], in_=ps[0:64, :], func=AF.Copy), reads=[ps], writes=[pc])
            S.op('dve', lambda e: e.tensor_scalar(out=acc[:], in0=pc[:, 3:515], scalar1=C.mcw[:, which, 3:4],
                                                  scalar2=C.mcb[:, which:which + 1], op0=ALU.mult, op1=ALU.add),
                 reads=[pc, C.mcw, C.mcb], writes=[acc])
            for tap in (2, 1, 0):
                S.op('dve', lambda e, tap=tap: e.scalar_tensor_tensor(out=acc[:], in0=pc[:, tap:tap + 512],
                                                                      scalar=C.mcw[:, which, tap:tap + 1], in1=acc[:],
                                                                      op0=ALU.mult, op1=ALU.add),
                     reads=[pc, C.mcw, acc], writes=[acc])
            S.op('dve', lambda e: e.tensor_copy(out=pc[:, 0:3], in_=pc[:, 512:515]), reads=[pc], writes=[pc])
            if which == 0:
                S.op('act', lambda e: e.activation(out=qc[:], in_=acc[:], func=AF.Silu), reads=[acc], writes=[qc])
            else:
                S.op('act', lambda e: e.activation(out=kcf[:], in_=acc[:], func=AF.Silu), reads=[acc], writes=[kcf])
                S.op('pool', lambda e: e.tensor_copy(out=kc[:], in_=kcf[:]), reads=[kcf], writes=[kc])
        e4, lf4, ii4, tmp4 = C.e4, C.lf4, C.ii4, C.tmp4
        qs4, ks4, eB4 = C.qs4[tt % 2], C.ks4[tt % 2], C.eB4[tt % 2]
        S.op('act', lambda e: e.activation(out=e4[:], in_=bAv[:, :, 65], func=AF.Exp, bias=C.mnfb[:, 0:1], scale=-1.0),
             reads=[bA, C.mnfb], writes=[e4])
        S.op('act', lambda e: e.activation(out=lf4[:], in_=e4[:], func=AF.Ln, bias=1.0, scale=1.0),
             reads=[e4], writes=[lf4])
        S.op('dve', lambda e: e.tensor_scalar(out=ii4[:], in0=bAv[:, :, 64], scalar1=C.mib[:, 0:1], scalar2=None,
                                              op0=ALU.add), reads=[bA, C.mib], writes=[ii4])
        S.op('pe', lambda e: e.matmul(bGt[:, 0:4], C.mU[:, :], lf4[:, :], start=True, stop=True),
             reads=[C.mU, lf4], writes=[bGt])
        S.op('pe', lambda e: e.matmul(bGt[:, 4:8], C.ones64[:, :], lf4[:, :], start=True, stop=True),
             reads=[C.ones64, lf4], writes=[bGt])
        S.op('act', lambda e: e.activation(out=qs4[:], in_=bGt[:, 0:4], func=AF.Exp, scale=-1.0), reads=[bGt], writes=[qs4])
        S.op('act', lambda e: e.activation(out=eB4[:], in_=bGt[:, 4:8], func=AF.Exp, scale=-1.0), reads=[bGt], writes=[eB4])
        S.op('dve', lambda e: e.tensor_tensor(out=tmp4[:], in0=bGt[:, 0:4], in1=ii4[:], op=ALU.add),
             reads=[bGt, ii4], writes=[tmp4])
        S.op('act', lambda e: e.activation(out=ks4[:], in_=tmp4[:], func=AF.Exp, bias=C.lnsc[:, 0:1], scale=1.0),
             reads=[tmp4, C.lnsc], writes=[ks4])
        Vt = C.Vt[tt % 2]
        for j in range(4):
            S.op('dve', lambda e, j=j: e.tensor_scalar(out=Vt[:, j, 0:64], in0=bAv[:, j, 0:64], scalar1=ks4[:, j:j + 1],
                                                       scalar2=None, op0=ALU.mult), reads=[bA, ks4], writes=[Vt])
        S.op('dve', lambda e: e.tensor_copy(out=Vt[:, :, 64], in_=ks4[:, :]), reads=[ks4], writes=[Vt])
        GS = C.GS[tt % 2]
        S.op('act', lambda e: e.activation(out=C.sg[:], in_=bB[:, 0:256].rearrange("p (j d) -> p j d", j=4),
                                           func=AF.Sigmoid), reads=[bB], writes=[C.sg])
        S.op('pool', lambda e: e.tensor_tensor(out=GS[:], in0=C.sg[:], in1=C.mg[:], op=ALU.mult),
             reads=[C.sg, C.mg], writes=[GS])
        for j in range(4):
            js = slice(j * 128, (j + 1) * 128)
            sm = C.sm[cidx % 2]
            Ktok = C.Ktok[cidx % 2]
            Cb_in = C.Cbf[cidx % 2]
            Cb_out = C.Cbf[(cidx + 1) % 2]
            S.op('pe', lambda e: e.matmul(bS[:, 0:128], kc[:, js], qc[:, js], start=True, stop=True),
                 reads=[kc, qc], writes=[bS])
            S.op('dve', lambda e: e.tensor_tensor(out=sm[:], in0=bS[:, 0:128], in1=C.mUb[:], op=ALU.mult),
                 reads=[bS, C.mUb], writes=[sm])
            S.op('pe', lambda e: e.transpose(out=bKC[:, 0:64], in_=kcf[:, js], identity=C.mid[:, :]),
                 reads=[kcf, C.mid], writes=[bKC])
            S.op('act', lambda e: e.activation(out=Ktok[:], in_=bKC[:, 0:64], func=AF.Copy), reads=[bKC], writes=[Ktok])
            S.op('pe', lambda e: e.matmul(bO[:, j * 65:(j + 1) * 65], sm[:, :], Vt[:, j, :], start=True, stop=False),
                 reads=[sm, Vt], writes=[bO])
            S.op('pe', lambda e: e.matmul(bO[:, j * 65:(j + 1) * 65], qc[:, js], Cb_in[:, :], start=False, stop=True),
                 reads=[qc, Cb_in], writes=[bO])
            S.op('pe', lambda e: e.matmul(bKC[0:64, 256:321], Ktok[:, :], Vt[:, j, :], start=True, stop=True),
                 reads=[Ktok, Vt], writes=[bKC])
            S.op('act', lambda e: e.activation(out=C.ctmp[:], in_=bKC[0:64, 256:321], func=AF.Copy,
                                               scale=eB4[0:64, j:j + 1]), reads=[bKC, eB4], writes=[C.ctmp])
            S.op('dve', lambda e: e.scalar_tensor_tensor(out=C.Cf[:], in0=C.Cf[:], scalar=eB4[0:64, j:j + 1],
                                                         in1=C.ctmp[:], op0=ALU.mult, op1=ALU.add),
                 reads=[C.Cf, eB4, C.ctmp], writes=[C.Cf])
            S.op('pool', lambda e: e.tensor_copy(out=Cb_out[:], in_=C.Cf[:]), reads=[C.Cf], writes=[Cb_out])
            cidx += 1
        bOv = bO[:, 0:260].rearrange("p (j d) -> p j d", j=4)
        dn4, fac4, hh, hsq, ss4, rstd4 = C.dn4, C.fac4, C.hh, C.hsq, C.ss4, C.rstd4
        S.op('dve', lambda e: e.tensor_tensor(out=dn4[:], in0=bOv[:, :, 64], in1=qs4[:], op=ALU.mult),
             reads=[bO, qs4], writes=[dn4])
        S.op('act', lambda e: e.activation(out=dn4[:], in_=dn4[:], func=AF.Abs), reads=[dn4], writes=[dn4])
        S.op('dve', lambda e: e.tensor_scalar(out=dn4[:], in0=dn4[:], scalar1=1.0, scalar2=None, op0=ALU.max),
             reads=[dn4], writes=[dn4])
        S.op('dve', lambda e: e.reciprocal(out=dn4[:], in_=dn4[:]), reads=[dn4], writes=[dn4])
        S.op('dve', lambda e: e.tensor_tensor(out=fac4[:], in0=dn4[:], in1=qs4[:], op=ALU.mult),
             reads=[dn4, qs4], writes=[fac4])
        for j in range(4):
            S.op('act', lambda e, j=j: e.activation(out=hh[:, j, :], in_=bOv[:, j, 0:64], func=AF.Copy,
                                                    scale=fac4[:, j:j + 1]), reads=[bO, fac4], writes=[hh])
        S.op('dve', lambda e: e.tensor_tensor(out=hsq[:], in0=hh[:], in1=hh[:], op=ALU.mult), reads=[hh], writes=[hsq])
        S.op('dve', lambda e: e.tensor_reduce(out=ss4[:], in_=hsq[:], axis=AX.X, op=ALU.add), reads=[hsq], writes=[ss4])
        S.op('act', lambda e: e.activation(out=ss4[:], in_=ss4[:], func=AF.Sqrt, bias=C.epsc[:, 0:1], scale=1.0 / HD),
             reads=[ss4, C.epsc], writes=[ss4])
        S.op('dve', lambda e: e.reciprocal(out=rstd4[:], in_=ss4[:]), reads=[ss4], writes=[rstd4])
        Yf = C.Yf
        for j in range(4):
            S.op('dve', lambda e, j=j: e.scalar_tensor_tensor(out=Yf[:, j, :], in0=hh[:, j, :], scalar=rstd4[:, j:j + 1],
                                                              in1=GS[:, j, :], op0=ALU.mult, op1=ALU.mult),
                 reads=[hh, rstd4, GS], writes=[Yf])
        for j in range(4):
            S.op('pe', lambda e, j=j: e.transpose(out=bS[0:64, j * 128:(j + 1) * 128], in_=Yf[:, j, :],
                                                  identity=C.mid128[:, :]), reads=[Yf, C.mid128], writes=[bS])
        Y = C.Y[tt % 2]
        S.op('act', lambda e: e.activation(out=Y[:], in_=bS[0:64, :], func=AF.Copy), reads=[bS], writes=[Y])
        S.dma(yout[0:64, tt * 512:(tt + 1) * 512], Y[:], reads=[Y], writes=[yout])


def ml_consts():
    s = np.arange(128)[:, None]
    j = np.arange(128)[None, :]
    return (s <= j).astype(np.float32)


def ml_unit_inputs(u, ws_l, inp, l, h):
    w = ws_l['w_in']
    wu = np.zeros((128, 8, 260), w.dtype)
    wu[:, :, 0:64] = w[:, :, O_MQ + 64 * h:O_MQ + 64 * h + 64]
    wu[:, :, 64:128] = w[:, :, O_MK + 64 * h:O_MK + 64 * h + 64]
    wu[:, :, 128:192] = w[:, :, O_MV + 64 * h:O_MV + 64 * h + 64]
    wu[:, :, 192] = w[:, :, O_MI + h]
    wu[:, :, 193] = w[:, :, O_MF + h]
    wu[:, :, 194:258] = w[:, :, O_MO + 64 * h:O_MO + 64 * h + 64]
    cw = inp['ml_conv_w'][l]
    cb = inp['ml_conv_b'][l]
    cwq = cw[:, 64 * h:64 * h + 64].T
    cwk = cw[:, 256 + 64 * h:256 + 64 * h + 64].T
    cwu = np.ascontiguousarray(np.stack([cwq, cwk], axis=1)).astype(np.float32)
    cbu = np.ascontiguousarray(np.stack([cb[64 * h:64 * h + 64], cb[256 + 64 * h:256 + 64 * h + 64]], axis=1)).astype(np.float32)
    ib = np.full((128, 1), inp['ml_i_bias'][l][h], np.float32)
    fb = np.full((128, 1), inp['ml_f_bias'][l][h], np.float32)
    g = np.ascontiguousarray(np.broadcast_to(inp['ml_h_g'][l][64 * h:64 * h + 64][None, None, :], (128, 4, 64))).astype(np.float32)
    return {"wu%d" % u: wu, "cw%d" % u: cwu, "cb%d" % u: cbu, "ib%d" % u: ib, "fb%d" % u: fb, "mg%d" % u: g}


NTOK = B * T // NCORE
NTT = 1024
MFF = DFF // 128


def emit_norm_mod(S, C, x, hn, a, shb, shi, ncols):
    for h0 in range(0, ncols, 512):
        hs = slice(h0, h0 + 512)
        bSS = C.nextbank()
        for k in range(8):
            sq = C.sq[k % 2]
            S.op('act', lambda e, k=k: e.activation(out=sq[:], in_=x[:, k, hs], func=AF.Square), reads=[x], writes=[sq])
            S.op('pe', lambda e, k=k: e.matmul(bSS[:, :], C.ones[:, :], sq[:], start=(k == 0), stop=(k == 7)),
                 reads=[C.ones, sq], writes=[bSS])
        rs = C.rs
        S.op('act', lambda e: e.activation(out=rs[:], in_=bSS[:, :], func=AF.Sqrt, bias=C.epsc[:, 0:1], scale=1.0 / D),
             reads=[bSS, C.epsc], writes=[rs])
        S.op('dve', lambda e: e.reciprocal(out=rs[:], in_=rs[:]), reads=[rs], writes=[rs])
        for k in range(8):
            t = C.t[k % 2]
            S.op('dve', lambda e, k=k: e.tensor_tensor(out=t[:], in0=x[:, k, hs], in1=rs[:], op=ALU.mult),
                 reads=[x, rs], writes=[t])
            S.op('act', lambda e, k=k: e.activation(out=hn[:, k, hs], in_=t[:], func=AF.Identity,
                                                    bias=shb[:, shi, k:k + 1], scale=a[:, k:k + 1]),
                 reads=[t, a, shb], writes=[hn])


def build_dense(post, nextnorm):
    nc = new_nc()
    with ExitStack() as es:
        S = Sched(nc, es)
        C = MixCtx()
        banks = [S.psum([128, 512], F32) for _ in range(8)]
        C.bi = 0

        def nextbank():
            C.bi += 1
            return banks[C.bi % 8]
        C.nextbank = nextbank
        C.ones = S.tile([128, 128], F32)
        S.op('dve', lambda e: e.memset(C.ones[:], 1.0), writes=[C.ones])
        C.epsc = S.tile([128, 1], F32)
        S.op('dve', lambda e: e.memset(C.epsc[:], EPS), writes=[C.epsc])
        C.sq = [S.tile([128, 512], F32) for _ in range(2)]
        C.t = [S.tile([128, 512], F32) for _ in range(2)]
        C.rs = S.tile([128, 512], F32)
        xT = S.dram_in("xT", [128, 8, NTOK], F32)
        vec = S.dram_in("vec", [128, 10, 8], F32)
        v = S.tile([128, 10, 8], F32)
        S.dma(v[:], vec[:], reads=[vec], writes=[v])
        a2 = S.tile([128, 8], F32)
        a1n = S.tile([128, 8], F32)
        S.op('dve', lambda e: e.tensor_scalar(out=a2[:], in0=v[:, 2, :], scalar1=1.0, scalar2=None, op0=ALU.add),
             reads=[v], writes=[a2])
        S.op('dve', lambda e: e.tensor_tensor(out=a2[:], in0=a2[:], in1=v[:, 1, :], op=ALU.mult), reads=[a2, v], writes=[a2])
        S.op('dve', lambda e: e.tensor_scalar(out=a1n[:], in0=v[:, 6, :], scalar1=1.0, scalar2=None, op0=ALU.add),
             reads=[v], writes=[a1n])
        S.op('dve', lambda e: e.tensor_tensor(out=a1n[:], in0=a1n[:], in1=v[:, 5, :], op=ALU.mult), reads=[a1n, v], writes=[a1n])
        outs = []
        if post:
            yT = S.dram_in("yT", [128, 8, NTOK], BF16)
            wo = S.dram_in("wo", [8, 128, 8, 128], BF16)
            wg = S.dram_in("wg", [MFF, 128, 8, 128], BF16)
            wu = S.dram_in("wu", [MFF, 128, 8, 128], BF16)
            wd = S.dram_in("wd", [8, 128, MFF, 128], BF16)
            xo = S.dram_out("xo", [128, 8, NTOK], F32)
            outs.append(xo)
            yt = [S.tile([128, 8, NTT], BF16) for _ in range(1)]
            hn2 = S.tile([128, 8, NTT], BF16)
            A = S.tile([128, MFF, NTT], BF16)
            wot = [S.tile([128, 8, 128], BF16) for _ in range(2)]
            wgt = [S.tile([128, 8, 128], BF16) for _ in range(2)]
            wut = [S.tile([128, 8, 128], BF16) for _ in range(2)]
            wdt = [S.tile([128, MFF, 128], BF16) for _ in range(2)]
            sgt = [S.tile([128, 512], F32) for _ in range(2)]
        if nextnorm:
            hno = S.dram_out("hno", [128, 8, NTOK], BF16)
            outs.append(hno)
            hnn = [S.tile([128, 8, NTT], BF16) for _ in range(1)]
        xt = [S.tile([128, 8, NTT], F32) for _ in range(2)]
        for ti in range(NTOK // NTT):
            ts = slice(ti * NTT, (ti + 1) * NTT)
            x = xt[ti % 2]
            S.dma(x[:], xT[:, :, ts], reads=[xT], writes=[x])
            if post:
                y = yt[0]
                S.dma(y[:], yT[:, :, ts], reads=[yT], writes=[y])
                wi = 0
                for m in range(8):
                    w = wot[m % 2]
                    S.dma(w[:], wo[m], reads=[wo], writes=[w])
                    for h0 in range(0, NTT, 512):
                        hs = slice(h0, h0 + 512)
                        ps = nextbank()
                        for k in range(8):
                            S.op('pe', lambda e, k=k: e.matmul(ps[:, :], w[:, k, :], y[:, k, hs], start=(k == 0), stop=(k == 7)),
                                 reads=[w, y], writes=[ps])
                        S.op('dve', lambda e: e.scalar_tensor_tensor(out=x[:, m, hs], in0=ps[:, :], scalar=v[:, 0, m:m + 1],
                                                                     in1=x[:, m, hs], op0=ALU.mult, op1=ALU.add),
                             reads=[ps, v, x], writes=[x])
                emit_norm_mod(S, C, x, hn2, a2, v, 3, NTT)
                for m in range(MFF):
                    w1 = wgt[m % 2]
                    w2 = wut[m % 2]
                    S.dma(w1[:], wg[m], reads=[wg], writes=[w1])
                    S.dma(w2[:], wu[m], reads=[wu], writes=[w2])
                    for h0 in range(0, NTT, 512):
                        hs = slice(h0, h0 + 512)
                        pg = nextbank()
                        pu = nextbank()
                        for k in range(8):
                            S.op('pe', lambda e, k=k: e.matmul(pg[:, :], w1[:, k, :], hn2[:, k, hs], start=(k == 0), stop=(k == 7)),
                                 reads=[w1, hn2], writes=[pg])
                        for k in range(8):
                            S.op('pe', lambda e, k=k: e.matmul(pu[:, :], w2[:, k, :], hn2[:, k, hs], start=(k == 0), stop=(k == 7)),
                                 reads=[w2, hn2], writes=[pu])
                        sg = sgt[(h0 // 512) % 2]
                        S.op('act', lambda e: e.activation(out=sg[:], in_=pg[:, :], func=AF.Silu), reads=[pg], writes=[sg])
                        S.op('dve', lambda e: e.tensor_tensor(out=A[:, m, hs], in0=pu[:, :], in1=sg[:], op=ALU.mult),
                             reads=[pu, sg], writes=[A])
                for f in range(8):
                    w = wdt[f % 2]
                    S.dma(w[:], wd[f], reads=[wd], writes=[w])
                    for h0 in range(0, NTT, 512):
                        hs = slice(h0, h0 + 512)
                        ps = nextbank()
                        for m in range(MFF):
                            S.op('pe', lambda e, m=m: e.matmul(ps[:, :], w[:, m, :], A[:, m, hs], start=(m == 0), stop=(m == MFF - 1)),
                                 reads=[w, A], writes=[ps])
                        S.op('dve', lambda e: e.scalar_tensor_tensor(out=x[:, f, hs], in0=ps[:, :], scalar=v[:, 4, f:f + 1],
                                                                     in1=x[:, f, hs], op0=ALU.mult, op1=ALU.add),
                             reads=[ps, v, x], writes=[x])
                S.dma(xo[:, :, ts], x[:], reads=[x], writes=[xo])
            if nextnorm:
                hq = hnn[0]
                emit_norm_mod(S, C, x, hq, a1n, v, 7, NTT)
                S.dma(hno[:, :, ts], hq[:], reads=[hq], writes=[hno])
        S.finish(outs)
    return nc


def pk8(vv):
    return np.ascontiguousarray(vv.reshape(8, 128).T)


def dense_weights(ws_l):
    wo = ws_l['w_out']
    wgu = ws_l['w_gu']
    wdn = ws_l['w_down']
    wo_t = np.ascontiguousarray(wo.reshape(128, 8, 8, 128).transpose(2, 0, 1, 3))
    wg_t = np.ascontiguousarray(wgu[:, :, :DFF].reshape(128, 8, MFF, 128).transpose(2, 0, 1, 3))
    wu_t = np.ascontiguousarray(wgu[:, :, DFF:].reshape(128, 8, MFF, 128).transpose(2, 0, 1, 3))
    wd_t = np.ascontiguousarray(wdn.reshape(128, MFF, 8, 128).transpose(2, 0, 1, 3))
    return {"wo": wo_t, "wg": wg_t, "wu": wu_t, "wd": wd_t}


DEBUG = False
MODQ = 6 * D // 4
MODCH = MODQ // 128
UROWS = 320
TWO = [[0, 1], [2, 3], [4, 5], [4, 5]]


CW = 4096


def plan_dense_layout():
    tiles = []
    for l in range(DEPTH):
        tiles += [(('wo', l, m), 1024) for m in range(8)]
        tiles += [((nm, l, m), 1024) for m in range(MFF) for nm in ('wg', 'wu')]
        tiles += [(('wd', l, f), MFF * 128) for f in range(8)]
    nchq = 1
    while True:
        pos = {}
        q, ch, off = 0, 0, 0
        ok = True
        for key, n in tiles:
            if off + n > CW:
                ch += 1
                off = 0
                if ch == nchq:
                    q += 1
                    ch = 0
            if q > 3:
                ok = False
                break
            pos[key] = (q, ch, off)
            off += n
        if ok:
            return nchq * CW, pos
        nchq += 1


WQ, WPOS = plan_dense_layout()


def build_fused(debug=False):
    nc = new_nc()
    with ExitStack() as es:
        S = Sched(nc, es)
        banks = [S.psum([128, 512], F32) for _ in range(8)]
        xT = S.dram_in("xT", [128, 8, NTOK], F32)
        cT = S.dram_in("cT", [128, 8, 1], F32)
        wada = S.dram_in("wada", [128, DEPTH, 8, MODQ], F32)
        bada = S.dram_in("bada", [128, DEPTH * MODCH], F32)
        wf = S.dram_in("wf", [128, WQ], F32)
        ngin = S.dram_in("ngin", [128, DEPTH, 2, 8], F32)
        yidx_d = S.dram_in("yidx", [128, NTOK // NTT, 8], mybir.dt.int32)
        fdm = S.dram_in("fox_dm", [128, 4, 512], F32)
        bdm = S.dram_in("moba_dm", [128, 4, 512], F32)
        boh = S.dram_in("moba_oh", [32, T], BF16)
        idd = S.dram_in("ident", [128, 128], F32)
        mU = S.dram_in("ml_U", [128, 128], F32)
        uin = {}
        for l in range(DEPTH):
            for u, kind in enumerate(['fox', 'fox', 'ml', 'moba', 'moba']):
                pf = "L%du%d_" % (l, u)
                d = {}
                if kind == 'fox':
                    d['wu'] = S.dram_in(pf + "wu", [128, 8, 200], F32)
                    d['g'] = S.dram_in(pf + "g", [64, 2], F32)
                    d['fb'] = S.dram_in(pf + "fb", [65, 1], F32)
                elif kind == 'moba':
                    d['wu'] = S.dram_in(pf + "wu", [128, 8, 200], F32)
                    d['g'] = S.dram_in(pf + "g", [64, 2], F32)
                    d['qc'] = S.dram_in(pf + "qc", [8, T], BF16)
                    d['kc'] = S.dram_in(pf + "kc", [8, T], BF16)
                else:
                    d['wu'] = S.dram_in(pf + "wu", [128, 8, 260], F32)
                    d['cw'] = S.dram_in(pf + "cw", [64, 2, 4], F32)
                    d['cb'] = S.dram_in(pf + "cb", [64, 2], F32)
                    d['ib'] = S.dram_in(pf + "ib", [128, 1], F32)
                    d['fb'] = S.dram_in(pf + "fb", [128, 1], F32)
                    d['mg'] = S.dram_in(pf + "mg", [128, 4, 64], F32)
                uin[(l, u)] = d
        xo = S.dram_out("xo", [128, 8, NTOK], F32)
        modl = S.dram_int("modl", [128, DEPTH * MODCH], F32)
        moda = S.dram_int("moda", [512, DEPTH * MODCH], F32)
        NWCH = WQ // CW
        wbl = S.dram_int("wbl", [NWCH * 128, CW], BF16)
        wba = S.dram_int("wba", [NWCH * 512, CW], BF16)
        hnl = S.dram_int("hnl", [8 * 128, NTOK], BF16)
        hna = S.dram_int("hna", [8 * 512, NTOK], BF16)
        yl = S.dram_int("yl", [UROWS * 4, NTOK], BF16)
        ya = S.dram_int("ya", [UROWS * 16, NTOK], BF16)
        xs = S.dram_int("xs", [128, 8, NTOK], F32)
        ylv = yl.t.rearrange("(u q) t -> u (q t)", q=4)
        hnlv = hnl.t.rearrange("(k p) t -> p k t", k=8)
        yav = ya.t.rearrange("r (a t) -> (r a) t", a=NTOK // NTT)

        vecall = S.tile([128, 4, DEPTH * MODCH], F32)
        ng = S.tile([128, DEPTH, 2, 8], F32)
        yidx = S.tile([128, NTOK // NTT, 8], mybir.dt.int32)
        S.dma(ng[:], ngin[:], reads=[ngin], writes=[ng])
        S.dma(yidx[:], yidx_d[:], reads=[yidx_d], writes=[yidx])
        ones = S.tile([128, 128], F32)
        S.op('dve', lambda e: e.memset(ones[:], 1.0), writes=[ones])
        epsc = S.tile([128, 1], F32)
        S.op('dve', lambda e: e.memset(epsc[:], EPS), writes=[epsc])
        vts = [S.tile([128, 10, 8], F32) for _ in range(DEPTH + 1)]
        a_t = [S.tile([128, 2, 8], F32) for _ in range(DEPTH + 1)]

        with ExitStack() as es2:
            S.es = es2
            c_sb = S.tile([128, 8, 1], F32)
            sg_sb = S.tile([128, 8, 1], F32)
            sc_sb = S.tile([128, 8, 1], F32)
            b_sb = S.tile([128, DEPTH * MODCH], F32)
            o_sb = S.tile([128, DEPTH * MODCH], F32)
            S.dma(c_sb[:], cT[:], reads=[cT], writes=[c_sb])
            S.dma(b_sb[:], bada[:], reads=[bada], writes=[b_sb])
            S.op('act', lambda e: e.activation(out=sg_sb[:], in_=c_sb[:], func=AF.Sigmoid), reads=[c_sb], writes=[sg_sb])
            S.op('dve', lambda e: e.tensor_tensor(out=sc_sb[:], in0=c_sb[:], in1=sg_sb[:], op=ALU.mult),
                 reads=[c_sb, sg_sb], writes=[sc_sb])
            wt = S.tile([128, 8, MODQ], F32)
            psm = banks[0]
            for l in range(DEPTH):
                S.dma(wt[:], wada[:, l, :, :], reads=[wada], writes=[wt])
                for ci in range(MODCH):
                    col = l * MODCH + ci
                    for k in range(8):
                        S.op('pe', lambda e, k=k: e.matmul(psm[:, col:col + 1], wt[:, k, ci * 128:(ci + 1) * 128],
                                                           sc_sb[:, k, 0:1], start=(k == 0), stop=(k == 7)),
                             reads=[wt, sc_sb], writes=[psm])
            S.op('dve', lambda e: e.tensor_tensor(out=o_sb[:], in0=psm[:, 0:DEPTH * MODCH], in1=b_sb[:], op=ALU.add),
                 reads=[psm, b_sb], writes=[o_sb])
            S.dma(modl[:], o_sb[:], reads=[o_sb], writes=[modl])
            S.collective(modl, moda)
            S.dma(vecall[:], moda.t.rearrange("(r p) c -> p r c", p=128), reads=[moda], writes=[vecall])
            fts = [S.tile([128, WC_TILE], F32) for _ in range(3)]
            bts = [S.tile([128, WC_TILE], BF16) for _ in range(3)]
            for i in range(WQ // WC_TILE):
                ft, bt = fts[i % 3], bts[i % 3]
                sl = slice(i * WC_TILE, (i + 1) * WC_TILE)
                S.dma(ft[:], wf[:, sl], reads=[wf], writes=[ft])
                eng = 'dve' if i % 2 == 0 else 'pool'
                S.op(eng, lambda e: e.tensor_copy(out=bt[:], in_=ft[:]), reads=[ft], writes=[bt])
                ch, co = (i * WC_TILE) // CW, (i * WC_TILE) % CW
                S.dma(wbl[ch * 128:(ch + 1) * 128, co:co + WC_TILE], bt[:], reads=[bt], writes=[wbl])
            S.collective_chunks(wbl, wba, NWCH, 128)
            S.barrier()
        S.es = es

        def vcol(l, j, k):
            ci = j * 8 + k
            return vecall[:, ci // MODCH, l * MODCH + ci % MODCH:l * MODCH + ci % MODCH + 1]

        def fill_vec(vt, at, lpost, lnext):
            S.op('dve', lambda e: e.memset(vt[:], 0.0), writes=[vt])
            items = []
            if lpost is not None:
                items += [(0, lpost, 2), (2, lpost, 4), (3, lpost, 3), (4, lpost, 5)]
                S.op('dve', lambda e: e.tensor_copy(out=vt[:, 1, :], in_=ng[:, lpost, 1, :]), reads=[ng], writes=[vt])
            if lnext is not None:
                items += [(6, lnext, 1), (7, lnext, 0)]
                S.op('dve', lambda e: e.tensor_copy(out=vt[:, 5, :], in_=ng[:, lnext, 0, :]), reads=[ng], writes=[vt])
            for row, l, j in items:
                for k in range(8):
                    S.op('dve', lambda e, k=k: e.tensor_copy(out=vt[:, row, k:k + 1], in_=vcol(l, j, k)),
                         reads=[vecall], writes=[vt])
            for ai, (srow, grow) in enumerate(((2, 1), (6, 5))):
                S.op('dve', lambda e: e.tensor_scalar(out=at[:, ai, :], in0=vt[:, srow, :], scalar1=1.0, scalar2=None,
                                                      op0=ALU.add), reads=[vt], writes=[at])
                S.op('dve', lambda e: e.tensor_tensor(out=at[:, ai, :], in0=at[:, ai, :], in1=vt[:, grow, :],
                                                      op=ALU.mult), reads=[at, vt], writes=[at])

        fill_vec(vts[0], a_t[0], None, 0)
        for l in range(DEPTH):
            fill_vec(vts[l + 1], a_t[l + 1], l, l + 1 if l + 1 < DEPTH else None)

        def dense_phase(l, post, nextnorm, xsrc, xdst):
            v = vts[0] if not post else vts[l + 1]
            at = a_t[0] if not post else a_t[l + 1]
            with ExitStack() as es2:
                S.es = es2
                C = MixCtx()
                C.bi = 0

                def nextbank():
                    C.bi += 1
                    return banks[C.bi % 8]
                C.nextbank = nextbank
                C.ones, C.epsc = ones, epsc
                C.sq = [S.tile([128, 512], F32) for _ in range(2)]
                C.t = [S.tile([128, 512], F32) for _ in range(2)]
                C.rs = S.tile([128, 512], F32)
                a2 = View(at, at.t[:, 0, :])
                a1n = View(at, at.t[:, 1, :])
                if post:
                    yt = S.tile([128, 8, NTT], BF16)
                    hn2 = S.tile([128, 8, NTT], BF16)
                    A = S.tile([128, MFF, NTT], BF16)
                    wot = [S.tile([128, 8, 128], BF16) for _ in range(2)]
                    wgt = [S.tile([128, 8, 128], BF16) for _ in range(2)]
                    wut = [S.tile([128, 8, 128], BF16) for _ in range(2)]
                    wdt = [S.tile([128, MFF, 128], BF16) for _ in range(2)]
                    sgt = [S.tile([128, 512], F32) for _ in range(2)]

                    def wsrc(key, kk):
                        q, ch, off = WPOS[key]
                        r0 = ch * 512 + q * 128
                        return wba.t[r0:r0 + 128, off:off + kk * 128].rearrange("p (k j) -> p k j", k=kk)
                if nextnorm:
                    hq = S.tile([128, 8, NTT], BF16)
                xt = [S.tile([128, 8, NTT], F32) for _ in range(2)]
                for ti in range(NTOK // NTT):
                    ts = slice(ti * NTT, (ti + 1) * NTT)
                    x = xt[ti % 2]
                    S.dma(x[:], xsrc[:, :, ts], reads=[xsrc], writes=[x])
                    if post:
                        y = yt
                        for k in range(8):
                            S.idma(y[:, k, :], yav, yidx[:, ti, k:k + 1], reads=[ya, yidx], writes=[y])
                        for m in range(8):
                            w = wot[m % 2]
                            S.dma(w[:], wsrc(('wo', l, m), 8), reads=[wba], writes=[w])
                            for h0 in range(0, NTT, 512):
                                hs = slice(h0, h0 + 512)
                                ps = nextbank()
                                for k in range(8):
                                    S.op('pe', lambda e, k=k: e.matmul(ps[:, :], w[:, k, :], y[:, k, hs], start=(k == 0),
                                                                       stop=(k == 7)), reads=[w, y], writes=[ps])
                                S.op('dve', lambda e: e.scalar_tensor_tensor(out=x[:, m, hs], in0=ps[:, :],
                                                                             scalar=v[:, 0, m:m + 1], in1=x[:, m, hs],
                                                                             op0=ALU.mult, op1=ALU.add),
                                     reads=[ps, v, x], writes=[x])
                        emit_norm_mod(S, C, x, hn2, a2, v, 3, NTT)
                        for m in range(MFF):
                            w1, w2 = wgt[m % 2], wut[m % 2]
                            S.dma(w1[:], wsrc(('wg', l, m), 8), reads=[wba], writes=[w1])
                            S.dma(w2[:], wsrc(('wu', l, m), 8), reads=[wba], writes=[w2])
                            for h0 in range(0, NTT, 512):
                                hs = slice(h0, h0 + 512)
                                pg = nextbank()
                                pu = nextbank()
                                for k in range(8):
                                    S.op('pe', lambda e, k=k: e.matmul(pg[:, :], w1[:, k, :], hn2[:, k, hs], start=(k == 0),
                                                                       stop=(k == 7)), reads=[w1, hn2], writes=[pg])
                                for k in range(8):
                                    S.op('pe', lambda e, k=k: e.matmul(pu[:, :], w2[:, k, :], hn2[:, k, hs], start=(k == 0),
                                                                       stop=(k == 7)), reads=[w2, hn2], writes=[pu])
                                sg = sgt[(h0 // 512) % 2]
                                S.op('act', lambda e: e.activation(out=sg[:], in_=pg[:, :], func=AF.Silu), reads=[pg], writes=[sg])
                                S.op('dve', lambda e: e.tensor_tensor(out=A[:, m, hs], in0=pu[:, :], in1=sg[:], op=ALU.mult),
                                     reads=[pu, sg], writes=[A])
                        for f in range(8):
                            w = wdt[f % 2]
                            S.dma(w[:], wsrc(('wd', l, f), MFF), reads=[wba], writes=[w])
                            for h0 in range(0, NTT, 512):
                                hs = slice(h0, h0 + 512)
                                ps = nextbank()
                                for m in range(MFF):
                                    S.op('pe', lambda e, m=m: e.matmul(ps[:, :], w[:, m, :], A[:, m, hs], start=(m == 0),
                                                                       stop=(m == MFF - 1)), reads=[w, A], writes=[ps])
                                S.op('dve', lambda e: e.scalar_tensor_tensor(out=x[:, f, hs], in0=ps[:, :],
                                                                             scalar=v[:, 4, f:f + 1], in1=x[:, f, hs],
                                                                             op0=ALU.mult, op1=ALU.add),
                                     reads=[ps, v, x], writes=[x])
                        S.dma(xdst[:, :, ts], x[:], reads=[x], writes=[xdst])
                    if nextnorm:
                        emit_norm_mod(S, C, x, hq, a1n, v, 7, NTT)
                        S.dma(hnlv[:, :, ts], hq[:], reads=[hq], writes=[hnl])
                if nextnorm:
                    S.collective_chunks(hnl, hna, 8, 128)
                S.barrier()
            S.es = es

        def mix_phase(l, kinds, slots):
            with ExitStack() as es2:
                S.es = es2
                C = mix_common(S, banks=banks, fused=True)
                if 'fox' in kinds or 'moba' in kinds:
                    attn_alloc(S, C, 'moba' in kinds)
                if 'fox' in kinds:
                    fox_alloc(S, C)
                if 'ml' in kinds:
                    ml_alloc(S, C)
                if 'moba' in kinds:
                    moba_alloc(S, C)
                for kind, u in zip(kinds, slots):
                    d = uin[(l, u)]
                    yout = View(yl, ylv[64 * u:64 * u + 64, :])
                    if kind == 'fox':
                        emit_fox_unit(S, C, hna, d['wu'], d['g'], d['fb'], fdm, yout)
                    elif kind == 'moba':
                        emit_moba_unit(S, C, hna, d['wu'], d['g'], d['qc'], d['kc'], boh, bdm, idd, yout)
                    else:
                        emit_ml_unit(S, C, hna, d['wu'], d['cw'], d['cb'], d['ib'], d['fb'], d['mg'], mU, idd, yout)
                S.barrier()
            S.es = es

        dense_phase(0, False, True, xT, None)
        for l in range(DEPTH):
            mix_phase(l, ['fox', 'fox', 'ml'], [0, 1, 2])
            mix_phase(l, ['moba', 'moba'], [3, 4])
            S.collective_chunks(yl, ya, UROWS * 4 // 128, 128)
            if debug:
                dy = S.dram_out("dbg_y", [UROWS * 4, NTOK], BF16)
                dh = S.dram_out("dbg_hn", [8 * 128, NTOK], BF16)
                dya = S.dram_out("dbg_ya", [UROWS * 16, NTOK], BF16)
                dv = S.dram_out("dbg_v", [128, 10, 8], F32)
                S.dma(dy[:], yl[:], reads=[yl], writes=[dy])
                S.dma(dh[:], hnl[:], reads=[hnl], writes=[dh])
                S.dma(dya[:], ya[:], reads=[ya], writes=[dya])
                S.dma(dv[:], vts[1][:], reads=[vts[1]], writes=[dv])
                S.finish([dy, dh, dya, dv])
                return nc
            last = l == DEPTH - 1
            dense_phase(l, True, not last, xT if l == 0 else xs, xo if last else xs)
        S.finish([xo])
    return nc


def dense_tiles_f32(inp, l):
    wo = w_to_pk(inp['w_out'][l]).reshape(128, 8, D)
    wgu = w_to_pk(inp['w_gate_up'][l]).reshape(128, 8, 2 * DFF)
    wdn = w_to_pk(inp['w_down'][l]).reshape(128, MFF, D)
    t = {}
    for m in range(8):
        t[('wo', l, m)] = wo[:, :, m * 128:(m + 1) * 128].reshape(128, -1)
        t[('wd', l, m)] = wdn[:, :, m * 128:(m + 1) * 128].reshape(128, -1)
    for m in range(MFF):
        t[('wg', l, m)] = wgu[:, :, m * 128:(m + 1) * 128].reshape(128, -1)
        t[('wu', l, m)] = wgu[:, :, DFF + m * 128:DFF + (m + 1) * 128].reshape(128, -1)
    return t


def kernel(**inp):
    inp = {k: np.asarray(v) for k, v in inp.items()}
    x = inp['x']
    wq = [np.zeros((128, WQ), np.float32) for _ in range(4)]
    for l in range(DEPTH):
        for key, arr in dense_tiles_f32(inp, l).items():
            q, ch, off = WPOS[key]
            wq[q][:, ch * CW + off:ch * CW + off + arr.shape[1]] = arr
    w_in_pk = [w_to_pk(inp['w_in'][l]).reshape(128, 8, IN_COLS) for l in range(DEPTH)]
    consts = {"fox_dm": fox_dmask(), "moba_dm": moba_dmask(), "moba_oh": moba_onehot(),
              "ident": np.eye(128, dtype=np.float32), "ml_U": ml_consts()}
    ngin = np.ascontiguousarray(np.stack([np.stack([pk8(inp['norm1_g'][l]), pk8(inp['norm2_g'][l])], axis=1)
                                          for l in range(DEPTH)], axis=1)).astype(np.float32)
    in_maps = []
    for c in range(NCORE):
        b, r = c // 4, c % 4
        m = dict(consts)
        m["xT"] = to_fm(x[b, r * NTOK:(r + 1) * NTOK, :])
        m["cT"] = np.ascontiguousarray(inp['c'][b].reshape(8, 128).T)[:, :, None].astype(np.float32)
        wsl = inp['w_ada'][:, :, r * MODQ:(r + 1) * MODQ]
        m["wada"] = np.ascontiguousarray(wsl.reshape(DEPTH, 8, 128, MODQ).transpose(2, 0, 1, 3))
        bsl = inp['b_ada'][:, r * MODQ:(r + 1) * MODQ].reshape(DEPTH, MODCH, 128)
        m["bada"] = np.ascontiguousarray(bsl.transpose(2, 0, 1)).reshape(128, DEPTH * MODCH).astype(np.float32)
        m["wf"] = wq[r]
        m["ngin"] = ngin
        idx = np.zeros((D,), np.int32)
        for f in range(D):
            if f < 384:
                h, dd = f // 64, f % 64
                rk, ur = min(h // 2, 2), 64 * (h % 2) + dd
            elif f < 640:
                h, dd = (f - 384) // 64, f % 64
                rk, ur = h, 128 + dd
            else:
                h, dd = (f - 640) // 64, f % 64
                rk, ur = min(h // 2, 2), 192 + 64 * (h % 2) + dd
            lr = ur * 4 + r
            idx[f] = (lr // 128) * 512 + rk * 128 + lr % 128
        nti = NTOK // NTT
        idx2 = idx.reshape(8, 128).T
        m["yidx"] = np.ascontiguousarray(np.stack([idx2 * nti + ti for ti in range(nti)], axis=1)).astype(np.int32)
        for l in range(DEPTH):
            ws_l = {'w_in': w_in_pk[l]}
            for u, kind in enumerate(['fox', 'fox', 'ml', 'moba', 'moba']):
                pf = "L%du%d_" % (l, u)
                if kind == 'fox':
                    dd_ = fox_unit_inputs(0, ws_l, inp, l, TWO[r][u])
                    m[pf + "wu"], m[pf + "g"], m[pf + "fb"] = dd_["wu0"], dd_["g0"], dd_["fb0"]
                elif kind == 'moba':
                    dd_ = moba_unit_inputs(0, ws_l, inp, l, TWO[r][u - 3])
                    m[pf + "wu"], m[pf + "g"], m[pf + "qc"], m[pf + "kc"] = dd_["wu0"], dd_["g0"], dd_["qc0"], dd_["kc0"]
                else:
                    dd_ = ml_unit_inputs(0, ws_l, inp, l, r)
                    for nm in ("wu", "cw", "cb", "ib", "fb", "mg"):
                        m[pf + nm] = dd_[nm + "0"]
        in_maps.append(m)
    if DEBUG:
        return run_spmd(build_fused(True), in_maps)
    res = run_spmd(build_fused(), in_maps)
    out = np.zeros((B, T, D), np.float32)
    for c in range(NCORE):
        b, q = c // 4, c % 4
        out[b, q * NTOK:(q + 1) * NTOK, :] = res[c]["xo"].transpose(2, 1, 0).reshape(NTOK, D)
    return out
```

```python
import numpy as np
from contextlib import ExitStack
import ml_dtypes
import concourse.bass as bass
import concourse.mybir as mybir
from concourse.bass_utils import run_bass_kernel_spmd

F32 = mybir.dt.float32
BF16 = mybir.dt.bfloat16
AF = mybir.ActivationFunctionType
ALU = mybir.AluOpType
AX = mybir.AxisListType
NPBF = ml_dtypes.bfloat16

D = 1024
B = 2
T = 16384
DEPTH = 2
HD = 64
NCORE = 8
DFF = 2816
FOXH, MLH, MOBH = 6, 4, 6
EPS = 1e-6
IN_COLS = 3342
O_FQ, O_FK, O_FV, O_FF = 0, 384, 768, 1152
O_MQ, O_MK, O_MV, O_MI, O_MF, O_MO = 1158, 1414, 1670, 1926, 1930, 1934
O_BQ, O_BK, O_BV = 2190, 2574, 2958
NEG = -30000.0


class Buf:
    def __init__(self, t=None):
        self.t = t
        self.w = None
        self.r = {}

    def __getitem__(self, idx):
        return self.t[idx]


class View:
    def __init__(self, parent, t):
        self.p = parent
        self.t = t

    def __getitem__(self, idx):
        return self.t[idx]

    @property
    def w(self):
        return self.p.w

    @w.setter
    def w(self, v):
        self.p.w = v

    @property
    def r(self):
        return self.p.r

    @r.setter
    def r(self, v):
        self.p.r = v


GROUPS = [[0, 1, 2, 3], [4, 5, 6, 7]]


class Sched:
    NDMA = 32

    def __init__(self, nc, es):
        self.nc = nc
        self.es = es
        self.eng = {'pe': nc.tensor, 'act': nc.scalar, 'dve': nc.vector, 'pool': nc.gpsimd,
                    'sp': nc.sync}
        self.esem = {k: es.enter_context(nc.semaphore('s_' + k)) for k in ['pe', 'act', 'dve', 'pool', 'cc']}
        self.ecnt = {k: 0 for k in self.esem}
        self.dsem = [es.enter_context(nc.semaphore('d%d' % i)) for i in range(self.NDMA)]
        self.dcnt = [0] * self.NDMA
        self.dnext = 0
        self.known = {k: {} for k in self.eng}
        self.out_tags = {}
        self.ntile = 0

    def tile(self, shape, dtype, name=None):
        self.ntile += 1
        name = name or ('t%d' % self.ntile)
        return Buf(self.es.enter_context(self.nc.sbuf_tensor(name, list(shape), dtype)))

    def psum(self, shape, dtype, name=None):
        self.ntile += 1
        name = name or ('p%d' % self.ntile)
        return Buf(self.es.enter_context(self.nc.psum_tensor(name, list(shape), dtype)))

    def dram_in(self, name, shape, dtype):
        return Buf(self.nc.dram_tensor(name, list(shape), dtype, kind="ExternalInput").ap())

    def dram_out(self, name, shape, dtype):
        return Buf(self.nc.dram_tensor(name, list(shape), dtype, kind="ExternalOutput").ap())

    def dram_int(self, name, shape, dtype):
        return Buf(self.nc.dram_tensor(name, list(shape), dtype).ap())

    def barrier(self):
        deps = {k: v for k, v in self.ecnt.items() if v > 0}
        for i in range(self.NDMA):
            if self.dcnt[i] > 0:
                deps[i] = self.dcnt[i]
        for e in self.eng:
            self._wait(e, dict(deps))

    def collective(self, src, dst, src_ap=None, dst_ap=None):
        self._wait('pool', self._deps('pool', [src], [dst]))
        self.ecnt['cc'] += 1
        sa = src.t if src_ap is None else src_ap
        da = dst.t if dst_ap is None else dst_ap
        self.eng['pool'].collective_compute("AllGather", ALU.bypass, replica_groups=GROUPS,
                                            ins=[sa.opt()], outs=[da.opt()]).then_inc(self.esem['cc'], 1)
        self._mark(('cc', self.ecnt['cc']), [src], [dst])

    def collective_chunks(self, src, dst, nch, rows):
        for ch in range(nch):
            self.collective(src, dst, src.t[ch * rows:(ch + 1) * rows, :], dst.t[ch * 4 * rows:(ch + 1) * 4 * rows, :])

    def idma(self, out_ap, in_ap, idx_ap, reads=(), writes=()):
        deps = self._deps('pool', reads, writes)
        i = self.dnext
        self.dnext = (i + 1) % self.NDMA
        if self.dcnt[i] > 0 and deps.get(i, 0) < self.dcnt[i]:
            deps[i] = self.dcnt[i]
        self._wait('pool', deps)
        self.dcnt[i] += 16
        self.eng['pool'].indirect_dma_start(out=out_ap, out_offset=None, in_=in_ap,
                                            in_offset=bass.IndirectOffsetOnAxis(ap=idx_ap, axis=0)
                                            ).then_inc(self.dsem[i], 16)
        self._mark((i, self.dcnt[i]), reads, writes)

    def _deps(self, e, reads, writes):
        deps = {}

        def add(tag):
            if tag is None:
                return
            k, v = tag
            if deps.get(k, 0) < v:
                deps[k] = v
        for b in reads:
            add(b.w)
        for b in writes:
            add(b.w)
            for k, v in b.r.items():
                add((k, v))
        if e == 'pe':
            deps.pop('pe', None)
        return deps

    def _wait(self, e, deps):
        kn = self.known[e]
        for k, v in deps.items():
            if kn.get(k, 0) < v:
                sem = self.esem[k] if isinstance(k, str) else self.dsem[k]
                self.eng[e].wait_ge(sem, v)
                kn[k] = v

    def _mark(self, tag, reads, writes):
        k, v = tag
        for b in writes:
            b.w = tag
            b.r = {}
        for b in reads:
            if b not in writes:
                if b.r.get(k, 0) < v:
                    b.r[k] = v

    def op(self, e, fn, reads=(), writes=()):
        self._wait(e, self._deps(e, reads, writes))
        ins = fn(self.eng[e])
        self.ecnt[e] += 1
        ins.then_inc(self.esem[e], 1)
        self._mark((e, self.ecnt[e]), reads, writes)

    def dma(self, out_ap, in_ap, reads=(), writes=(), q='sp'):
        deps = self._deps(q, reads, writes)
        i = self.dnext
        self.dnext = (i + 1) % self.NDMA
        if self.dcnt[i] > 0 and deps.get(i, 0) < self.dcnt[i]:
            deps[i] = self.dcnt[i]
        self._wait(q, deps)
        self.dcnt[i] += 16
        self.eng[q].dma_start(out=out_ap, in_=in_ap).then_inc(self.dsem[i], 16)
        tag = (i, self.dcnt[i])
        self._mark(tag, reads, writes)
        return tag

    def finish(self, out_bufs):
        deps = {}
        for b in out_bufs:
            if b.w is not None:
                k, v = b.w
                deps[k] = max(deps.get(k, 0), v)
        for i in range(self.NDMA):
            if self.dcnt[i] > 0:
                deps[i] = self.dcnt[i]
        self.known['sp'] = {}
        self._wait('sp', deps)


def new_nc():
    return bass.Bass("TRN2", target_bir_lowering=False)


def run_spmd(nc, in_maps):
    res = run_bass_kernel_spmd(nc, in_maps, core_ids=list(range(NCORE)))
    return res.results


MODC = 6 * D // NCORE
WCOLS_L = 8 * IN_COLS + 8 * D + 8 * 2 * DFF + 22 * D
WCOLS = DEPTH * WCOLS_L
WC_CORE = (WCOLS + NCORE - 1) // NCORE
WC_TILE = 2048
WC_CORE = ((WC_CORE + WC_TILE - 1) // WC_TILE) * WC_TILE


def build_prep():
    nc = new_nc()
    with ExitStack() as es:
        S = Sched(nc, es)
        cT = S.dram_in("cT", [128, 8, B], F32)
        wada = S.dram_in("wada", [128, DEPTH, 8, MODC], F32)
        bada = S.dram_in("bada", [B, DEPTH, MODC], F32)
        wf = S.dram_in("wf", [128, WC_CORE], F32)
        mod = S.dram_out("mod", [B, DEPTH, MODC], F32)
        wb = S.dram_out("wb", [128, WC_CORE], BF16)

        c_sb = S.tile([128, 8, B], F32)
        sc_sb = S.tile([128, 8, B], F32)
        sg_sb = S.tile([128, 8, B], F32)
        b_sb = S.tile([B, DEPTH, MODC], F32)
        o_sb = S.tile([B, DEPTH, MODC], F32)
        S.dma(c_sb[:], cT[:], reads=[cT], writes=[c_sb])
        S.dma(b_sb[:], bada[:], reads=[bada], writes=[b_sb])
        S.op('act', lambda e: e.activation(out=sg_sb[:], in_=c_sb[:], func=AF.Sigmoid),
             reads=[c_sb], writes=[sg_sb])
        S.op('dve', lambda e: e.tensor_tensor(out=sc_sb[:], in0=c_sb[:], in1=sg_sb[:], op=ALU.mult),
             reads=[c_sb, sg_sb], writes=[sc_sb])
        wts = [S.tile([128, 8, MODC], F32) for _ in range(2)]
        pss = [S.psum([B, 512], F32) for _ in range(2)]
        pi = 0
        for l in range(DEPTH):
            wt = wts[l % 2]
            S.dma(wt[:], wada[:, l, :, :], reads=[wada], writes=[wt])
            for (c0, cn) in ((0, 512), (512, MODC - 512)):
                ps = pss[pi % 2]
                pi += 1
                for k in range(8):
                    S.op('pe', lambda e, k=k, ps=ps, wt=wt, c0=c0, cn=cn: e.matmul(
                        ps[:, 0:cn], sc_sb[:, k, :], wt[:, k, c0:c0 + cn], start=(k == 0), stop=(k == 7)),
                        reads=[sc_sb, wt], writes=[ps])
                S.op('dve', lambda e, ps=ps, l=l, c0=c0, cn=cn: e.tensor_tensor(
                    out=o_sb[:, l, c0:c0 + cn], in0=ps[:, 0:cn], in1=b_sb[:, l, c0:c0 + cn], op=ALU.add),
                    reads=[ps, b_sb], writes=[o_sb])
        S.dma(mod[:], o_sb[:], reads=[o_sb], writes=[mod])
        fts = [S.tile([128, WC_TILE], F32) for _ in range(3)]
        bts = [S.tile([128, WC_TILE], BF16) for _ in range(3)]
        for i in range(WC_CORE // WC_TILE):
            ft = fts[i % 3]
            bt = bts[i % 3]
            sl = slice(i * WC_TILE, (i + 1) * WC_TILE)
            S.dma(ft[:], wf[:, sl], reads=[wf], writes=[ft])
            eng = 'dve' if i % 2 == 0 else 'pool'
            S.op(eng, lambda e, ft=ft, bt=bt: e.tensor_copy(out=bt[:], in_=ft[:]), reads=[ft], writes=[bt])
            S.dma(wb[:, sl], bt[:], reads=[bt], writes=[wb])
        S.finish([mod, wb])
    return nc


def w_to_pk(w):
    K, N = w.shape
    return np.ascontiguousarray(w.reshape(K // 128, 128, N).transpose(1, 0, 2)).reshape(128, -1)


def run_prep(c, w_ada, b_ada, w_in, w_out, w_gate_up, w_down):
    cT = np.ascontiguousarray(c.T.reshape(8, 128, B).transpose(1, 0, 2))
    flat = []
    for l in range(DEPTH):
        flat += [w_to_pk(w_in[l]), w_to_pk(w_out[l]), w_to_pk(w_gate_up[l]), w_to_pk(w_down[l])]
    flat = np.concatenate(flat, axis=1)
    pad = NCORE * WC_CORE - flat.shape[1]
    flat = np.concatenate([flat, np.zeros((128, pad), np.float32)], axis=1)
    in_maps = []
    for i in range(NCORE):
        wsl = w_ada[:, :, i * MODC:(i + 1) * MODC]
        wsl = np.ascontiguousarray(wsl.reshape(DEPTH, 8, 128, MODC).transpose(2, 0, 1, 3))
        bsl = np.ascontiguousarray(np.broadcast_to(b_ada[None, :, i * MODC:(i + 1) * MODC], (B, DEPTH, MODC)))
        in_maps.append({"cT": cT, "wada": wsl, "bada": bsl,
                        "wf": np.ascontiguousarray(flat[:, i * WC_CORE:(i + 1) * WC_CORE])})
    res = run_spmd(build_prep(), in_maps)
    mod = np.concatenate([r["mod"] for r in res], axis=2)
    mod = np.ascontiguousarray(mod.transpose(1, 0, 2))
    wbf = np.concatenate([r["wb"] for r in res], axis=1)[:, :WCOLS]
    ws = []
    off = 0
    for l in range(DEPTH):
        d = {}
        for name, kk, n in (("w_in", 8, IN_COLS), ("w_out", 8, D), ("w_gu", 8, 2 * DFF), ("w_down", 22, D)):
            d[name] = wbf[:, off:off + kk * n].reshape(128, kk, n)
            off += kk * n
        ws.append(d)
    return mod, ws


NQG = T // 512


class MixCtx:
    pass


def load_hn_plain(S, hn, hnT, tt):
    S.dma(hn[:], hnT[:, :, tt * 512:(tt + 1) * 512], reads=[hnT], writes=[hn])


def load_hn_gathered(S, hn, hna, tt):
    q = tt // 8
    src = hna.t.rearrange("(k r p) t -> r p k t", k=8, r=4)[q]
    S.dma(hn[:], src[:, :, (tt % 8) * 512:(tt % 8 + 1) * 512], reads=[hna], writes=[hn])


def load_w(S, C, wu, wu_d):
    if C.w32 is None:
        S.dma(wu[:], wu_d[:], reads=[wu_d], writes=[wu])
    else:
        n = wu_d.t.shape[2]
        S.dma(C.w32[:, :, 0:n], wu_d[:], reads=[wu_d], writes=[C.w32])
        S.op('dve', lambda e: e.tensor_copy(out=wu[:], in_=C.w32[:, :, 0:n]), reads=[C.w32], writes=[wu])


def mix_common(S, banks=None, fused=False):
    C = MixCtx()
    C.load_hn = load_hn_gathered if fused else load_hn_plain
    C.w32 = S.tile([128, 8, 260], F32) if fused else None
    C.banks = banks if banks is not None else [S.psum([128, 512], F32) for _ in range(8)]
    C.ones64 = S.tile([128, 128], F32)
    S.op('dve', lambda e: e.memset(C.ones64[:], 1.0), writes=[C.ones64])
    C.epsc = S.tile([128, 1], F32)
    S.op('dve', lambda e: e.memset(C.epsc[:], EPS), writes=[C.epsc])
    C.lnsc = S.tile([128, 1], F32)
    S.op('dve', lambda e: e.memset(C.lnsc[:], float(np.log(HD ** -0.5))), writes=[C.lnsc])
    C.sel = S.tile([65, 64], F32)
    S.op('dve', lambda e: e.memset(C.sel[:], 0.0), writes=[C.sel])
    S.op('dve', lambda e: e.memset(C.sel[64:65, :], 1.0), writes=[C.sel])
    C.hn = [S.tile([128, 8, 512], BF16) for _ in range(2)]
    C.sq = [S.tile([128, 512], F32) for _ in range(2)]
    C.rs = [S.tile([128, 512], F32) for _ in range(2)]
    return C


def attn_alloc(S, C, with_pageB):
    C.Qa = S.tile([128, T], BF16)
    C.QaB = S.tile([128, T // 2], BF16) if with_pageB else None
    C.Ka = S.tile([128, T], BF16)
    NT_ = T // 512
    C.QF = [Buf() for _ in range(NT_)]
    C.QR = [Buf() for _ in range(NT_)]
    C.KF = [Buf() for _ in range(NT_)]
    C.KR = [Buf() for _ in range(NT_)]
    C.QBF = [Buf() for _ in range(NT_)]
    C.QBR = [Buf() for _ in range(NT_)]
    C.Va = S.tile([128, T // 128, 65], BF16)
    S.op('pool', lambda e: e.memset(C.Va[:], 1.0), writes=[C.Va])
    C.P = [S.tile([128, 512], BF16) for _ in range(5)]
    C.M = [S.tile([128, 512], F32) for _ in range(2)]
    C.dm = S.tile([128, 4, 512], F32)
    C.osb = [S.tile([65, 512], F32) for _ in range(2)]
    C.rd = [S.tile([64, 512], F32) for _ in range(2)]
    C.ysb = [S.tile([64, 512], BF16) for _ in range(2)]


def emit_qknorm(S, C, ps, lo, gcol, dests, idx):
    hi = lo + 64
    sq = C.sq[idx % 2]
    rs = C.rs[idx % 2]
    bSS = C.banks[3 + 4 * (idx % 2)]
    S.op('act', lambda e: e.activation(out=sq[lo:hi, :], in_=ps[lo:hi, :], func=AF.Square),
         reads=[ps], writes=[sq])
    S.op('pe', lambda e: e.matmul(bSS[lo:hi, :], C.ones64[lo:hi, lo:hi], sq[lo:hi, :], start=True, stop=True),
         reads=[C.ones64, sq], writes=[bSS])
    S.op('act', lambda e: e.activation(out=sq[lo:hi, :], in_=bSS[lo:hi, :], func=AF.Ln, bias=C.epsc[lo:hi, 0:1],
                                       scale=1.0 / HD), reads=[bSS, C.epsc], writes=[sq])
    S.op('act', lambda e: e.activation(out=rs[lo:hi, :], in_=sq[lo:hi, :], func=AF.Exp, scale=-0.5),
         reads=[sq], writes=[rs])
    for ap, bufs in dests:
        S.op('dve', lambda e, ap=ap: e.scalar_tensor_tensor(out=ap, in0=ps[lo:hi, :], scalar=gcol, in1=rs[lo:hi, :],
                                                            op0=ALU.mult, op1=ALU.mult),
             reads=[ps, rs], writes=bufs)


def emit_attention(S, C, yout, prow, drow, dsplit=False):
    Qa, QaB, Ka, Va, dm = C.Qa, C.QaB, C.Ka, C.Va, C.dm
    banks = C.banks
    for g in range(NQG):
        n = 4 * g + 4
        bO = banks[4 + g % 2]
        q0 = g * 512

        def emitS(kt):
            lo, hi = drow if kt >= 4 * g else prow
            if kt < 64 or QaB is None:
                Qp, qc = Qa, q0
            else:
                Qp, qc = QaB, q0 - T // 2
            bS = banks[kt % 4]
            kq = kt // 4
            if Qp is Qa:
                rd = [C.KF[kq], C.KR[kq], C.QF[g], C.QR[g]]
            else:
                rd = [C.KF[kq], C.KR[kq], C.QBF[g], C.QBR[g]]
            if dsplit and 4 * g <= kt < 4 * g + 2:
                S.op('pe', lambda e: e.matmul(bS[:, 0:256], Ka[lo:hi, kt * 128:(kt + 1) * 128], Qp[lo:hi, qc:qc + 256],
                                              start=True, stop=True), reads=rd, writes=[bS])
                plo, phi = prow
                S.op('pe', lambda e: e.matmul(bS[:, 256:512], Ka[plo:phi, kt * 128:(kt + 1) * 128],
                                              Qp[plo:phi, qc + 256:qc + 512], start=True, stop=True),
                     reads=rd, writes=[bS])
            else:
                S.op('pe', lambda e: e.matmul(bS[:, :], Ka[lo:hi, kt * 128:(kt + 1) * 128], Qp[lo:hi, qc:qc + 512],
                                              start=True, stop=True), reads=rd, writes=[bS])
            P = C.P[kt % 5]
            if kt >= 4 * g:
                M = C.M[kt % 2]
                S.op('dve', lambda e: e.tensor_tensor(out=M[:], in0=bS[:], in1=dm[:, kt - 4 * g, :], op=ALU.add),
                     reads=[bS, dm], writes=[M])
                S.op('act', lambda e: e.activation(out=P[:], in_=M[:], func=AF.Exp), reads=[M], writes=[P])
            else:
                S.op('act', lambda e: e.activation(out=P[:], in_=bS[:], func=AF.Exp), reads=[bS], writes=[P])

        def emitO(kt):
            P = C.P[kt % 5]
            S.op('pe', lambda e: e.matmul(bO[0:65, :], Va[:, kt, :], P[:], start=(kt == 0), stop=(kt == n - 1)),
                 reads=[Va, P], writes=[bO])
        LAG = 3
        for i in range(n + LAG):
            if i < n:
                emitS(i)
            if i >= LAG:
                emitO(i - LAG)
        osb = C.osb[g % 2]
        rd = C.rd[g % 2]
        ysb = C.ysb[g % 2]
        bD = banks[6]
        S.op('act', lambda e: e.activation(out=osb[:], in_=bO[0:65, :], func=AF.Copy), reads=[bO], writes=[osb])
        S.op('pe', lambda e: e.matmul(bD[0:64, :], C.sel[:, :], osb[:], start=True, stop=True),
             reads=[C.sel, osb], writes=[bD])
        S.op('dve', lambda e: e.reciprocal(out=rd[:], in_=bD[0:64, :]), reads=[bD], writes=[rd])
        S.op('dve', lambda e: e.tensor_tensor(out=ysb[:], in0=osb[0:64, :], in1=rd[:], op=ALU.mult),
             reads=[osb, rd], writes=[ysb])
        S.dma(yout[0:64, q0:q0 + 512], ysb[:], reads=[ysb], writes=[yout])


def emit_proj_mm(S, C, hnT, wu, tt, qcols, kcols, vcols):
    hn = C.hn[tt % 2]
    if tt == 0:
        C.load_hn(S, hn, hnT, 0)
    if tt + 1 < T // 512:
        C.load_hn(S, C.hn[(tt + 1) % 2], hnT, tt + 1)
    st = 4 * (tt % 2)
    bQ, bK, bV = C.banks[st], C.banks[st + 1], C.banks[st + 2]
    nq = qcols[1] - qcols[0]
    nk = kcols[1] - kcols[0]
    nv = vcols[1] - vcols[0]
    for k in range(8):
        S.op('pe', lambda e, k=k: e.matmul(bQ[0:nq, :], wu[:, k, qcols[0]:qcols[1]], hn[:, k, :],
                                           start=(k == 0), stop=(k == 7)), reads=[wu, hn], writes=[bQ])
    for k in range(8):
        S.op('pe', lambda e, k=k: e.matmul(bK[0:nk, :], wu[:, k, kcols[0]:kcols[1]], hn[:, k, :],
                                           start=(k == 0), stop=(k == 7)), reads=[wu, hn], writes=[bK])
    for j in range(4):
        for k in range(8):
            S.op('pe', lambda e, k=k, j=j: e.matmul(bV[:, j * nv:(j + 1) * nv], hn[:, k, j * 128:(j + 1) * 128],
                                                    wu[:, k, vcols[0]:vcols[1]], start=(k == 0), stop=(k == 7)),
                 reads=[wu, hn], writes=[bV])
    return bQ, bK, bV


def fox_alloc(S, C):
    C.e1 = S.tile([65, 512], F32)
    C.lf = S.tile([65, 512], F32)
    C.cn = [S.tile([65, 512], F32) for _ in range(2)]
    C.rt = [S.tile([65, 512], F32) for _ in range(2)]
    C.stg = [S.tile([65, 3, 512], BF16) for _ in range(2)]
    C.onesrow = S.tile([65, 512], F32)
    S.op('dve', lambda e: e.memset(C.onesrow[:], 1.0), writes=[C.onesrow])
    C.fwu = S.tile([128, 8, 200], BF16)
    C.fg = S.tile([64, 2], F32)
    C.fgs = S.tile([64, 1], F32)
    C.ffb = S.tile([65, 1], F32)
    C.fnb = S.tile([65, 1], F32)


def emit_fox_unit(S, C, hnT, wu_d, g_d, fb_d, dm_d, yout):
    Qa, Ka, Va = C.Qa, C.Ka, C.Va
    wu, gq, gs, fb, nfb = C.fwu, C.fg, C.fgs, C.ffb, C.fnb
    load_w(S, C, wu, wu_d)
    S.dma(gq[:], g_d[:], reads=[g_d], writes=[gq])
    S.dma(fb[64:65, :], fb_d[64:65, :], reads=[fb_d], writes=[fb])
    S.dma(C.dm[:], dm_d[:], reads=[dm_d], writes=[C.dm])
    S.op('dve', lambda e: e.tensor_scalar(out=gs[:], in0=gq[:, 0:1], scalar1=HD ** -0.5, scalar2=None,
                                          op0=ALU.mult), reads=[gq], writes=[gs])
    S.op('dve', lambda e: e.tensor_scalar(out=nfb[64:65, :], in0=fb[64:65, :], scalar1=-1.0, scalar2=None,
                                          op0=ALU.mult), reads=[fb], writes=[nfb])
    S.op('pool', lambda e: e.memset(Qa[64:70, :], 1.0), writes=C.QR)
    S.op('pool', lambda e: e.memset(Ka[64:70, :], -1.0), writes=C.KR)
    for tt in range(T // 512):
        cs = slice(tt * 512, (tt + 1) * 512)
        bQ, bK, bV = emit_proj_mm(S, C, hnT, wu, tt, (0, 65), (65, 129), (129, 193))
        emit_qknorm(S, C, bQ, 0, gs[:, 0:1], [(Qa[0:64, cs], [C.QF[tt]])], 2 * tt)
        emit_qknorm(S, C, bK, 0, gq[:, 1:2], [(Ka[0:64, cs], [C.KF[tt]])], 2 * tt + 1)
        S.op('act', lambda e: e.activation(out=Va[:, 4 * tt:4 * tt + 4, 0:64],
                                           in_=bV[:, 0:256].rearrange("p (j d) -> p j d", j=4), func=AF.Copy),
             reads=[bV], writes=[Va])
        e1, lf = C.e1, C.lf
        cn, cnp = C.cn[tt % 2], C.cn[(tt + 1) % 2]
        rt = C.rt
        stg = C.stg[tt % 2]
        r = slice(64, 65)
        S.op('act', lambda e: e.activation(out=e1[r, :], in_=bQ[r, :], func=AF.Exp, bias=nfb[r, 0:1], scale=-1.0),
             reads=[bQ, nfb], writes=[e1])
        S.op('act', lambda e: e.activation(out=lf[r, :], in_=e1[r, :], func=AF.Ln, bias=1.0, scale=1.0),
             reads=[e1], writes=[lf])
        init = 0.0 if tt == 0 else cnp[r, 511:512]
        S.op('dve', lambda e: e.tensor_tensor_scan(out=cn[r, :], data0=C.onesrow[r, :], data1=lf[r, :], initial=init,
                                                   op0=ALU.mult, op1=ALU.add),
             reads=[C.onesrow, lf, cnp], writes=[cn])
        S.op('dve', lambda e: e.tensor_copy(out=stg[r, 0, :], in_=cn[r, :]), reads=[cn], writes=[stg])
        S.op('dve', lambda e: e.tensor_tensor(out=rt[0][r, :], in0=cn[r, :], in1=stg[r, 0, :], op=ALU.subtract),
             reads=[cn, stg], writes=[rt[0]])
        S.op('dve', lambda e: e.tensor_copy(out=stg[r, 1, :], in_=rt[0][r, :]), reads=[rt[0]], writes=[stg])
        S.op('dve', lambda e: e.tensor_tensor(out=rt[1][r, :], in0=rt[0][r, :], in1=stg[r, 1, :], op=ALU.subtract),
             reads=[rt[0], stg], writes=[rt[1]])
        S.op('dve', lambda e: e.tensor_copy(out=stg[r, 2, :], in_=rt[1][r, :]), reads=[rt[1]], writes=[stg])
        for i in range(3):
            S.dma(Qa[64 + i:65 + i, cs], stg[r, i, :], reads=[stg], writes=[C.QR[tt]], q='pool')
            S.dma(Ka[67 + i:68 + i, cs], stg[r, i, :], reads=[stg], writes=[C.KR[tt]], q='pool')
    emit_attention(S, C, yout, (0, 70), (0, 70))


def fox_dmask():
    m = np.zeros((128, 4, 512), np.float32)
    k = np.arange(128)[:, None]
    q = np.arange(512)[None, :]
    for a in range(4):
        m[:, a, :] = np.where(a * 128 + k <= q, 0.0, NEG)
    return m


def build_mix(kinds):
    nc = new_nc()
    with ExitStack() as es:
        S = Sched(nc, es)
        hnT = S.dram_in("hnT", [128, 8, T], BF16)
        C = mix_common(S)
        outs = []
        if 'fox' in kinds or 'moba' in kinds:
            attn_alloc(S, C, 'moba' in kinds)
        if 'fox' in kinds:
            fox_alloc(S, C)
            fdm = S.dram_in("fox_dm", [128, 4, 512], F32)
        if 'ml' in kinds:
            ml_alloc(S, C)
            mU = S.dram_in("ml_U", [128, 128], F32)
        if 'moba' in kinds or 'ml' in kinds:
            idd = S.dram_in("ident", [128, 128], F32)
        if 'moba' in kinds:
            moba_alloc(S, C)
            bdm = S.dram_in("moba_dm", [128, 4, 512], F32)
            boh = S.dram_in("moba_oh", [32, T], BF16)
        for u, kind in enumerate(kinds):
            if kind == 'fox':
                wu_d = S.dram_in("wu%d" % u, [128, 8, 200], BF16)
                g_d = S.dram_in("g%d" % u, [64, 2], F32)
                fb_d = S.dram_in("fb%d" % u, [65, 1], F32)
                yout = S.dram_out("y%d" % u, [64, T], BF16)
                emit_fox_unit(S, C, hnT, wu_d, g_d, fb_d, fdm, yout)
                outs.append(yout)
            elif kind == 'moba':
                wu_d = S.dram_in("wu%d" % u, [128, 8, 200], BF16)
                g_d = S.dram_in("g%d" % u, [64, 2], F32)
                qc_d = S.dram_in("qc%d" % u, [8, T], BF16)
                kc_d = S.dram_in("kc%d" % u, [8, T], BF16)
                yout = S.dram_out("y%d" % u, [64, T], BF16)
                emit_moba_unit(S, C, hnT, wu_d, g_d, qc_d, kc_d, boh, bdm, idd, yout)
                outs.append(yout)
            elif kind == 'ml':
                wu_d = S.dram_in("wu%d" % u, [128, 8, 260], BF16)
                cw_d = S.dram_in("cw%d" % u, [64, 2, 4], F32)
                cb_d = S.dram_in("cb%d" % u, [64, 2], F32)
                ib_d = S.dram_in("ib%d" % u, [128, 1], F32)
                fb_d = S.dram_in("fb%d" % u, [128, 1], F32)
                g_d = S.dram_in("mg%d" % u, [128, 4, 64], F32)
                yout = S.dram_out("y%d" % u, [64, T], BF16)
                emit_ml_unit(S, C, hnT, wu_d, cw_d, cb_d, ib_d, fb_d, g_d, mU, idd, yout)
                outs.append(yout)
        S.finish(outs)
    return nc


def to_fm(a):
    n, f = a.shape
    return np.ascontiguousarray(a.reshape(n, f // 128, 128).transpose(2, 1, 0))


def fox_unit_inputs(u, ws_l, inp, l, h):
    w = ws_l['w_in']
    wu = np.zeros((128, 8, 200), w.dtype)
    wu[:, :, 0:64] = w[:, :, O_FQ + 64 * h:O_FQ + 64 * h + 64]
    wu[:, :, 64] = w[:, :, O_FF + h]
    wu[:, :, 65:129] = w[:, :, O_FK + 64 * h:O_FK + 64 * h + 64]
    wu[:, :, 129:193] = w[:, :, O_FV + 64 * h:O_FV + 64 * h + 64]
    g = np.stack([inp['fox_q_g'][l], inp['fox_k_g'][l]], axis=1).astype(np.float32)
    fb = np.zeros((65, 1), np.float32)
    fb[64, 0] = inp['fox_f_bias'][l][h]
    return {"wu%d" % u: wu, "g%d" % u: np.ascontiguousarray(g), "fb%d" % u: fb}


def moba_alloc(S, C):
    C.bwu = S.tile([128, 8, 200], BF16)
    C.bg = S.tile([64, 2], F32)
    C.bgs = S.tile([64, 1], F32)
    C.qn32 = [S.tile([64, 512], F32) for _ in range(2)]
    C.kn32 = [S.tile([64, 512], F32) for _ in range(2)]
    C.kmT = S.tile([64, 64], F32)
    C.gsb = S.tile([128, 4, 64], F32)
    C.m8 = S.tile([128, 4, 8], F32)
    C.thr = S.tile([128, 4], F32)
    C.MB = S.tile([128, 4, 2, 128], F32)
    S.op('pool', lambda e: e.memset(C.MB[:], 0.0), writes=[C.MB])
    C.ident = S.tile([128, 128], F32)
    C.mstg = [S.tile([32, 512], BF16) for _ in range(4)]


def emit_moba_unit(S, C, hnT, wu_d, g_d, qc_d, kc_d, oh_d, dm_d, id_d, yout):
    Qa, QaB, Ka, Va = C.Qa, C.QaB, C.Ka, C.Va
    wu, gq, gs = C.bwu, C.bg, C.bgs
    H2 = T // 2
    load_w(S, C, wu, wu_d)
    S.dma(gq[:], g_d[:], reads=[g_d], writes=[gq])
    S.dma(C.dm[:], dm_d[:], reads=[dm_d], writes=[C.dm])
    S.dma(C.ident[:], id_d[:], reads=[id_d], writes=[C.ident])
    S.op('dve', lambda e: e.tensor_scalar(out=gs[:], in0=gq[:, 0:1], scalar1=HD ** -0.5, scalar2=None,
                                          op0=ALU.mult), reads=[gq], writes=[gs])
    S.op('pool', lambda e: e.memset(Qa[64:96, :], 0.0), writes=C.QR)
    S.op('pool', lambda e: e.memset(QaB[64:96, :], 0.0), writes=C.QBR)
    S.op('pool', lambda e: e.memset(Ka[64:96, :], 0.0), writes=C.KR)
    S.dma(Qa[64:72, :], qc_d[:], reads=[qc_d], writes=C.QR)
    S.dma(QaB[64:72, :], qc_d[:, H2:], reads=[qc_d], writes=C.QBR)
    S.dma(Ka[64:72, :], kc_d[:], reads=[kc_d], writes=C.KR)
    S.dma(Ka[96:128, :], oh_d[:], reads=[oh_d], writes=C.KR)
    S.op('dve', lambda e: e.memset(C.kmT[:], 0.0), writes=[C.kmT])
    for tt in range(T // 512):
        cs = slice(tt * 512, (tt + 1) * 512)
        csB = slice(tt * 512 - H2, (tt + 1) * 512 - H2)
        second = tt >= 16
        bQ, bK, bV = emit_proj_mm(S, C, hnT, wu, tt, (0, 64), (65, 129), (129, 193))
        qn = C.qn32[tt % 2]
        kn = C.kn32[tt % 2]
        qd = [(Qa[0:64, cs], [C.QF[tt]]), (qn[:, :], [qn])]
        if second:
            qd.append((QaB[0:64, csB], [C.QBF[tt]]))
        emit_qknorm(S, C, bQ, 0, gs[:, 0:1], qd, 2 * tt)
        emit_qknorm(S, C, bK, 0, gq[:, 1:2], [(Ka[0:64, cs], [C.KF[tt]]), (kn[:, :], [kn])], 2 * tt + 1)
        S.op('act', lambda e: e.activation(out=Va[:, 4 * tt:4 * tt + 4, 0:64],
                                           in_=bV[:, 0:256].rearrange("p (j d) -> p j d", j=4), func=AF.Copy),
             reads=[bV], writes=[Va])
        S.op('dve', lambda e: e.tensor_reduce(out=C.kmT[:, 2 * tt:2 * tt + 2],
                                              in_=kn[:, :].rearrange("p (a n) -> p a n", a=2),
                                              axis=AX.X, op=ALU.add), reads=[kn], writes=[C.kmT])
        o4 = 4 * ((tt + 1) % 2)
        bG = C.banks[2 + o4]
        for j in range(4):
            S.op('pe', lambda e, j=j: e.matmul(bG[:, j * 64:(j + 1) * 64], qn[:, j * 128:(j + 1) * 128],
                                               C.kmT[:, :], start=True, stop=True),
                 reads=[qn, C.kmT], writes=[bG])
        gsb, m8, thr, MB = C.gsb, C.m8, C.thr, C.MB
        S.op('act', lambda e: e.activation(out=gsb[:], in_=bG[:, 0:256].rearrange("p (j n) -> p j n", j=4),
                                           func=AF.Copy), reads=[bG], writes=[gsb])
        for half in range(2):
            qblk = 2 * tt + half
            S.op('dve', lambda e: e.memset(gsb[:, 2 * half:2 * half + 2, qblk:64], -1e30), writes=[gsb])
        for j in range(4):
            S.op('dve', lambda e, j=j: e.max(out=m8[:, j, :], in_=gsb[:, j, :]), reads=[gsb], writes=[m8])
        S.op('dve', lambda e: e.tensor_scalar(out=thr[:], in0=m8[:, :, 2], scalar1=-1e29, scalar2=None, op0=ALU.max),
             reads=[m8], writes=[thr])
        for j in range(4):
            S.op('dve', lambda e, j=j: e.tensor_scalar(out=MB[:, j, :, 0:32],
                                                       in0=gsb[:, j, :].rearrange("p (a n) -> p a n", a=2),
                                                       scalar1=thr[:, j:j + 1], scalar2=-NEG,
                                                       op0=ALU.is_ge, op1=ALU.mult), reads=[gsb, thr], writes=[MB])
        S.op('dve', lambda e: e.tensor_scalar(out=MB[:, :, :, 0:32], in0=MB[:, :, :, 0:32], scalar1=NEG, scalar2=None,
                                              op0=ALU.add), reads=[MB], writes=[MB])
        for pg in range(2 if second else 1):
            bT = C.banks[pg + o4]
            for j in range(4):
                S.op('pe', lambda e, j=j, pg=pg: e.transpose(out=bT[:, j * 128:(j + 1) * 128], in_=MB[:, j, pg, :],
                                                            identity=C.ident[:, :]),
                     reads=[MB, C.ident], writes=[bT])
            stg = C.mstg[(2 * tt + pg) % 4]
            S.op('act', lambda e: e.activation(out=stg[:], in_=bT[0:32, :], func=AF.Copy), reads=[bT], writes=[stg])
            if pg == 0:
                S.dma(Qa[96:128, cs], stg[:], reads=[stg], writes=[C.QR[tt]], q='pool')
            else:
                S.dma(QaB[96:128, csB], stg[:], reads=[stg], writes=[C.QBR[tt]], q='pool')
    emit_attention(S, C, yout, (0, 128), (0, 72), dsplit=True)


def moba_dmask():
    m = np.full((128, 4, 512), NEG, np.float32)
    k = np.arange(128)[:, None]
    q = np.arange(512)[None, :]
    for a in range(4):
        kk = a * 128 + k
        ok = (kk // 256 == q // 256) & (kk <= q)
        if a < 2:
            ok = ok | (q >= 256)
        m[:, a, :] = np.where(ok, 0.0, NEG)
    return m


def moba_consts(h):
    slope = float(2.0 ** (-8.0 * (h + 1) / MOBH))
    s1 = np.float32(np.float32(slope).astype(NPBF))
    s2 = np.float32(np.float32(np.float32(slope) - s1).astype(NPBF))
    pos = np.arange(T)
    a = (pos // 256).astype(np.float32)
    b = (pos % 256).astype(np.float32)
    one = np.ones(T, np.float32)
    qc = np.stack([a, b, a, b, 256 * s1 * one, s1 * one, 256 * s2 * one, s2 * one]).astype(NPBF)
    kc = np.stack([-256 * s1 * one, -s1 * one, -256 * s2 * one, -s2 * one, a, b, a, b]).astype(NPBF)
    return qc, kc


def moba_onehot():
    pos = np.arange(T)
    oh = ((pos[None, :] // 256) % 32 == np.arange(32)[:, None]).astype(np.float32)
    return oh.astype(NPBF)


def moba_unit_inputs(u, ws_l, inp, l, h):
    w = ws_l['w_in']
    wu = np.zeros((128, 8, 200), w.dtype)
    wu[:, :, 0:64] = w[:, :, O_BQ + 64 * h:O_BQ + 64 * h + 64]
    wu[:, :, 65:129] = w[:, :, O_BK + 64 * h:O_BK + 64 * h + 64]
    wu[:, :, 129:193] = w[:, :, O_BV + 64 * h:O_BV + 64 * h + 64]
    g = np.stack([inp['moba_q_g'][l], inp['moba_k_g'][l]], axis=1).astype(np.float32)
    qc, kc = moba_consts(h)
    return {"wu%d" % u: wu, "g%d" % u: np.ascontiguousarray(g), "qc%d" % u: qc, "kc%d" % u: kc}


def ml_alloc(S, C):
    C.mwu = S.tile([128, 8, 260], BF16)
    C.mcw = S.tile([64, 2, 4], F32)
    C.mcb = S.tile([64, 2], F32)
    C.mib = S.tile([128, 1], F32)
    C.mfb = S.tile([128, 1], F32)
    C.mnfb = S.tile([128, 1], F32)
    C.mg = S.tile([128, 4, 64], F32)
    C.mU = S.tile([128, 128], F32)
    C.mUb = S.tile([128, 128], BF16)
    C.mid = S.tile([64, 64], F32)
    C.pc = [S.tile([64, 515], F32) for _ in range(2)]
    C.acc = [S.tile([64, 512], F32) for _ in range(2)]
    C.qc = [S.tile([64, 512], BF16) for _ in range(2)]
    C.kc = [S.tile([64, 512], BF16) for _ in range(2)]
    C.kcf = [S.tile([64, 512], F32) for _ in range(2)]
    C.e4 = S.tile([128, 4], F32)
    C.lf4 = S.tile([128, 4], F32)
    C.ii4 = S.tile([128, 4], F32)
    C.tmp4 = S.tile([128, 4], F32)
    C.qs4 = [S.tile([128, 4], F32) for _ in range(2)]
    C.ks4 = [S.tile([128, 4], F32) for _ in range(2)]
    C.eB4 = [S.tile([128, 4], F32) for _ in range(2)]
    C.Vt = [S.tile([128, 4, 65], BF16) for _ in range(2)]
    C.sg = S.tile([128, 4, 64], F32)
    C.GS = [S.tile([128, 4, 64], F32) for _ in range(2)]
    C.sm = [S.tile([128, 128], BF16) for _ in range(2)]
    C.Ktok = [S.tile([128, 64], BF16) for _ in range(2)]
    C.ctmp = S.tile([64, 65], F32)
    C.Cf = S.tile([64, 65], F32)
    C.Cbf = [S.tile([64, 65], BF16) for _ in range(2)]
    C.dn4 = S.tile([128, 4], F32)
    C.fac4 = S.tile([128, 4], F32)
    C.hh = S.tile([128, 4, 64], F32)
    C.hsq = S.tile([128, 4, 64], F32)
    C.ss4 = S.tile([128, 4], F32)
    C.rstd4 = S.tile([128, 4], F32)
    C.Y = [S.tile([64, 512], BF16) for _ in range(2)]
    C.Yf = S.tile([128, 4, 64], F32)
    C.mid128 = S.tile([128, 128], F32)


def emit_ml_unit(S, C, hnT, wu_d, cw_d, cb_d, ib_d, fb_d, g_d, U_d, id_d, yout):
    wu = C.mwu
    load_w(S, C, wu, wu_d)
    S.dma(C.mcw[:], cw_d[:], reads=[cw_d], writes=[C.mcw])
    S.dma(C.mcb[:], cb_d[:], reads=[cb_d], writes=[C.mcb])
    S.dma(C.mib[:], ib_d[:], reads=[ib_d], writes=[C.mib])
    S.dma(C.mfb[:], fb_d[:], reads=[fb_d], writes=[C.mfb])
    S.dma(C.mg[:], g_d[:], reads=[g_d], writes=[C.mg])
    S.dma(C.mU[:], U_d[:], reads=[U_d], writes=[C.mU])
    S.dma(C.mid[:], id_d[0:64, 0:64], reads=[id_d], writes=[C.mid])
    S.dma(C.mid128[:], id_d[:, :], reads=[id_d], writes=[C.mid128])
    S.op('dve', lambda e: e.tensor_copy(out=C.mUb[:], in_=C.mU[:]), reads=[C.mU], writes=[C.mUb])
    S.op('dve', lambda e: e.tensor_scalar(out=C.mnfb[:], in0=C.mfb[:], scalar1=-1.0, scalar2=None, op0=ALU.mult),
         reads=[C.mfb], writes=[C.mnfb])
    for i in range(2):
        S.op('dve', lambda e, i=i: e.memset(C.pc[i][:, 0:3], 0.0), writes=[C.pc[i]])
    S.op('dve', lambda e: e.memset(C.Cf[:], 0.0), writes=[C.Cf])
    S.op('dve', lambda e: e.memset(C.Cbf[0][:], 0.0), writes=[C.Cbf[0]])
    b = C.banks
    cidx = 0
    for tt in range(T // 512):
        hn = C.hn[tt % 2]
        if tt == 0:
            C.load_hn(S, hn, hnT, 0)
        if tt + 1 < T // 512:
            C.load_hn(S, C.hn[(tt + 1) % 2], hnT, tt + 1)
        bQ, bK, bA, bB, bGt, bS, bKC, bO = b
        for k in range(8):
            S.op('pe', lambda e, k=k: e.matmul(bQ[0:64, :], wu[:, k, 0:64], hn[:, k, :], start=(k == 0), stop=(k == 7)),
                 reads=[wu, hn], writes=[bQ])
        for k in range(8):
            S.op('pe', lambda e, k=k: e.matmul(bK[0:64, :], wu[:, k, 64:128], hn[:, k, :], start=(k == 0), stop=(k == 7)),
                 reads=[wu, hn], writes=[bK])
        for j in range(4):
            for k in range(8):
                S.op('pe', lambda e, k=k, j=j: e.matmul(bA[:, j * 66:(j + 1) * 66], hn[:, k, j * 128:(j + 1) * 128],
                                                        wu[:, k, 128:194], start=(k == 0), stop=(k == 7)),
                     reads=[wu, hn], writes=[bA])
        for j in range(4):
            for k in range(8):
                S.op('pe', lambda e, k=k, j=j: e.matmul(bB[:, j * 64:(j + 1) * 64], hn[:, k, j * 128:(j + 1) * 128],
                                                        wu[:, k, 194:258], start=(k == 0), stop=(k == 7)),
                     reads=[wu, hn], writes=[bB])
        bAv = bA[:, 0:264].rearrange("p (j d) -> p j d", j=4)
        qc, kc, kcf = C.qc[tt % 2], C.kc[tt % 2], C.kcf[tt % 2]
        for which, ps in ((0, bQ), (1, bK)):
            pc = C.pc[which]
            acc = C.acc[which]
            S.op('act', lambda e: e.activation(out=pc[:, 3:515], in_=ps[0:64, :], func=AF.Copy), reads=[ps], writes=[pc])
            S.op('dve', lambda e: e.tensor_scalar(out=acc[:], in0=pc[:, 3:515], scalar1=C.mcw[:, which, 3:4],
                                                  scalar2=C.mcb[:, which:which + 1], op0=ALU.mult, op1=ALU.add),
                 reads=[pc, C.mcw, C.mcb], writes=[acc])
            for tap in (2, 1, 0):
                S.op('dve', lambda e, tap=tap: e.scalar_tensor_tensor(out=acc[:], in0=pc[:, tap:tap + 512],
                                                                      scalar=C.mcw[:, which, tap:tap + 1], in1=acc[:],
                                                                      op0=ALU.mult, op1=ALU.add),
                     reads=[pc, C.mcw, acc], writes=[acc])
            S.op('dve', lambda e: e.tensor_copy(out=pc[:, 0:3], in_=pc[:, 512:515]), reads=[pc], writes=[pc])
            if which == 0:
                S.op('act', lambda e: e.activation(out=qc[:], in_=acc[:], func=AF.Silu), reads=[acc], writes=[qc])
            else:
                S.op('act', lambda e: e.activation(out=kcf[:], in_=acc[:], func=AF.Silu), reads=[acc], writes=[kcf])
                S.op('pool', lambda e: e.tensor_copy(out=kc[:], in_=kcf[:]), reads=[kcf], writes=[kc])
        e4, lf4, ii4, tmp4 = C.e4, C.lf4, C.ii4, C.tmp4
        qs4, ks4, eB4 = C.qs4[tt % 2], C.ks4[tt % 2], C.eB4[tt % 2]
        S.op('act', lambda e: e.activation(out=e4[:], in_=bAv[:, :, 65], func=AF.Exp, bias=C.mnfb[:, 0:1], scale=-1.0),
             reads=[bA, C.mnfb], writes=[e4])
        S.op('act', lambda e: e.activation(out=lf4[:], in_=e4[:], func=AF.Ln, bias=1.0, scale=1.0),
             reads=[e4], writes=[lf4])
        S.op('dve', lambda e: e.tensor_scalar(out=ii4[:], in0=bAv[:, :, 64], scalar1=C.mib[:, 0:1], scalar2=None,
                                              op0=ALU.add), reads=[bA, C.mib], writes=[ii4])
        S.op('pe', lambda e: e.matmul(bGt[:, 0:4], C.mU[:, :], lf4[:, :], start=True, stop=True),
             reads=[C.mU, lf4], writes=[bGt])
        S.op('pe', lambda e: e.matmul(bGt[:, 4:8], C.ones64[:, :], lf4[:, :], start=True, stop=True),
             reads=[C.ones64, lf4], writes=[bGt])
        S.op('act', lambda e: e.activation(out=qs4[:], in_=bGt[:, 0:4], func=AF.Exp, scale=-1.0), reads=[bGt], writes=[qs4])
        S.op('act', lambda e: e.activation(out=eB4[:], in_=bGt[:, 4:8], func=AF.Exp, scale=-1.0), reads=[bGt], writes=[eB4])
        S.op('dve', lambda e: e.tensor_tensor(out=tmp4[:], in0=bGt[:, 0:4], in1=ii4[:], op=ALU.add),
             reads=[bGt, ii4], writes=[tmp4])
        S.op('act', lambda e: e.activation(out=ks4[:], in_=tmp4[:], func=AF.Exp, bias=C.lnsc[:, 0:1], scale=1.0),
             reads=[tmp4, C.lnsc], writes=[ks4])
        Vt = C.Vt[tt % 2]
        for j in range(4):
            S.op('dve', lambda e, j=j: e.tensor_scalar(out=Vt[:, j, 0:64], in0=bAv[:, j, 0:64], scalar1=ks4[:, j:j + 1],
                                                       scalar2=None, op0=ALU.mult), reads=[bA, ks4], writes=[Vt])
        S.op('dve', lambda e: e.tensor_copy(out=Vt[:, :, 64], in_=ks4[:, :]), reads=[ks4], writes=[Vt])
        GS = C.GS[tt % 2]
        S.op('act', lambda e: e.activation(out=C.sg[:], in_=bB[:, 0:256].rearrange("p (j d) -> p j d", j=4),
                                           func=AF.Sigmoid), reads=[bB], writes=[C.sg])
        S.op('pool', lambda e: e.tensor_tensor(out=GS[:], in0=C.sg[:], in1=C.mg[:], op=ALU.mult),
             reads=[C.sg, C.mg], writes=[GS])
        for j in range(4):
            js = slice(j * 128, (j + 1) * 128)
            sm = C.sm[cidx % 2]
            Ktok = C.Ktok[cidx % 2]
            Cb_in = C.Cbf[cidx % 2]
            Cb_out = C.Cbf[(cidx + 1) % 2]
            S.op('pe', lambda e: e.matmul(bS[:, 0:128], kc[:, js], qc[:, js], start=True, stop=True),
                 reads=[kc, qc], writes=[bS])
            S.op('dve', lambda e: e.tensor_tensor(out=sm[:], in0=bS[:, 0:128], in1=C.mUb[:], op=ALU.mult),
                 reads=[bS, C.mUb], writes=[sm])
            S.op('pe', lambda e: e.transpose(out=bKC[:, 0:64], in_=kcf[:, js], identity=C.mid[:, :]),
                 reads=[kcf, C.mid], writes=[bKC])
            S.op('act', lambda e: e.activation(out=Ktok[:], in_=bKC[:, 0:64], func=AF.Copy), reads=[bKC], writes=[Ktok])
            S.op('pe', lambda e: e.matmul(bO[:, j * 65:(j + 1) * 65], sm[:, :], Vt[:, j, :], start=True, stop=False),
                 reads=[sm, Vt], writes=[bO])
            S.op('pe', lambda e: e.matmul(bO[:, j * 65:(j + 1) * 65], qc[:, js], Cb_in[:, :], start=False, stop=True),
                 reads=[qc, Cb_in], writes=[bO])
            S.op('pe', lambda e: e.matmul(bKC[0:64, 256:321], Ktok[:, :], Vt[:, j, :], start=True, stop=True),
                 reads=[Ktok, Vt], writes=[bKC])
            S.op('act', lambda e: e.activation(out=C.ctmp[:], in_=bKC[0:64, 256:321], func=AF.Copy,
                                               scale=eB4[0:64, j:j + 1]), reads=[bKC, eB4], writes=[C.ctmp])
            S.op('dve', lambda e: e.scalar_tensor_tensor(out=C.Cf[:], in0=C.Cf[:], scalar=eB4[0:64, j:j + 1],
                                                         in1=C.ctmp[:], op0=ALU.mult, op1=ALU.add),
                 reads=[C.Cf, eB4, C.ctmp], writes=[C.Cf])
            S.op('pool', lambda e: e.tensor_copy(out=Cb_out[:], in_=C.Cf[:]), reads=[C.Cf], writes=[Cb_out])
            cidx += 1
        bOv = bO[:, 0:260].rearrange("p (j d) -> p j d", j=4)
        dn4, fac4, hh, hsq, ss4, rstd4 = C.dn4, C.fac4, C.hh, C.hsq, C.ss4, C.rstd4
        S.op('dve', lambda e: e.tensor_tensor(out=dn4[:], in0=bOv[:, :, 64], in1=qs4[:], op=ALU.mult),
             reads=[bO, qs4], writes=[dn4])
        S.op('act', lambda e: e.activation(out=dn4[:], in_=dn4[:], func=AF.Abs), reads=[dn4], writes=[dn4])
        S.op('dve', lambda e: e.tensor_scalar(out=dn4[:], in0=dn4[:], scalar1=1.0, scalar2=None, op0=ALU.max),
             reads=[dn4], writes=[dn4])
        S.op('dve', lambda e: e.reciprocal(out=dn4[:], in_=dn4[:]), reads=[dn4], writes=[dn4])
        S.op('dve', lambda e: e.tensor_tensor(out=fac4[:], in0=dn4[:], in1=qs4[:], op=ALU.mult),
             reads=[dn4, qs4], writes=[fac4])
        for j in range(4):
            S.op('act', lambda e, j=j: e.activation(out=hh[:, j, :], in_=bOv[:, j, 0:64], func=AF.Copy,
                                                    scale=fac4[:, j:j + 1]), reads=[bO, fac4], writes=[hh])
        S.op('dve', lambda e: e.tensor_tensor(out=hsq[:], in0=hh[:], in1=hh[:], op=ALU.mult), reads=[hh], writes=[hsq])
        S.op('dve', lambda e: e.tensor_reduce(out=ss4[:], in_=hsq[:], axis=AX.X, op=ALU.add), reads=[hsq], writes=[ss4])
        S.op('act', lambda e: e.activation(out=ss4[:], in_=ss4[:], func=AF.Sqrt, bias=C.epsc[:, 0:1], scale=1.0 / HD),
             reads=[ss4, C.epsc], writes=[ss4])
        S.op('dve', lambda e: e.reciprocal(out=rstd4[:], in_=ss4[:]), reads=[ss4], writes=[rstd4])
        Yf = C.Yf
        for j in range(4):
            S.op('dve', lambda e, j=j: e.scalar_tensor_tensor(out=Yf[:, j, :], in0=hh[:, j, :], scalar=rstd4[:, j:j + 1],
                                                              in1=GS[:, j, :], op0=ALU.mult, op1=ALU.mult),
                 reads=[hh, rstd4, GS], writes=[Yf])
        for j in range(4):
            S.op('pe', lambda e, j=j: e.transpose(out=bS[0:64, j * 128:(j + 1) * 128], in_=Yf[:, j, :],
                                                  identity=C.mid128[:, :]), reads=[Yf, C.mid128], writes=[bS])
        Y = C.Y[tt % 2]
        S.op('act', lambda e: e.activation(out=Y[:], in_=bS[0:64, :], func=AF.Copy), reads=[bS], writes=[Y])
        S.dma(yout[0:64, tt * 512:(tt + 1) * 512], Y[:], reads=[Y], writes=[yout])


def ml_consts():
    s = np.arange(128)[:, None]
    j = np.arange(128)[None, :]
    return (s <= j).astype(np.float32)


def ml_unit_inputs(u, ws_l, inp, l, h):
    w = ws_l['w_in']
    wu = np.zeros((128, 8, 260), w.dtype)
    wu[:, :, 0:64] = w[:, :, O_MQ + 64 * h:O_MQ + 64 * h + 64]
    wu[:, :, 64:128] = w[:, :, O_MK + 64 * h:O_MK + 64 * h + 64]
    wu[:, :, 128:192] = w[:, :, O_MV + 64 * h:O_MV + 64 * h + 64]
    wu[:, :, 192] = w[:, :, O_MI + h]
    wu[:, :, 193] = w[:, :, O_MF + h]
    wu[:, :, 194:258] = w[:, :, O_MO + 64 * h:O_MO + 64 * h + 64]
    cw = inp['ml_conv_w'][l]
    cb = inp['ml_conv_b'][l]
    cwq = cw[:, 64 * h:64 * h + 64].T
    cwk = cw[:, 256 + 64 * h:256 + 64 * h + 64].T
    cwu = np.ascontiguousarray(np.stack([cwq, cwk], axis=1)).astype(np.float32)
    cbu = np.ascontiguousarray(np.stack([cb[64 * h:64 * h + 64], cb[256 + 64 * h:256 + 64 * h + 64]], axis=1)).astype(np.float32)
    ib = np.full((128, 1), inp['ml_i_bias'][l][h], np.float32)
    fb = np.full((128, 1), inp['ml_f_bias'][l][h], np.float32)
    g = np.ascontiguousarray(np.broadcast_to(inp['ml_h_g'][l][64 * h:64 * h + 64][None, None, :], (128, 4, 64))).astype(np.float32)
    return {"wu%d" % u: wu, "cw%d" % u: cwu, "cb%d" % u: cbu, "ib%d" % u: ib, "fb%d" % u: fb, "mg%d" % u: g}


NTOK = B * T // NCORE
NTT = 1024
MFF = DFF // 128


def emit_norm_mod(S, C, x, hn, a, shb, shi, ncols):
    for h0 in range(0, ncols, 512):
        hs = slice(h0, h0 + 512)
        bSS = C.nextbank()
        for k in range(8):
            sq = C.sq[k % 2]
            S.op('act', lambda e, k=k: e.activation(out=sq[:], in_=x[:, k, hs], func=AF.Square), reads=[x], writes=[sq])
            S.op('pe', lambda e, k=k: e.matmul(bSS[:, :], C.ones[:, :], sq[:], start=(k == 0), stop=(k == 7)),
                 reads=[C.ones, sq], writes=[bSS])
        rs = C.rs
        S.op('act', lambda e: e.activation(out=rs[:], in_=bSS[:, :], func=AF.Sqrt, bias=C.epsc[:, 0:1], scale=1.0 / D),
             reads=[bSS, C.epsc], writes=[rs])
        S.op('dve', lambda e: e.reciprocal(out=rs[:], in_=rs[:]), reads=[rs], writes=[rs])
        for k in range(8):
            t = C.t[k % 2]
            S.op('dve', lambda e, k=k: e.tensor_tensor(out=t[:], in0=x[:, k, hs], in1=rs[:], op=ALU.mult),
                 reads=[x, rs], writes=[t])
            S.op('act', lambda e, k=k: e.activation(out=hn[:, k, hs], in_=t[:], func=AF.Identity,
                                                    bias=shb[:, shi, k:k + 1], scale=a[:, k:k + 1]),
                 reads=[t, a, shb], writes=[hn])


def build_dense(post, nextnorm):
    nc = new_nc()
    with ExitStack() as es:
        S = Sched(nc, es)
        C = MixCtx()
        banks = [S.psum([128, 512], F32) for _ in range(8)]
        C.bi = 0

        def nextbank():
            C.bi += 1
            return banks[C.bi % 8]
        C.nextbank = nextbank
        C.ones = S.tile([128, 128], F32)
        S.op('dve', lambda e: e.memset(C.ones[:], 1.0), writes=[C.ones])
        C.epsc = S.tile([128, 1], F32)
        S.op('dve', lambda e: e.memset(C.epsc[:], EPS), writes=[C.epsc])
        C.sq = [S.tile([128, 512], F32) for _ in range(2)]
        C.t = [S.tile([128, 512], F32) for _ in range(2)]
        C.rs = S.tile([128, 512], F32)
        xT = S.dram_in("xT", [128, 8, NTOK], F32)
        vec = S.dram_in("vec", [128, 10, 8], F32)
        v = S.tile([128, 10, 8], F32)
        S.dma(v[:], vec[:], reads=[vec], writes=[v])
        a2 = S.tile([128, 8], F32)
        a1n = S.tile([128, 8], F32)
        S.op('dve', lambda e: e.tensor_scalar(out=a2[:], in0=v[:, 2, :], scalar1=1.0, scalar2=None, op0=ALU.add),
             reads=[v], writes=[a2])
        S.op('dve', lambda e: e.tensor_tensor(out=a2[:], in0=a2[:], in1=v[:, 1, :], op=ALU.mult), reads=[a2, v], writes=[a2])
        S.op('dve', lambda e: e.tensor_scalar(out=a1n[:], in0=v[:, 6, :], scalar1=1.0, scalar2=None, op0=ALU.add),
             reads=[v], writes=[a1n])
        S.op('dve', lambda e: e.tensor_tensor(out=a1n[:], in0=a1n[:], in1=v[:, 5, :], op=ALU.mult), reads=[a1n, v], writes=[a1n])
        outs = []
        if post:
            yT = S.dram_in("yT", [128, 8, NTOK], BF16)
            wo = S.dram_in("wo", [8, 128, 8, 128], BF16)
            wg = S.dram_in("wg", [MFF, 128, 8, 128], BF16)
            wu = S.dram_in("wu", [MFF, 128, 8, 128], BF16)
            wd = S.dram_in("wd", [8, 128, MFF, 128], BF16)
            xo = S.dram_out("xo", [128, 8, NTOK], F32)
            outs.append(xo)
            yt = [S.tile([128, 8, NTT], BF16) for _ in range(1)]
            hn2 = S.tile([128, 8, NTT], BF16)
            A = S.tile([128, MFF, NTT], BF16)
            wot = [S.tile([128, 8, 128], BF16) for _ in range(2)]
            wgt = [S.tile([128, 8, 128], BF16) for _ in range(2)]
            wut = [S.tile([128, 8, 128], BF16) for _ in range(2)]
            wdt = [S.tile([128, MFF, 128], BF16) for _ in range(2)]
            sgt = [S.tile([128, 512], F32) for _ in range(2)]
        if nextnorm:
            hno = S.dram_out("hno", [128, 8, NTOK], BF16)
            outs.append(hno)
            hnn = [S.tile([128, 8, NTT], BF16) for _ in range(1)]
        xt = [S.tile([128, 8, NTT], F32) for _ in range(2)]
        for ti in range(NTOK // NTT):
            ts = slice(ti * NTT, (ti + 1) * NTT)
            x = xt[ti % 2]
            S.dma(x[:], xT[:, :, ts], reads=[xT], writes=[x])
            if post:
                y = yt[0]
                S.dma(y[:], yT[:, :, ts], reads=[yT], writes=[y])
                wi = 0
                for m in range(8):
                    w = wot[m % 2]
                    S.dma(w[:], wo[m], reads=[wo], writes=[w])
                    for h0 in range(0, NTT, 512):
                        hs = slice(h0, h0 + 512)
                        ps = nextbank()
                        for k in range(8):
                            S.op('pe', lambda e, k=k: e.matmul(ps[:, :], w[:, k, :], y[:, k, hs], start=(k == 0), stop=(k == 7)),
                                 reads=[w, y], writes=[ps])
                        S.op('dve', lambda e: e.scalar_tensor_tensor(out=x[:, m, hs], in0=ps[:, :], scalar=v[:, 0, m:m + 1],
                                                                     in1=x[:, m, hs], op0=ALU.mult, op1=ALU.add),
                             reads=[ps, v, x], writes=[x])
                emit_norm_mod(S, C, x, hn2, a2, v, 3, NTT)
                for m in range(MFF):
                    w1 = wgt[m % 2]
                    w2 = wut[m % 2]
                    S.dma(w1[:], wg[m], reads=[wg], writes=[w1])
                    S.dma(w2[:], wu[m], reads=[wu], writes=[w2])
                    for h0 in range(0, NTT, 512):
                        hs = slice(h0, h0 + 512)
                        pg = nextbank()
                        pu = nextbank()
                        for k in range(8):
                            S.op('pe', lambda e, k=k: e.matmul(pg[:, :], w1[:, k, :], hn2[:, k, hs], start=(k == 0), stop=(k == 7)),
                                 reads=[w1, hn2], writes=[pg])
                        for k in range(8):
                            S.op('pe', lambda e, k=k: e.matmul(pu[:, :], w2[:, k, :], hn2[:, k, hs], start=(k == 0), stop=(k == 7)),
                                 reads=[w2, hn2], writes=[pu])
                        sg = sgt[(h0 // 512) % 2]
                        S.op('act', lambda e: e.activation(out=sg[:], in_=pg[:, :], func=AF.Silu), reads=[pg], writes=[sg])
                        S.op('dve', lambda e: e.tensor_tensor(out=A[:, m, hs], in0=pu[:, :], in1=sg[:], op=ALU.mult),
                             reads=[pu, sg], writes=[A])
                for f in range(8):
                    w = wdt[f % 2]
                    S.dma(w[:], wd[f], reads=[wd], writes=[w])
                    for h0 in range(0, NTT, 512):
                        hs = slice(h0, h0 + 512)
                        ps = nextbank()
                        for m in range(MFF):
                            S.op('pe', lambda e, m=m: e.matmul(ps[:, :], w[:, m, :], A[:, m, hs], start=(m == 0), stop=(m == MFF - 1)),
                                 reads=[w, A], writes=[ps])
                        S.op('dve', lambda e: e.scalar_tensor_tensor(out=x[:, f, hs], in0=ps[:, :], scalar=v[:, 4, f:f + 1],
                                                                     in1=x[:, f, hs], op0=ALU.mult, op1=ALU.add),
                             reads=[ps, v, x], writes=[x])
                S.dma(xo[:, :, ts], x[:], reads=[x], writes=[xo])
            if nextnorm:
                hq = hnn[0]
                emit_norm_mod(S, C, x, hq, a1n, v, 7, NTT)
                S.dma(hno[:, :, ts], hq[:], reads=[hq], writes=[hno])
        S.finish(outs)
    return nc


def pk8(vv):
    return np.ascontiguousarray(vv.reshape(8, 128).T)


def dense_weights(ws_l):
    wo = ws_l['w_out']
    wgu = ws_l['w_gu']
    wdn = ws_l['w_down']
    wo_t = np.ascontiguousarray(wo.reshape(128, 8, 8, 128).transpose(2, 0, 1, 3))
    wg_t = np.ascontiguousarray(wgu[:, :, :DFF].reshape(128, 8, MFF, 128).transpose(2, 0, 1, 3))
    wu_t = np.ascontiguousarray(wgu[:, :, DFF:].reshape(128, 8, MFF, 128).transpose(2, 0, 1, 3))
    wd_t = np.ascontiguousarray(wdn.reshape(128, MFF, 8, 128).transpose(2, 0, 1, 3))
    return {"wo": wo_t, "wg": wg_t, "wu": wu_t, "wd": wd_t}


DEBUG = False
MODQ = 6 * D // 4
MODCH = MODQ // 128
UROWS = 320
TWO = [[0, 1], [2, 3], [4, 5], [4, 5]]


CW = 4096


def plan_dense_layout():
    tiles = []
    for l in range(DEPTH):
        tiles += [(('wo', l, m), 1024) for m in range(8)]
        tiles += [((nm, l, m), 1024) for m in range(MFF) for nm in ('wg', 'wu')]
        tiles += [(('wd', l, f), MFF * 128) for f in range(8)]
    nchq = 1
    while True:
        pos = {}
        q, ch, off = 0, 0, 0
        ok = True
        for key, n in tiles:
            if off + n > CW:
                ch += 1
                off = 0
                if ch == nchq:
                    q += 1
                    ch = 0
            if q > 3:
                ok = False
                break
            pos[key] = (q, ch, off)
            off += n
        if ok:
            return nchq * CW, pos
        nchq += 1


WQ, WPOS = plan_dense_layout()


def build_fused(debug=False):
    nc = new_nc()
    with ExitStack() as es:
        S = Sched(nc, es)
        banks = [S.psum([128, 512], F32) for _ in range(8)]
        xT = S.dram_in("xT", [128, 8, NTOK], F32)
        cT = S.dram_in("cT", [128, 8, 1], F32)
        wada = S.dram_in("wada", [128, DEPTH, 8, MODQ], F32)
        bada = S.dram_in("bada", [128, DEPTH * MODCH], F32)
        wf = S.dram_in("wf", [128, WQ], F32)
        ngin = S.dram_in("ngin", [128, DEPTH, 2, 8], F32)
        yidx_d = S.dram_in("yidx", [128, NTOK // NTT, 8], mybir.dt.int32)
        fdm = S.dram_in("fox_dm", [128, 4, 512], F32)
        bdm = S.dram_in("moba_dm", [128, 4, 512], F32)
        boh = S.dram_in("moba_oh", [32, T], BF16)
        idd = S.dram_in("ident", [128, 128], F32)
        mU = S.dram_in("ml_U", [128, 128], F32)
        uin = {}
        for l in range(DEPTH):
            for u, kind in enumerate(['fox', 'fox', 'ml', 'moba', 'moba']):
                pf = "L%du%d_" % (l, u)
                d = {}
                if kind == 'fox':
                    d['wu'] = S.dram_in(pf + "wu", [128, 8, 200], F32)
                    d['g'] = S.dram_in(pf + "g", [64, 2], F32)
                    d['fb'] = S.dram_in(pf + "fb", [65, 1], F32)
                elif kind == 'moba':
                    d['wu'] = S.dram_in(pf + "wu", [128, 8, 200], F32)
                    d['g'] = S.dram_in(pf + "g", [64, 2], F32)
                    d['qc'] = S.dram_in(pf + "qc", [8, T], BF16)
                    d['kc'] = S.dram_in(pf + "kc", [8, T], BF16)
                else:
                    d['wu'] = S.dram_in(pf + "wu", [128, 8, 260], F32)
                    d['cw'] = S.dram_in(pf + "cw", [64, 2, 4], F32)
                    d['cb'] = S.dram_in(pf + "cb", [64, 2], F32)
                    d['ib'] = S.dram_in(pf + "ib", [128, 1], F32)
                    d['fb'] = S.dram_in(pf + "fb", [128, 1], F32)
                    d['mg'] = S.dram_in(pf + "mg", [128, 4, 64], F32)
                uin[(l, u)] = d
        xo = S.dram_out("xo", [128, 8, NTOK], F32)
        modl = S.dram_int("modl", [128, DEPTH * MODCH], F32)
        moda = S.dram_int("moda", [512, DEPTH * MODCH], F32)
        NWCH = WQ // CW
        wbl = S.dram_int("wbl", [NWCH * 128, CW], BF16)
        wba = S.dram_int("wba", [NWCH * 512, CW], BF16)
        hnl = S.dram_int("hnl", [8 * 128, NTOK], BF16)
        hna = S.dram_int("hna", [8 * 512, NTOK], BF16)
        yl = S.dram_int("yl", [UROWS * 4, NTOK], BF16)
        ya = S.dram_int("ya", [UROWS * 16, NTOK], BF16)
        xs = S.dram_int("xs", [128, 8, NTOK], F32)
        ylv = yl.t.rearrange("(u q) t -> u (q t)", q=4)
        hnlv = hnl.t.rearrange("(k p) t -> p k t", k=8)
        yav = ya.t.rearrange("r (a t) -> (r a) t", a=NTOK // NTT)

        vecall = S.tile([128, 4, DEPTH * MODCH], F32)
        ng = S.tile([128, DEPTH, 2, 8], F32)
        yidx = S.tile([128, NTOK // NTT, 8], mybir.dt.int32)
        S.dma(ng[:], ngin[:], reads=[ngin], writes=[ng])
        S.dma(yidx[:], yidx_d[:], reads=[yidx_d], writes=[yidx])
        ones = S.tile([128, 128], F32)
        S.op('dve', lambda e: e.memset(ones[:], 1.0), writes=[ones])
        epsc = S.tile([128, 1], F32)
        S.op('dve', lambda e: e.memset(epsc[:], EPS), writes=[epsc])
        vts = [S.tile([128, 10, 8], F32) for _ in range(DEPTH + 1)]
        a_t = [S.tile([128, 2, 8], F32) for _ in range(DEPTH + 1)]

        with ExitStack() as es2:
            S.es = es2
            c_sb = S.tile([128, 8, 1], F32)
            sg_sb = S.tile([128, 8, 1], F32)
            sc_sb = S.tile([128, 8, 1], F32)
            b_sb = S.tile([128, DEPTH * MODCH], F32)
            o_sb = S.tile([128, DEPTH * MODCH], F32)
            S.dma(c_sb[:], cT[:], reads=[cT], writes=[c_sb])
            S.dma(b_sb[:], bada[:], reads=[bada], writes=[b_sb])
            S.op('act', lambda e: e.activation(out=sg_sb[:], in_=c_sb[:], func=AF.Sigmoid), reads=[c_sb], writes=[sg_sb])
            S.op('dve', lambda e: e.tensor_tensor(out=sc_sb[:], in0=c_sb[:], in1=sg_sb[:], op=ALU.mult),
                 reads=[c_sb, sg_sb], writes=[sc_sb])
            wt = S.tile([128, 8, MODQ], F32)
            psm = banks[0]
            for l in range(DEPTH):
                S.dma(wt[:], wada[:, l, :, :], reads=[wada], writes=[wt])
                for ci in range(MODCH):
                    col = l * MODCH + ci
                    for k in range(8):
                        S.op('pe', lambda e, k=k: e.matmul(psm[:, col:col + 1], wt[:, k, ci * 128:(ci + 1) * 128],
                                                           sc_sb[:, k, 0:1], start=(k == 0), stop=(k == 7)),
                             reads=[wt, sc_sb], writes=[psm])
            S.op('dve', lambda e: e.tensor_tensor(out=o_sb[:], in0=psm[:, 0:DEPTH * MODCH], in1=b_sb[:], op=ALU.add),
                 reads=[psm, b_sb], writes=[o_sb])
            S.dma(modl[:], o_sb[:], reads=[o_sb], writes=[modl])
            S.collective(modl, moda)
            S.dma(vecall[:], moda.t.rearrange("(r p) c -> p r c", p=128), reads=[moda], writes=[vecall])
            fts = [S.tile([128, WC_TILE], F32) for _ in range(3)]
            bts = [S.tile([128, WC_TILE], BF16) for _ in range(3)]
            for i in range(WQ // WC_TILE):
                ft, bt = fts[i % 3], bts[i % 3]
                sl = slice(i * WC_TILE, (i + 1) * WC_TILE)
                S.dma(ft[:], wf[:, sl], reads=[wf], writes=[ft])
                eng = 'dve' if i % 2 == 0 else 'pool'
                S.op(eng, lambda e: e.tensor_copy(out=bt[:], in_=ft[:]), reads=[ft], writes=[bt])
                ch, co = (i * WC_TILE) // CW, (i * WC_TILE) % CW
                S.dma(wbl[ch * 128:(ch + 1) * 128, co:co + WC_TILE], bt[:], reads=[bt], writes=[wbl])
            S.collective_chunks(wbl, wba, NWCH, 128)
            S.barrier()
        S.es = es

        def vcol(l, j, k):
            ci = j * 8 + k
            return vecall[:, ci // MODCH, l * MODCH + ci % MODCH:l * MODCH + ci % MODCH + 1]

        def fill_vec(vt, at, lpost, lnext):
            S.op('dve', lambda e: e.memset(vt[:], 0.0), writes=[vt])
            items = []
            if lpost is not None:
                items += [(0, lpost, 2), (2, lpost, 4), (3, lpost, 3), (4, lpost, 5)]
                S.op('dve', lambda e: e.tensor_copy(out=vt[:, 1, :], in_=ng[:, lpost, 1, :]), reads=[ng], writes=[vt])
            if lnext is not None:
                items += [(6, lnext, 1), (7, lnext, 0)]
                S.op('dve', lambda e: e.tensor_copy(out=vt[:, 5, :], in_=ng[:, lnext, 0, :]), reads=[ng], writes=[vt])
            for row, l, j in items:
                for k in range(8):
                    S.op('dve', lambda e, k=k: e.tensor_copy(out=vt[:, row, k:k + 1], in_=vcol(l, j, k)),
                         reads=[vecall], writes=[vt])
            for ai, (srow, grow) in enumerate(((2, 1), (6, 5))):
                S.op('dve', lambda e: e.tensor_scalar(out=at[:, ai, :], in0=vt[:, srow, :], scalar1=1.0, scalar2=None,
                                                      op0=ALU.add), reads=[vt], writes=[at])
                S.op('dve', lambda e: e.tensor_tensor(out=at[:, ai, :], in0=at[:, ai, :], in1=vt[:, grow, :],
                                                      op=ALU.mult), reads=[at, vt], writes=[at])

        fill_vec(vts[0], a_t[0], None, 0)
        for l in range(DEPTH):
            fill_vec(vts[l + 1], a_t[l + 1], l, l + 1 if l + 1 < DEPTH else None)

        def dense_phase(l, post, nextnorm, xsrc, xdst):
            v = vts[0] if not post else vts[l + 1]
            at = a_t[0] if not post else a_t[l + 1]
            with ExitStack() as es2:
                S.es = es2
                C = MixCtx()
                C.bi = 0

                def nextbank():
                    C.bi += 1
                    return banks[C.bi % 8]
                C.nextbank = nextbank
                C.ones, C.epsc = ones, epsc
                C.sq = [S.tile([128, 512], F32) for _ in range(2)]
                C.t = [S.tile([128, 512], F32) for _ in range(2)]
                C.rs = S.tile([128, 512], F32)
                a2 = View(at, at.t[:, 0, :])
                a1n = View(at, at.t[:, 1, :])
                if post:
                    yt = S.tile([128, 8, NTT], BF16)
                    hn2 = S.tile([128, 8, NTT], BF16)
                    A = S.tile([128, MFF, NTT], BF16)
                    wot = [S.tile([128, 8, 128], BF16) for _ in range(2)]
                    wgt = [S.tile([128, 8, 128], BF16) for _ in range(2)]
                    wut = [S.tile([128, 8, 128], BF16) for _ in range(2)]
                    wdt = [S.tile([128, MFF, 128], BF16) for _ in range(2)]
                    sgt = [S.tile([128, 512], F32) for _ in range(2)]

                    def wsrc(key, kk):
                        q, ch, off = WPOS[key]
                        r0 = ch * 512 + q * 128
                        return wba.t[r0:r0 + 128, off:off + kk * 128].rearrange("p (k j) -> p k j", k=kk)
                if nextnorm:
                    hq = S.tile([128, 8, NTT], BF16)
                xt = [S.tile([128, 8, NTT], F32) for _ in range(2)]
                for ti in range(NTOK // NTT):
                    ts = slice(ti * NTT, (ti + 1) * NTT)
                    x = xt[ti % 2]
                    S.dma(x[:], xsrc[:, :, ts], reads=[xsrc], writes=[x])
                    if post:
                        y = yt
                        for k in range(8):
                            S.idma(y[:, k, :], yav, yidx[:, ti, k:k + 1], reads=[ya, yidx], writes=[y])
                        for m in range(8):
                            w = wot[m % 2]
                            S.dma(w[:], wsrc(('wo', l, m), 8), reads=[wba], writes=[w])
                            for h0 in range(0, NTT, 512):
                                hs = slice(h0, h0 + 512)
                                ps = nextbank()
                                for k in range(8):
                                    S.op('pe', lambda e, k=k: e.matmul(ps[:, :], w[:, k, :], y[:, k, hs], start=(k == 0),
                                                                       stop=(k == 7)), reads=[w, y], writes=[ps])
                                S.op('dve', lambda e: e.scalar_tensor_tensor(out=x[:, m, hs], in0=ps[:, :],
                                                                             scalar=v[:, 0, m:m + 1], in1=x[:, m, hs],
                                                                             op0=ALU.mult, op1=ALU.add),
                                     reads=[ps, v, x], writes=[x])
                        emit_norm_mod(S, C, x, hn2, a2, v, 3, NTT)
                        for m in range(MFF):
                            w1, w2 = wgt[m % 2], wut[m % 2]
                            S.dma(w1[:], wsrc(('wg', l, m), 8), reads=[wba], writes=[w1])
                            S.dma(w2[:], wsrc(('wu', l, m), 8), reads=[wba], writes=[w2])
                            for h0 in range(0, NTT, 512):
                                hs = slice(h0, h0 + 512)
                                pg = nextbank()
                                pu = nextbank()
                                for k in range(8):
                                    S.op('pe', lambda e, k=k: e.matmul(pg[:, :], w1[:, k, :], hn2[:, k, hs], start=(k == 0),
                                                                       stop=(k == 7)), reads=[w1, hn2], writes=[pg])
                                for k in range(8):
                                    S.op('pe', lambda e, k=k: e.matmul(pu[:, :], w2[:, k, :], hn2[:, k, hs], start=(k == 0),
                                                                       stop=(k == 7)), reads=[w2, hn2], writes=[pu])
                                sg = sgt[(h0 // 512) % 2]
                                S.op('act', lambda e: e.activation(out=sg[:], in_=pg[:, :], func=AF.Silu), reads=[pg], writes=[sg])
                                S.op('dve', lambda e: e.tensor_tensor(out=A[:, m, hs], in0=pu[:, :], in1=sg[:], op=ALU.mult),
                                     reads=[pu, sg], writes=[A])
                        for f in range(8):
                            w = wdt[f % 2]
                            S.dma(w[:], wsrc(('wd', l, f), MFF), reads=[wba], writes=[w])
                            for h0 in range(0, NTT, 512):
                                hs = slice(h0, h0 + 512)
                                ps = nextbank()
                                for m in range(MFF):
                                    S.op('pe', lambda e, m=m: e.matmul(ps[:, :], w[:, m, :], A[:, m, hs], start=(m == 0),
                                                                       stop=(m == MFF - 1)), reads=[w, A], writes=[ps])
                                S.op('dve', lambda e: e.scalar_tensor_tensor(out=x[:, f, hs], in0=ps[:, :],
                                                                             scalar=v[:, 4, f:f + 1], in1=x[:, f, hs],
                                                                             op0=ALU.mult, op1=ALU.add),
                                     reads=[ps, v, x], writes=[x])
                        S.dma(xdst[:, :, ts], x[:], reads=[x], writes=[xdst])
                    if nextnorm:
                        emit_norm_mod(S, C, x, hq, a1n, v, 7, NTT)
                        S.dma(hnlv[:, :, ts], hq[:], reads=[hq], writes=[hnl])
                if nextnorm:
                    S.collective_chunks(hnl, hna, 8, 128)
                S.barrier()
            S.es = es

        def mix_phase(l, kinds, slots):
            with ExitStack() as es2:
                S.es = es2
                C = mix_common(S, banks=banks, fused=True)
                if 'fox' in kinds or 'moba' in kinds:
                    attn_alloc(S, C, 'moba' in kinds)
                if 'fox' in kinds:
                    fox_alloc(S, C)
                if 'ml' in kinds:
                    ml_alloc(S, C)
                if 'moba' in kinds:
                    moba_alloc(S, C)
                for kind, u in zip(kinds, slots):
                    d = uin[(l, u)]
                    yout = View(yl, ylv[64 * u:64 * u + 64, :])
                    if kind == 'fox':
                        emit_fox_unit(S, C, hna, d['wu'], d['g'], d['fb'], fdm, yout)
                    elif kind == 'moba':
                        emit_moba_unit(S, C, hna, d['wu'], d['g'], d['qc'], d['kc'], boh, bdm, idd, yout)
                    else:
                        emit_ml_unit(S, C, hna, d['wu'], d['cw'], d['cb'], d['ib'], d['fb'], d['mg'], mU, idd, yout)
                S.barrier()
            S.es = es

        dense_phase(0, False, True, xT, None)
        for l in range(DEPTH):
            mix_phase(l, ['fox', 'fox', 'ml'], [0, 1, 2])
            NCH_A = 192 * 4 // 128
            for ch in range(NCH_A):
                S.collective(yl, ya, yl.t[ch * 128:(ch + 1) * 128, :], ya.t[ch * 512:(ch + 1) * 512, :])
            mix_phase(l, ['moba', 'moba'], [3, 4])
            for ch in range(NCH_A, UROWS * 4 // 128):
                S.collective(yl, ya, yl.t[ch * 128:(ch + 1) * 128, :], ya.t[ch * 512:(ch + 1) * 512, :])
            if debug:
                dy = S.dram_out("dbg_y", [UROWS * 4, NTOK], BF16)
                dh = S.dram_out("dbg_hn", [8 * 128, NTOK], BF16)
                dya = S.dram_out("dbg_ya", [UROWS * 16, NTOK], BF16)
                dv = S.dram_out("dbg_v", [128, 10, 8], F32)
                S.dma(dy[:], yl[:], reads=[yl], writes=[dy])
                S.dma(dh[:], hnl[:], reads=[hnl], writes=[dh])
                S.dma(dya[:], ya[:], reads=[ya], writes=[dya])
                S.dma(dv[:], vts[1][:], reads=[vts[1]], writes=[dv])
                S.finish([dy, dh, dya, dv])
                return nc
            last = l == DEPTH - 1
            dense_phase(l, True, not last, xT if l == 0 else xs, xo if last else xs)
        S.finish([xo])
    return nc


def dense_tiles_f32(inp, l):
    wo = w_to_pk(inp['w_out'][l]).reshape(128, 8, D)
    wgu = w_to_pk(inp['w_gate_up'][l]).reshape(128, 8, 2 * DFF)
    wdn = w_to_pk(inp['w_down'][l]).reshape(128, MFF, D)
    t = {}
    for m in range(8):
        t[('wo', l, m)] = wo[:, :, m * 128:(m + 1) * 128].reshape(128, -1)
        t[('wd', l, m)] = wdn[:, :, m * 128:(m + 1) * 128].reshape(128, -1)
    for m in range(MFF):
        t[('wg', l, m)] = wgu[:, :, m * 128:(m + 1) * 128].reshape(128, -1)
        t[('wu', l, m)] = wgu[:, :, DFF + m * 128:DFF + (m + 1) * 128].reshape(128, -1)
    return t


def kernel(**inp):
    inp = {k: np.asarray(v) for k, v in inp.items()}
    x = inp['x']
    wq = [np.zeros((128, WQ), np.float32) for _ in range(4)]
    for l in range(DEPTH):
        for key, arr in dense_tiles_f32(inp, l).items():
            q, ch, off = WPOS[key]
            wq[q][:, ch * CW + off:ch * CW + off + arr.shape[1]] = arr
    w_in_pk = [w_to_pk(inp['w_in'][l]).reshape(128, 8, IN_COLS) for l in range(DEPTH)]
    consts = {"fox_dm": fox_dmask(), "moba_dm": moba_dmask(), "moba_oh": moba_onehot(),
              "ident": np.eye(128, dtype=np.float32), "ml_U": ml_consts()}
    ngin = np.ascontiguousarray(np.stack([np.stack([pk8(inp['norm1_g'][l]), pk8(inp['norm2_g'][l])], axis=1)
                                          for l in range(DEPTH)], axis=1)).astype(np.float32)
    in_maps = []
    for c in range(NCORE):
        b, r = c // 4, c % 4
        m = dict(consts)
        m["xT"] = to_fm(x[b, r * NTOK:(r + 1) * NTOK, :])
        m["cT"] = np.ascontiguousarray(inp['c'][b].reshape(8, 128).T)[:, :, None].astype(np.float32)
        wsl = inp['w_ada'][:, :, r * MODQ:(r + 1) * MODQ]
        m["wada"] = np.ascontiguousarray(wsl.reshape(DEPTH, 8, 128, MODQ).transpose(2, 0, 1, 3))
        bsl = inp['b_ada'][:, r * MODQ:(r + 1) * MODQ].reshape(DEPTH, MODCH, 128)
        m["bada"] = np.ascontiguousarray(bsl.transpose(2, 0, 1)).reshape(128, DEPTH * MODCH).astype(np.float32)
        m["wf"] = wq[r]
        m["ngin"] = ngin
        idx = np.zeros((D,), np.int32)
        for f in range(D):
            if f < 384:
                h, dd = f // 64, f % 64
                rk, ur = min(h // 2, 2), 64 * (h % 2) + dd
            elif f < 640:
                h, dd = (f - 384) // 64, f % 64
                rk, ur = h, 128 + dd
            else:
                h, dd = (f - 640) // 64, f % 64
                rk, ur = min(h // 2, 2), 192 + 64 * (h % 2) + dd
            lr = ur * 4 + r
            idx[f] = (lr // 128) * 512 + rk * 128 + lr % 128
        nti = NTOK // NTT
        idx2 = idx.reshape(8, 128).T
        m["yidx"] = np.ascontiguousarray(np.stack([idx2 * nti + ti for ti in range(nti)], axis=1)).astype(np.int32)
        for l in range(DEPTH):
            ws_l = {'w_in': w_in_pk[l]}
            for u, kind in enumerate(['fox', 'fox', 'ml', 'moba', 'moba']):
                pf = "L%du%d_" % (l, u)
                if kind == 'fox':
                    dd_ = fox_unit_inputs(0, ws_l, inp, l, TWO[r][u])
                    m[pf + "wu"], m[pf + "g"], m[pf + "fb"] = dd_["wu0"], dd_["g0"], dd_["fb0"]
                elif kind == 'moba':
                    dd_ = moba_unit_inputs(0, ws_l, inp, l, TWO[r][u - 3])
                    m[pf + "wu"], m[pf + "g"], m[pf + "qc"], m[pf + "kc"] = dd_["wu0"], dd_["g0"], dd_["qc0"], dd_["kc0"]
                else:
                    dd_ = ml_unit_inputs(0, ws_l, inp, l, r)
                    for nm in ("wu", "cw", "cb", "ib", "fb", "mg"):
                        m[pf + nm] = dd_[nm + "0"]
        in_maps.append(m)
    if DEBUG:
        return run_spmd(build_fused(True), in_maps)
    res = run_spmd(build_fused(), in_maps)
    out = np.zeros((B, T, D), np.float32)
    for c in range(NCORE):
        b, q = c // 4, c % 4
        out[b, q * NTOK:(q + 1) * NTOK, :] = res[c]["xo"].transpose(2, 1, 0).reshape(NTOK, D)
    return out
```
